# Optimizing a Trainium2 kernel written in Bass

```python
import jax, jax.numpy as jnp
from jax import lax
import numpy as np

D_MODEL = 1024
BATCH = 8
SEQ = 4096
DEPTH = 1

N_HEADS = 8
N_KV_HEADS = 2
HEAD_DIM = 64
GQA_GROUP = N_HEADS // N_KV_HEADS
ATTN_DIM = N_HEADS * HEAD_DIM
KV_DIM = N_KV_HEADS * HEAD_DIM
WINDOW = 128
ATTN_BLOCK = 128
ROT_DIM = HEAD_DIM // 4
ROPE_THETA = 500000.0
CONV_DIM = D_MODEL // 2
CONV_WIDTH = 3
SPLIT_SIZES = (ATTN_DIM, KV_DIM, KV_DIM, CONV_DIM, CONV_DIM, CONV_DIM, D_MODEL, D_MODEL)
PROJ_WIDTH = sum(SPLIT_SIZES)
SPLIT_POINTS = tuple(int(v) for v in np.cumsum(SPLIT_SIZES)[:-1])
N_GROUPS = 4
EXPERTS_PER_GROUP = 8
N_EXPERTS = N_GROUPS * EXPERTS_PER_GROUP
TOP_K = 2
D_FF_EXPERT = D_MODEL // 4
MOE_BLOCK = 128
NORM_EPS = 1e-6
MASK_VALUE = -1e30

kernel_name = "hybrid_swa_shortconv_hiermoe_encoder"


def rmsnorm(x, g):
    xf = x.astype(jnp.float32)
    xf = xf * lax.rsqrt(jnp.mean(xf * xf, axis=-1, keepdims=True) + NORM_EPS)
    return xf.astype(x.dtype) * g


def rope_tables(seq_len):
    inv_freq = ROPE_THETA ** (-jnp.arange(0, ROT_DIM, 2, dtype=jnp.float32) / ROT_DIM)
    ang = jnp.arange(seq_len, dtype=jnp.float32)[:, None] * inv_freq[None, :]
    return jnp.cos(ang), jnp.sin(ang)


def partial_rope(x, cos, sin):
    half = ROT_DIM // 2
    c = cos[None, :, None, :].astype(x.dtype)
    s = sin[None, :, None, :].astype(x.dtype)
    x1 = x[..., :half]
    x2 = x[..., half:ROT_DIM]
    return jnp.concatenate([x1 * c - x2 * s, x2 * c + x1 * s, x[..., ROT_DIM:]], axis=-1)


def band_keys(t):
    b, s, hk, hd = t.shape
    nb = s // ATTN_BLOCK
    tp = jnp.pad(t, ((0, 0), (WINDOW, WINDOW), (0, 0), (0, 0))).reshape(b, nb + 2, ATTN_BLOCK, hk, hd)
    return jnp.concatenate([tp[:, :-2], tp[:, 1:-1], tp[:, 2:]], axis=2)


def windowed_gqa(q, k, v, sink):
    b, s, _, hd = q.shape
    nb = s // ATTN_BLOCK
    qb = q.reshape(b, nb, ATTN_BLOCK, N_KV_HEADS, GQA_GROUP, hd)
    kw = band_keys(k)
    vw = band_keys(v)
    scores = jnp.einsum('bnqhgd,bnkhd->bnhgqk', qb, kw,
                        preferred_element_type=jnp.float32) * (HEAD_DIM ** -0.5)
    blk = jnp.arange(nb)[:, None, None] * ATTN_BLOCK
    qpos = blk + jnp.arange(ATTN_BLOCK)[None, :, None]
    kpos = blk - WINDOW + jnp.arange(3 * ATTN_BLOCK)[None, None, :]
    valid = (jnp.abs(qpos - kpos) <= WINDOW) & (kpos >= 0) & (kpos < s)
    scores = jnp.where(valid[None, :, None, None], scores, MASK_VALUE)
    sink_b = sink.astype(jnp.float32).reshape(1, 1, N_KV_HEADS, GQA_GROUP, 1, 1)
    m = jnp.maximum(jnp.max(scores, axis=-1, keepdims=True), sink_b)
    e = jnp.exp(scores - m)
    p = e / (jnp.sum(e, axis=-1, keepdims=True) + jnp.exp(sink_b - m))
    o = jnp.einsum('bnhgqk,bnkhd->bnqhgd', p.astype(v.dtype), vw)
    return o.reshape(b, s, N_HEADS * hd)


def short_gated_conv(gate_b, gate_c, xc, conv_w, conv_b):
    u = gate_c * xc
    up = jnp.pad(u, ((0, 0), (1, 1), (0, 0)))
    conv = up[:, :-2] * conv_w[0] + up[:, 1:-1] * conv_w[1] + up[:, 2:] * conv_w[2] + conv_b
    return gate_b * conv


def hierarchical_moe(h, w_rg, b_rg, w_re, b_re, w_gate, w_up, w_down):
    b, s, d = h.shape
    t = b * s
    hf = h.reshape(t, d)
    hf32 = hf.astype(jnp.float32)
    g_logits = hf32 @ w_rg.astype(jnp.float32) + b_rg.astype(jnp.float32)
    grp = jnp.argmax(g_logits, axis=-1)
    p_grp = jnp.take_along_axis(jax.nn.softmax(g_logits, axis=-1), grp[:, None], axis=-1)
    e_logits = (hf32 @ w_re.astype(jnp.float32) + b_re.astype(jnp.float32)).reshape(t, N_GROUPS, EXPERTS_PER_GROUP)
    e_in = jnp.take_along_axis(e_logits, grp[:, None, None], axis=1)[:, 0]
    top_p, top_i = lax.top_k(jax.nn.softmax(e_in, axis=-1), TOP_K)
    top_p = top_p / jnp.sum(top_p, axis=-1, keepdims=True)
    gates = (p_grp * top_p).reshape(t * TOP_K)
    expert = (grp[:, None] * EXPERTS_PER_GROUP + top_i).reshape(t * TOP_K)
    n_assign = t * TOP_K
    order = jnp.argsort(expert)
    e_sorted = expert[order]
    tok = order // TOP_K
    counts = jnp.bincount(expert, length=N_EXPERTS)
    starts = jnp.cumsum(counts) - counts
    padded = ((counts + MOE_BLOCK - 1) // MOE_BLOCK) * MOE_BLOCK
    pad_ends = jnp.cumsum(padded)
    pad_starts = pad_ends - padded
    dest = pad_starts[e_sorted] + (jnp.arange(n_assign) - starts[e_sorted])
    n_rows = n_assign + N_EXPERTS * MOE_BLOCK
    n_blocks = n_rows // MOE_BLOCK
    x_pad = jnp.zeros((n_rows, d), h.dtype).at[dest].set(hf[tok])
    block_e = jnp.minimum(jnp.searchsorted(pad_ends, jnp.arange(n_blocks) * MOE_BLOCK, side='right'),
                          N_EXPERTS - 1)

    def expert_block(args):
        xb, e = args
        hid = jax.nn.silu(xb @ w_gate[e]) * (xb @ w_up[e])
        return hid @ w_down[e]

    y_pad = lax.map(expert_block, (x_pad.reshape(n_blocks, MOE_BLOCK, d), block_e)).reshape(n_rows, d)
    y = y_pad[dest] * gates[order][:, None].astype(h.dtype)
    out = jnp.zeros((t, d), h.dtype).at[tok].add(y)
    return out.reshape(b, s, d)


def setup_inputs(seed: int = 0) -> dict:
    key = jax.random.key(seed)
    ks = jax.random.split(key, 20)
    f32 = jnp.float32

    def nrm(k, shape, fan_in):
        return jax.random.normal(k, shape, f32) * (fan_in ** -0.5)

    def gain(k, shape):
        return 1.0 + 0.02 * jax.random.normal(k, shape, f32)

    return {
        "x": jax.random.normal(ks[0], (BATCH, SEQ, D_MODEL), f32),
        "norm1_g": gain(ks[1], (DEPTH, D_MODEL)),
        "w_in": nrm(ks[2], (DEPTH, D_MODEL, PROJ_WIDTH), D_MODEL),
        "q_norm_g": gain(ks[3], (DEPTH, HEAD_DIM)),
        "k_norm_g": gain(ks[4], (DEPTH, HEAD_DIM)),
        "attn_sink": 0.5 * jax.random.normal(ks[5], (DEPTH, N_HEADS), f32),
        "conv_w": nrm(ks[6], (DEPTH, CONV_WIDTH, CONV_DIM), CONV_WIDTH),
        "conv_b": 0.02 * jax.random.normal(ks[7], (DEPTH, CONV_DIM), f32),
        "w_attn_proj": nrm(ks[8], (DEPTH, ATTN_DIM, D_MODEL), ATTN_DIM),
        "w_conv_proj": nrm(ks[9], (DEPTH, CONV_DIM, D_MODEL), CONV_DIM),
        "w_out": nrm(ks[10], (DEPTH, D_MODEL, D_MODEL), D_MODEL),
        "norm2_g": gain(ks[11], (DEPTH, D_MODEL)),
        "w_router_group": nrm(ks[12], (DEPTH, D_MODEL, N_GROUPS), D_MODEL),
        "b_router_group": 0.01 * jax.random.normal(ks[13], (DEPTH, N_GROUPS), f32),
        "w_router_expert": nrm(ks[14], (DEPTH, D_MODEL, N_EXPERTS), D_MODEL),
        "b_router_expert": 0.01 * jax.random.normal(ks[15], (DEPTH, N_EXPERTS), f32),
        "w_gate_e": nrm(ks[16], (DEPTH, N_EXPERTS, D_MODEL, D_FF_EXPERT), D_MODEL),
        "w_up_e": nrm(ks[17], (DEPTH, N_EXPERTS, D_MODEL, D_FF_EXPERT), D_MODEL),
        "w_down_e": nrm(ks[18], (DEPTH, N_EXPERTS, D_FF_EXPERT, D_MODEL), D_FF_EXPERT),
    }


def reference(x, norm1_g, w_in, q_norm_g, k_norm_g, attn_sink, conv_w, conv_b,
              w_attn_proj, w_conv_proj, w_out, norm2_g, w_router_group, b_router_group,
              w_router_expert, b_router_expert, w_gate_e, w_up_e, w_down_e):
    b, s, _ = x.shape
    cos, sin = rope_tables(s)
    for l in range(DEPTH):
        h = rmsnorm(x, norm1_g[l])
        proj = h @ w_in[l]
        q, k, v, cb, cc, cx, g_attn, g_conv = jnp.split(proj, SPLIT_POINTS, axis=-1)
        q = rmsnorm(q.reshape(b, s, N_HEADS, HEAD_DIM), q_norm_g[l])
        k = rmsnorm(k.reshape(b, s, N_KV_HEADS, HEAD_DIM), k_norm_g[l])
        v = v.reshape(b, s, N_KV_HEADS, HEAD_DIM)
        q = partial_rope(q, cos, sin)
        k = partial_rope(k, cos, sin)
        attn = windowed_gqa(q, k, v, attn_sink[l])
        conv = short_gated_conv(cb, cc, cx, conv_w[l], conv_b[l])
        merged = (jax.nn.sigmoid(g_attn) * (attn @ w_attn_proj[l])
                  + jax.nn.sigmoid(g_conv) * (conv @ w_conv_proj[l]))
        x = x + merged @ w_out[l]
        h2 = rmsnorm(x, norm2_g[l])
        x = x + hierarchical_moe(h2, w_router_group[l], b_router_group[l],
                                 w_router_expert[l], b_router_expert[l],
                                 w_gate_e[l], w_up_e[l], w_down_e[l])
    return x
```

```python
import contextlib
import numpy as np
import concourse.bass as bass
import concourse.mybir as mybir
from concourse.bass_utils import run_bass_kernel_spmd

F32 = mybir.dt.float32
BF16 = mybir.dt.bfloat16
AF = mybir.ActivationFunctionType
ALU = mybir.AluOpType
AX = mybir.AxisListType

PE, ACT, DVE, POOL, SP = "pe", "act", "dve", "pool", "sp"
CAL = {}
USE_CAL = False

D = 1024
S = 4096
NT = S // 128
T = 256
SUB = T // 128
NS = S // T
PW = 4352
Q0, K0, V0, CB0, CC0, CX0, GA0, GC0 = 0, 512, 640, 768, 1280, 1792, 2304, 3328
NE = 32
EPS = 1e-6
KVR = 8
UR = 3
RB = 256
SB = RB // 128
NB = (2 * S) // RB + NE
NROW = NB * RB


class Prog:
    def __init__(self):
        self.ops = []
        self.res = {}

    DEF_COST = {PE: 200.0, ACT: 700.0, DVE: 450.0, POOL: 1200.0, SP: 100.0}

    def op(self, eng, fn, reads=(), writes=(), dma=None, cost=None, lat=None):
        if cost is None:
            cost = 100.0 if dma is not None else self.DEF_COST[eng]
        if lat is None:
            lat = 3500.0 if dma is not None else 0.0
        o = dict(id=len(self.ops), eng=eng, fn=fn, dma=dma, deps=set(), sig=None, used=False, cost=cost, lat=lat)
        try:
            cal = CAL.get(fn.__code__.co_firstlineno)
            if cal is not None and dma is None and USE_CAL:
                o["cost"] = float(cal)
        except Exception:
            pass
        for r in reads:
            st = self.res.setdefault(r, [None, []])
            if st[0] is not None:
                o["deps"].add(st[0]["id"])
        for w in writes:
            st = self.res.setdefault(w, [None, []])
            if st[0] is not None:
                o["deps"].add(st[0]["id"])
            for rd in st[1]:
                o["deps"].add(rd["id"])
        for r in reads:
            self.res[r][1].append(o)
        for w in writes:
            self.res[w] = [o, []]
        o["deps"].discard(o["id"])
        self.ops.append(o)
        return o

    def barrier(self, eng, fn, extra=()):
        last = {}
        for o in self.ops:
            key = ("dma", o["dma"]) if o["dma"] is not None else ("eng", o["eng"])
            last[key] = o["id"]
        o = dict(id=len(self.ops), eng=eng, fn=fn, dma=None, deps=set(last.values()), sig=None, used=False, cost=100.0, lat=0.0)
        for r in list(self.res.keys()) + list(extra):
            self.res[r] = [o, []]
        self.ops.append(o)
        return o

    def schedule(self):
        import heapq
        ops = self.ops
        n = len(ops)
        ndeps = [len(o["deps"]) for o in ops]
        users = [[] for _ in range(n)]
        for o in ops:
            for d in o["deps"]:
                users[d].append(o["id"])
        ready = [0.0] * n
        fin = [0.0] * n
        heaps = {PE: [], ACT: [], DVE: [], POOL: [], SP: []}
        free = {PE: 0.0, ACT: 0.0, DVE: 0.0, POOL: 0.0, SP: 0.0}
        for o in ops:
            if ndeps[o["id"]] == 0:
                heapq.heappush(heaps[o["eng"]], (0.0, o["id"]))
        order = []
        SEM = 400.0
        while len(order) < n:
            best = None
            for eng, h in heaps.items():
                if not h:
                    continue
                t = max(free[eng], h[0][0])
                if best is None or t < best[0]:
                    best = (t, eng)
            t, eng = best
            h = heaps[eng]
            cand = []
            while h and h[0][0] <= t:
                cand.append(heapq.heappop(h))
            pick = min(cand, key=lambda c: c[1])
            for c in cand:
                if c is not pick:
                    heapq.heappush(h, c)
            i = pick[1]
            o = ops[i]
            start = max(free[eng], ready[i])
            free[eng] = start + o["cost"]
            fin[i] = start + o["cost"] + o["lat"]
            order.append(i)
            for u in users[i]:
                ready[u] = max(ready[u], fin[i] + SEM)
                ndeps[u] -= 1
                if ndeps[u] == 0:
                    heapq.heappush(heaps[ops[u]["eng"]], (ready[u], u))
        self.model_time = max(fin)
        return order

    def emit(self, nc, reorder=True):
        ops = self.ops
        order = self.schedule() if reorder else list(range(len(ops)))
        ops_sched = [ops[i] for i in order]
        for o in ops:
            if o["eng"] == PE and o["dma"] is None:
                o["deps"] = {d for d in o["deps"]
                             if not (ops[d]["eng"] == PE and ops[d]["dma"] is None)}
            for d in o["deps"]:
                ops[d]["used"] = True
        semkeys = {}
        counts = {}
        for o in ops_sched:
            key = ("dma", o["dma"]) if o["dma"] is not None else ("eng", o["eng"])
            if o["dma"] is not None or o["used"]:
                inc = 16 if o["dma"] is not None else 1
                counts[key] = counts.get(key, 0) + inc
                o["sig"] = (key, counts[key], inc)
                semkeys[key] = None
        with contextlib.ExitStack() as es:
            for i, key in enumerate(semkeys):
                semkeys[key] = es.enter_context(nc.semaphore("s%d" % i))
            block = es.enter_context(nc.Block())
            per_eng = {PE: [], ACT: [], DVE: [], POOL: [], SP: []}
            for o in ops_sched:
                per_eng[o["eng"]].append(o)

            def mk(eng_name, eng_ops):
                def body(e):
                    known = {}
                    for o in eng_ops:
                        need = {}
                        for d in o["deps"]:
                            key, val, _ = ops[d]["sig"]
                            if need.get(key, 0) < val:
                                need[key] = val
                        for key, val in need.items():
                            if known.get(key, 0) < val:
                                e.wait_ge(semkeys[key], val)
                                known[key] = val
                        ins = o["fn"](e)
                        if o["sig"] is not None:
                            key, val, inc = o["sig"]
                            ins.then_inc(semkeys[key], inc)
                    if eng_name == SP:
                        for key, val in counts.items():
                            if known.get(key, 0) < val:
                                e.wait_ge(semkeys[key], val)
                return body

            reg = {PE: block.tensor, ACT: block.scalar, DVE: block.vector,
                   POOL: block.gpsimd, SP: block.sync}
            for eng_name, eng_ops in per_eng.items():
                reg[eng_name](mk(eng_name, eng_ops))
        return len(semkeys)


class Arena:
    def __init__(self, ap, size):
        self.ap = ap
        self.size = size
        self.off = 0

    def alloc(self, cols, dt=BF16):
        if dt == F32:
            self.off += self.off & 1
            a = self.ap[:, self.off:self.off + 2 * cols].bitcast(F32)
            self.off += 2 * cols
        else:
            a = self.ap[:, self.off:self.off + cols]
            self.off += cols
        assert self.off <= self.size, ("arena overflow", self.off, self.size)
        return a


def build_nc(debug_phase1=False):
    nc = bass.Bass("TRN2", target_bir_lowering=False)

    def din(name, shape, dt=F32):
        return nc.dram_tensor(name, list(shape), dt, kind="ExternalInput").ap()

    x = din("x", [S, D])
    w_in = din("w_in", [D, PW])
    w_a = din("w_a", [512, D])
    w_c = din("w_c", [512, D])
    w_out = din("w_out", [D, D])
    w_r = din("w_r", [128, 8 * 36])
    wg_l = din("wg_l", [NE * 128, 2048])
    wu_l = din("wu_l", [NE * 128, 2048])
    wd_l = din("wd_l", [NE * 128, 2048])
    g1b_d = din("g1b", [128, D])
    g2b_d = din("g2b", [128, D])
    qkg_d = din("qkg", [128, 640])
    cwl_d = din("cwl", [128, 12])
    cbl_d = din("cbl", [128, 4])
    sinkb_d = din("sinkb", [2, 8])
    brb_d = din("brb", [128, 36])
    ropeC_d = din("ropeC", [128, NT * 16])
    ropeS_d = din("ropeS", [128, NT * 16])
    cmat_d = din("cmat", [128, 512])
    cmoe_d = din("cmoe", [128, 472])
    zrows_d = din("zrows", [RB, D // 2])
    out = nc.dram_tensor("out", [S, D], F32, kind="ExternalOutput").ap()
    h2_d = nc.dram_tensor("h2_scr", [S, D], BF16).ap()
    xs_d = nc.dram_tensor("xs_scr", [NROW, D], BF16).ap()
    wgb_d = nc.dram_tensor("wgb_scr", [NE * 128, 2048], BF16).ap()
    wub_d = nc.dram_tensor("wub_scr", [NE * 128, 2048], BF16).ap()
    wdb_d = nc.dram_tensor("wdb_scr", [NE * 128, 2048], BF16).ap()
    ys_d = nc.dram_tensor("ys_scr", [NROW, D], F32).ap()

    P = Prog()
    ARENA = 106000
    with contextlib.ExitStack() as es:
        arena_t = es.enter_context(nc.sbuf_tensor("arena", [128, ARENA], BF16))
        psb = [es.enter_context(nc.psum_tensor("ps%d" % i, [128, 512], F32)) for i in range(8)]

        pstate = {"h": 0}

        def psum(cols):
            if cols <= 256:
                h = pstate["h"]
                pstate["h"] = (h + 1) % 16
                b, half = h // 2, h % 2
                return psb[b][:, half * 256:half * 256 + cols], ["psb%d" % b]
            h = pstate["h"]
            h += h & 1
            h %= 16
            pstate["h"] = (h + 2) % 16
            b = h // 2
            return psb[b][:, 0:cols], ["psb%d" % b]

        def psum_bf(cols):
            a, names = psum(512)
            return a.bitcast(BF16)[:, 0:cols], names

        A0 = Arena(arena_t, ARENA)
        ohA = A0.alloc(NT * NE).rearrange("p (i e) -> p i e", i=NT)
        ohB = A0.alloc(NT * NE).rearrange("p (i e) -> p i e", i=NT)
        gAB = A0.alloc(2 * NT, F32)
        persist_end = A0.off

        A = Arena(arena_t, ARENA)
        A.off = persist_end
        Win = A.alloc(8 * PW).rearrange("p (k n) -> p k n", k=8)
        Wa = A.alloc(4 * D).rearrange("p (k n) -> p k n", k=4)
        Wc = A.alloc(4 * D).rearrange("p (k n) -> p k n", k=4)
        Wout = A.alloc(8 * D).rearrange("p (k n) -> p k n", k=8)
        Wr = A.alloc(8 * 36).rearrange("p (k n) -> p k n", k=8)
        hT = [A.alloc(8 * T).rearrange("p (k n) -> p k n", k=8) for _ in range(2)]
        xring = [A.alloc(D, F32) for _ in range(2)]
        xres = [A.alloc(D, F32) for _ in range(2)]
        g1b = A.alloc(D, F32)
        g2b = A.alloc(D, F32)
        qkg = A.alloc(640, F32)
        ropeC = A.alloc(NT * 16, F32).rearrange("p (i c) -> p i c", i=NT)
        ropeS = A.alloc(NT * 16, F32).rearrange("p (i c) -> p i c", i=NT)
        hb = A.alloc(D)
        h2b = A.alloc(D)
        kT = A.alloc(2 * KVR * 128).rearrange("p (g n) -> p g n", g=2)
        vring = A.alloc(KVR * 2 * 128).rearrange("p (s g n) -> p s g n", s=KVR, g=2)
        uring = [A.alloc(4 * (T + 2)).rearrange("p (c n) -> p c n", c=4) for _ in range(UR)]
        qT = [A.alloc(8 * 128).rearrange("p (h n) -> p h n", h=8) for _ in range(2)]
        sq = A.alloc(512, F32)
        xn = A.alloc(512, F32)
        rt1 = A.alloc(128, F32)
        rt2 = A.alloc(128, F32)
        qkb = A.alloc(512)
        pT = [A.alloc(512) for _ in range(4)]
        aT = A.alloc(4 * T).rearrange("p (c n) -> p c n", c=4)
        rD = A.alloc(512, F32)
        cbT = A.alloc(4 * T).rearrange("p (c n) -> p c n", c=4)
        cct = [A.alloc(T, F32) for _ in range(2)]
        cv1 = A.alloc(T, F32)
        cv2 = A.alloc(T, F32)
        cT = A.alloc(4 * T).rearrange("p (c n) -> p c n", c=4)
        ta = [A.alloc(T) for _ in range(2)]
        tcg = [A.alloc(T) for _ in range(2)]
        m1 = A.alloc(T, F32)
        m2 = A.alloc(T, F32)
        mergedT = A.alloc(8 * T).rearrange("p (k n) -> p k n", k=8)
        h2T = [A.alloc(8 * 128).rearrange("p (k n) -> p k n", k=8) for _ in range(2)]
        identb = A.alloc(128)
        maskPb = A.alloc(128)
        maskNb = A.alloc(128)
        onesD2 = A.alloc(128)
        esrow = A.alloc(8 * 128).rearrange("p (h n) -> p h n", h=8)
        cmat = A.alloc(512, F32)
        cw = A.alloc(12, F32)
        cbias = A.alloc(4, F32)
        brb = A.alloc(36, F32)
        st = A.alloc(64, F32)
        rtmp = A.alloc(256, F32)
        sk = A.alloc(64, F32)
        skb = A.alloc(16)
        print("phase1 arena bytes", A.off * 2)

        mhalf = st[:, 0:16]
        ss1 = [st[:, 16 + r:17 + r] for r in range(2)]
        rs1 = [st[:, 18 + r:19 + r] for r in range(2)]
        ss2 = [st[:, 20 + r:21 + r] for r in range(2)]
        rs2 = [st[:, 22 + r:23 + r] for r in range(2)]
        ssq = st[:, 24:32]
        rsq = st[:, 32:40]
        vsq = st[:, 40:48]

        def dma(eng, out_ap, in_ap, key, reads=(), writes=(), lat=None):
            return P.op(eng, lambda e: e.dma_start(out=out_ap, in_=in_ap), reads=reads, writes=writes, dma=key, lat=lat)

        dma(SP, cmat, cmat_d, "c_cmat", writes=["cmat"])
        dma(SP, g1b, g1b_d, "c_g1b", writes=["g1b"])
        dma(SP, qkg, qkg_d, "c_qkg", writes=["qkg"])
        dma(SP, ropeC, ropeC_d.rearrange("p (i c) -> p i c", i=NT), "c_ropeC", writes=["ropeC"])
        dma(SP, ropeS, ropeS_d.rearrange("p (i c) -> p i c", i=NT), "c_ropeS", writes=["ropeS"])
        dma(SP, cw, cwl_d, "c_cw", writes=["cw"])
        dma(SP, cbias, cbl_d, "c_cb", writes=["cbias"])
        dma(SP, brb, brb_d, "c_brb", writes=["brb"])
        dma(SP, sk[0:2, 0:8], sinkb_d, "c_sink", writes=["sk"])
        dma(SP, g2b, g2b_d, "c_g2b", writes=["g2b"])
        for (c0, c1, nm_) in ((512, 768, "C"), (1280, 2304, "C"), (0, 512, "D"), (768, 1280, "D"), (2304, 4352, "D")):
            for k in range(8):
                dma(POOL, Win[:, k, c0:c1], w_in[k * 128:(k + 1) * 128, c0:c1], "w_in" + nm_, writes=["Win%s_%d_%d" % (nm_, k, c0)])
        WinC_n = ["WinC_%d_%d" % (k, c0) for k in range(8) for c0 in (512, 1280)]
        WinD_n = ["WinD_%d_%d" % (k, c0) for k in range(8) for c0 in (0, 768, 2304)]
        dma(POOL, Wa, w_a.rearrange("(c p) n -> p c n", p=128), "w_a", writes=["Wa"])
        dma(POOL, Wc, w_c.rearrange("(c p) n -> p c n", p=128), "w_c", writes=["Wc"])
        dma(POOL, Wout, w_out.rearrange("(k p) n -> p k n", p=128), "w_out", writes=["Wout"])
        dma(POOL, Wr, w_r.rearrange("p (k n) -> p k n", k=8), "w_r", writes=["Wr"])

        P.op(DVE, lambda e: e.memset(mhalf, -0.5), writes=["mhalf"])
        P.op(DVE, lambda e: e.tensor_copy(out=identb, in_=cmat[:, 0:128]), reads=["cmat"], writes=["identb"])
        P.op(DVE, lambda e: e.tensor_copy(out=maskPb, in_=cmat[:, 128:256]), reads=["cmat"], writes=["maskPb"])
        P.op(DVE, lambda e: e.tensor_copy(out=maskNb, in_=cmat[:, 256:384]), reads=["cmat"], writes=["maskNb"])
        P.op(DVE, lambda e: e.tensor_copy(out=onesD2[0:2, :], in_=cmat[0:2, 384:512]), reads=["cmat"], writes=["onesD2"])
        P.op(DVE, lambda e: e.tensor_scalar(out=g1b, in0=g1b, scalar1=32.0, scalar2=None, op0=ALU.mult), reads=["g1b"], writes=["g1b"])
        P.op(DVE, lambda e: e.tensor_scalar(out=g2b, in0=g2b, scalar1=32.0, scalar2=None, op0=ALU.mult), reads=["g2b"], writes=["g2b"])
        P.op(DVE, lambda e: e.tensor_scalar(out=qkg, in0=qkg, scalar1=8.0, scalar2=None, op0=ALU.mult), reads=["qkg"], writes=["qkg"])
        P.op(POOL, lambda e: e.memset(vring[:, :, :, 64:128], 1.0), writes=["vones"])
        for r in range(UR):
            P.op(POOL, lambda e, r=r: e.memset(uring[r], 0.0), writes=["u%d" % r, "uh0_%d" % r, "uh1_%d" % r])
        sel = cmat[0:2, 0:2]
        e_f = sk[0:2, 8:16]
        hi_f = sk[0:2, 16:24]
        lo_f = sk[0:2, 24:32]
        fin = sk[0:2, 32:40]
        P.op(ACT, lambda e: e.activation(out=e_f, in_=sk[0:2, 0:8], func=AF.Exp), reads=["sk"], writes=["sk_e"])
        P.op(DVE, lambda e: e.tensor_copy(out=skb[0:2, 0:8], in_=e_f), reads=["sk_e"], writes=["skb"])
        P.op(DVE, lambda e: e.tensor_copy(out=hi_f, in_=skb[0:2, 0:8]), reads=["skb"], writes=["sk_hi"])
        P.op(DVE, lambda e: e.tensor_tensor(out=lo_f, in0=e_f, in1=hi_f, op=ALU.subtract), reads=["sk_e", "sk_hi"], writes=["sk_lo"])
        P.op(DVE, lambda e: e.tensor_scalar(out=fin, in0=hi_f, scalar1=sel[:, 0:1], scalar2=None, op0=ALU.mult), reads=["sk_hi", "cmat"], writes=["sk_fin"])
        P.op(DVE, lambda e: e.scalar_tensor_tensor(out=fin, in0=lo_f, scalar=sel[:, 1:2], in1=fin, op0=ALU.mult, op1=ALU.add), reads=["sk_lo", "sk_fin", "cmat"], writes=["sk_fin"])
        P.op(DVE, lambda e: e.tensor_copy(out=esrow[0:2, :, :], in_=fin[:, :, None].broadcast_to([2, 8, 128])), reads=["sk_fin"], writes=["esrow"])

        import os
        KLEVEL = int(os.environ.get("KLEVEL", "3"))
        KNS = int(os.environ.get("KNS", str(NS)))
        def rms_rstd(ss, rs, n, tag):
            w = ss.shape[-1]
            P.op(POOL, lambda e: e.tensor_scalar(out=rs, in0=ss, scalar1=float(n * EPS), scalar2=None, op0=ALU.add),
                 reads=["ss" + tag], writes=["rs" + tag])
            P.op(POOL, lambda e: e.tensor_tensor(out=rs, in0=rs, in1=mhalf[:, 0:w], op=ALU.pow),
                 reads=["rs" + tag, "mhalf"], writes=["rs" + tag])

        def qk_post(ps_ap, ps_names, H, gain, i):
            W = H * 64
            P.op(ACT, lambda e: e.activation(out=sq[:, 0:W], in_=ps_ap, func=AF.Square), reads=ps_names, writes=["sq"])
            P.op(DVE, lambda e: e.tensor_reduce(out=ssq[:, 0:H], in_=sq[:, 0:W].rearrange("p (h d) -> p h d", h=H), axis=AX.X, op=ALU.add),
                 reads=["sq"], writes=["ssq"])
            P.op(POOL, lambda e: e.tensor_scalar(out=vsq[:, 0:H], in0=ssq[:, 0:H], scalar1=float(64 * EPS), scalar2=None, op0=ALU.add),
                 reads=["ssq"], writes=["vsq"])
            P.op(POOL, lambda e: e.tensor_tensor(out=rsq[:, 0:H], in0=vsq[:, 0:H], in1=mhalf[:, 0:H], op=ALU.pow),
                 reads=["vsq", "mhalf"], writes=["rsq"])
            xn3 = xn[:, 0:W].rearrange("p (h d) -> p h d", h=H)
            P.op(DVE, lambda e: e.tensor_tensor(out=xn3, in0=ps_ap.rearrange("p (h d) -> p h d", h=H),
                                                in1=rsq[:, 0:H, None].broadcast_to([128, H, 64]), op=ALU.mult),
                 reads=ps_names + ["rsq"], writes=["xn"])
            P.op(DVE, lambda e: e.tensor_tensor(out=xn[:, 0:W], in0=xn[:, 0:W], in1=gain, op=ALU.mult),
                 reads=["xn", "qkg"], writes=["xn"])
            qb3 = qkb[:, 0:W].rearrange("p (h d) -> p h d", h=H)
            P.op(ACT, lambda e: e.activation(out=qkb[:, 0:W], in_=xn[:, 0:W], func=AF.Copy), reads=["xn"], writes=["qkb"])
            t1 = rt1[:, 0:H * 16].rearrange("p (h c) -> p h c", h=H)
            t2 = rt2[:, 0:H * 16].rearrange("p (h c) -> p h c", h=H)
            rc = ropeC[:, i:i + 1, :]
            rs_ = ropeS[:, i:i + 1, :]
            P.op(POOL, lambda e: e.tensor_tensor(out=t1, in0=xn3[:, :, 0:16], in1=rc.broadcast_to([128, H, 16]), op=ALU.mult),
                 reads=["xn", "ropeC"], writes=["rt1"])
            P.op(POOL, lambda e: e.tensor_tensor(out=t2[:, :, 0:8], in0=xn3[:, :, 8:16], in1=rs_[:, :, 0:8].broadcast_to([128, H, 8]), op=ALU.mult),
                 reads=["xn", "ropeS"], writes=["rt2a"])
            P.op(POOL, lambda e: e.tensor_tensor(out=t2[:, :, 8:16], in0=xn3[:, :, 0:8], in1=rs_[:, :, 8:16].broadcast_to([128, H, 8]), op=ALU.mult),
                 reads=["xn", "ropeS"], writes=["rt2b"])
            P.op(POOL, lambda e: e.tensor_tensor(out=qb3[:, :, 0:16], in0=t1, in1=t2, op=ALU.add),
                 reads=["rt1", "rt2a", "rt2b", "qkb"], writes=["qkb"])

        zsrc = zrows_d.bitcast(BF16)

        def stage_B(s):
            if (not debug_phase1) and s >= 2:
                for b_ in range((s - 2) * 5, min(NB, (s - 1) * 5)):
                    dma(SP, xs_d[b_ * RB:(b_ + 1) * RB, :], zsrc, "zf%d" % (b_ % 4), reads=["tick%d" % (s - 1)], writes=["xs_z%d" % b_])
            if (not debug_phase1) and s >= 1:
                for ex_ in (range(2 * (s - 1), 2 * (s - 1) + 2) if s < NS - 1 else range(2 * (s - 1), NE)):
                    for (src_t, dst_t, nm) in ((wg_l, wgb_d, "g"), (wu_l, wub_d, "u"), (wd_l, wdb_d, "d")):
                        dma(POOL, dst_t[ex_ * 128:(ex_ + 1) * 128, :], src_t[ex_ * 128:(ex_ + 1) * 128, :], "pc%s%d" % (nm, ex_ % 2),
                            reads=["tick%d" % (s - 1)], writes=["wb_%s%d" % (nm, ex_)])
            for j in range(SUB):
                i = s * SUB + j
                r = i % 2
                xs = xring[r]
                dma(SP, xs, x[i * 128:(i + 1) * 128, :], "x%d" % r, writes=["x%d" % r])
                P.op(ACT, lambda e, xs=xs, r=r: e.activation(out=hb, in_=xs, func=AF.Square, accum_out=ss1[r]),
                     reads=["x%d" % r], writes=["hb", "ss1_%d" % r], cost=1200)
                rms_rstd(ss1[r], rs1[r], D, "1_%d" % r)
                P.op(DVE, lambda e, xs=xs, r=r: e.scalar_tensor_tensor(out=hb, in0=xs, scalar=rs1[r], in1=g1b, op0=ALU.mult, op1=ALU.mult),
                     reads=["x%d" % r, "rs1_%d" % r, "g1b"], writes=["hb"], cost=1200)
                pt, pn = psum_bf(1024)
                for k in range(8):
                    P.op(PE, lambda e, k=k, pt=pt: e.transpose(out=pt[:, k * 128:(k + 1) * 128], in_=hb[:, k * 128:(k + 1) * 128], identity=identb),
                         reads=["hb", "identb"], writes=pn, cost=100)
                dst = hT[s % 2][:, :, j * 128:(j + 1) * 128]
                P.op(ACT, lambda e, pt=pt, dst=dst: e.activation(out=dst, in_=pt.rearrange("p (k n) -> p k n", k=8), func=AF.Copy),
                     reads=pn, writes=["hT%d_%d" % (s % 2, j)] + (["tick%d" % s] if j == SUB - 1 else []), cost=1200)

        def stage_C(s):
            hTs = hT[s % 2]
            for j in range(SUB):
                i = s * SUB + j
                slot = i % KVR
                pk, pkn = psum(256)
                for k in range(8):
                    P.op(PE, lambda e, k=k, pk=pk, j=j: e.matmul(pk, lhsT=hTs[:, k, j * 128:(j + 1) * 128], rhs=Win[:, k, K0:K0 + 256], start=(k == 0), stop=(k == 7)),
                         reads=["hT%d_%d" % (s % 2, j)] + WinC_n, writes=pkn, cost=160)
                P.op(ACT, lambda e, pk=pk, slot=slot: e.activation(out=vring[:, slot, :, 0:64], in_=pk[:, 128:256].rearrange("p (g d) -> p g d", g=2), func=AF.Copy),
                     reads=pkn, writes=["v%d" % slot])
                qk_post(pk[:, 0:128], pkn, 2, qkg[:, 512:640], i)
                pt, pn = psum_bf(256)
                for g in range(2):
                    P.op(PE, lambda e, g=g, pt=pt: e.transpose(out=pt[0:64, g * 128:(g + 1) * 128], in_=qkb[:, g * 64:(g + 1) * 64], identity=identb),
                         reads=["qkb", "identb"], writes=pn, cost=100)
                P.op(ACT, lambda e, pt=pt, slot=slot: e.activation(out=kT[0:64, :, slot * 128:(slot + 1) * 128], in_=pt[0:64, 0:256].rearrange("p (g n) -> p g n", g=2), func=AF.Copy),
                     reads=pn, writes=["k%d" % slot])
            us = uring[s % UR]
            for c in range(4):
                pc, pcn = psum(T)
                for k in range(8):
                    P.op(PE, lambda e, k=k, pc=pc, c=c: e.matmul(pc, lhsT=Win[:, k, CC0 + c * 128:CC0 + (c + 1) * 128], rhs=hTs[:, k, :], start=(k == 0), stop=(k == 7)),
                         reads=["hT%d_%d" % (s % 2, j) for j in range(SUB)] + WinC_n, writes=pcn, cost=160)
                px, pxn = psum(T)
                for k in range(8):
                    P.op(PE, lambda e, k=k, px=px, c=c: e.matmul(px, lhsT=Win[:, k, CX0 + c * 128:CX0 + (c + 1) * 128], rhs=hTs[:, k, :], start=(k == 0), stop=(k == 7)),
                         reads=["hT%d_%d" % (s % 2, j) for j in range(SUB)] + WinC_n, writes=pxn, cost=160)
                cc_ = cct[c % 2]
                P.op(ACT, lambda e, pc=pc, cc_=cc_: e.activation(out=cc_, in_=pc, func=AF.Copy), reads=pcn, writes=["cct%d" % (c % 2)])
                P.op(DVE, lambda e, px=px, cc_=cc_, c=c: e.tensor_tensor(out=us[:, c, 1:T + 1], in0=px, in1=cc_, op=ALU.mult),
                     reads=pxn + ["cct%d" % (c % 2)], writes=["u%d" % (s % UR)])
            if s > 0:
                up = uring[(s - 1) % UR]
                P.op(POOL, lambda e: e.tensor_copy(out=us[:, :, 0:1], in_=up[:, :, T:T + 1]),
                     reads=["u%d" % ((s - 1) % UR)], writes=["uh0_%d" % (s % UR)])
                P.op(POOL, lambda e: e.tensor_copy(out=up[:, :, T + 1:T + 2], in_=us[:, :, 1:2]),
                     reads=["u%d" % (s % UR)], writes=["uh1_%d" % ((s - 1) % UR)])
            else:
                P.op(POOL, lambda e: e.memset(us[:, :, 0:1], 0.0), writes=["uh0_%d" % (s % UR)])
            if s == KNS - 1:
                P.op(POOL, lambda e: e.memset(us[:, :, T + 1:T + 2], 0.0), writes=["uh1_%d" % (s % UR)])

        def router(i, h2Ti):
            pr, prn = psum(36)
            for k in range(8):
                P.op(PE, lambda e, k=k: e.matmul(pr, lhsT=h2Ti[:, k, :], rhs=Wr[:, k, :], start=(k == 0), stop=(k == 7)),
                     reads=["h2T%d" % (i % 2), "Wr"], writes=prn, cost=70)
            lg = rtmp[:, 0:36]
            gmax = rtmp[:, 36:37]
            goh = rtmp[:, 40:44]
            gd = rtmp[:, 44:48]
            gex = rtmp[:, 48:52]
            gsum = rtmp[:, 52:53]
            pg = rtmp[:, 53:54]
            tmp48 = rtmp[:, 64:96].rearrange("p (g j) -> p g j", g=4)
            ein = rtmp[:, 96:104]
            mx1 = rtmp[:, 104:105]
            oh1 = rtmp[:, 112:120]
            e2 = rtmp[:, 120:128]
            mx2 = rtmp[:, 105:106]
            oh2 = rtmp[:, 128:136]
            d12 = rtmp[:, 106:107]
            t12 = rtmp[:, 107:108]
            pa = rtmp[:, 108:109]
            gq1 = rtmp[:, 109:110]
            gq2 = rtmp[:, 110:111]
            nt12 = rtmp[:, 111:112]
            gw = rtmp[:, 136:144]
            gw2 = rtmp[:, 144:152]
            R = "rt_"
            P.op(DVE, lambda e: e.tensor_tensor(out=lg, in0=pr, in1=brb, op=ALU.add), reads=prn + ["brb"], writes=[R + "lg"])
            P.op(DVE, lambda e: e.tensor_reduce(out=gmax, in_=lg[:, 0:4], axis=AX.X, op=ALU.max), reads=[R + "lg"], writes=[R + "gmax"])
            P.op(DVE, lambda e: e.tensor_scalar(out=goh, in0=lg[:, 0:4], scalar1=gmax, scalar2=None, op0=ALU.is_equal), reads=[R + "lg", R + "gmax"], writes=[R + "goh"])
            P.op(DVE, lambda e: e.tensor_scalar(out=gd, in0=lg[:, 0:4], scalar1=gmax, scalar2=None, op0=ALU.subtract), reads=[R + "lg", R + "gmax"], writes=[R + "gd"])
            P.op(ACT, lambda e: e.activation(out=gex, in_=gd, func=AF.Exp, accum_out=gsum), reads=[R + "gd"], writes=[R + "gex", R + "gsum"])
            P.op(DVE, lambda e: e.reciprocal(out=pg, in_=gsum), reads=[R + "gsum"], writes=[R + "pg"])
            P.op(DVE, lambda e: e.tensor_tensor(out=tmp48, in0=lg[:, 4:36].rearrange("p (g j) -> p g j", g=4),
                                                in1=goh[:, :, None].broadcast_to([128, 4, 8]), op=ALU.mult),
                 reads=[R + "lg", R + "goh"], writes=[R + "tmp48"])
            P.op(DVE, lambda e: e.tensor_reduce(out=ein, in_=tmp48.rearrange("p g j -> p j g"), axis=AX.X, op=ALU.add), reads=[R + "tmp48"], writes=[R + "ein"])
            P.op(DVE, lambda e: e.tensor_reduce(out=mx1, in_=ein, axis=AX.X, op=ALU.max), reads=[R + "ein"], writes=[R + "mx1"])
            P.op(DVE, lambda e: e.tensor_scalar(out=oh1, in0=ein, scalar1=mx1, scalar2=None, op0=ALU.is_equal), reads=[R + "ein", R + "mx1"], writes=[R + "oh1"])
            P.op(DVE, lambda e: e.scalar_tensor_tensor(out=e2, in0=oh1, scalar=-1e30, in1=ein, op0=ALU.mult, op1=ALU.add), reads=[R + "oh1", R + "ein"], writes=[R + "e2"])
            P.op(DVE, lambda e: e.tensor_reduce(out=mx2, in_=e2, axis=AX.X, op=ALU.max), reads=[R + "e2"], writes=[R + "mx2"])
            P.op(DVE, lambda e: e.tensor_scalar(out=oh2, in0=e2, scalar1=mx2, scalar2=None, op0=ALU.is_equal), reads=[R + "e2", R + "mx2"], writes=[R + "oh2"])
            P.op(DVE, lambda e: e.tensor_tensor(out=d12, in0=mx1, in1=mx2, op=ALU.subtract), reads=[R + "mx1", R + "mx2"], writes=[R + "d12"])
            P.op(ACT, lambda e: e.activation(out=t12, in_=d12, func=AF.Tanh, scale=0.5), reads=[R + "d12"], writes=[R + "t12"])
            P.op(DVE, lambda e: e.tensor_scalar(out=pa, in0=pg, scalar1=0.25, scalar2=None, op0=ALU.mult), reads=[R + "pg"], writes=[R + "pa"])
            P.op(DVE, lambda e: e.scalar_tensor_tensor(out=gq1, in0=t12, scalar=1.0, in1=pa, op0=ALU.add, op1=ALU.mult), reads=[R + "t12", R + "pa"], writes=[R + "gq1"])
            P.op(DVE, lambda e: e.tensor_scalar(out=nt12, in0=t12, scalar1=-1.0, scalar2=1.0, op0=ALU.mult, op1=ALU.add), reads=[R + "t12"], writes=[R + "nt12"])
            P.op(DVE, lambda e: e.tensor_tensor(out=gq2, in0=nt12, in1=pa, op=ALU.mult), reads=[R + "nt12", R + "pa"], writes=[R + "gq2"])
            P.op(DVE, lambda e: e.tensor_tensor(out=ohA[:, i, :].rearrange("p (g j) -> p g j", g=4),
                                                in0=goh[:, :, None].broadcast_to([128, 4, 8]),
                                                in1=oh1[:, None, :].broadcast_to([128, 4, 8]), op=ALU.mult),
                 reads=[R + "goh", R + "oh1"], writes=["ohA"])
            P.op(DVE, lambda e: e.tensor_tensor(out=ohB[:, i, :].rearrange("p (g j) -> p g j", g=4),
                                                in0=goh[:, :, None].broadcast_to([128, 4, 8]),
                                                in1=oh2[:, None, :].broadcast_to([128, 4, 8]), op=ALU.mult),
                 reads=[R + "goh", R + "oh2"], writes=["ohB"])
            P.op(DVE, lambda e: e.tensor_copy(out=gAB[:, i:i + 1], in_=gq1), reads=[R + "gq1"], writes=["gAB"])
            P.op(DVE, lambda e: e.tensor_copy(out=gAB[:, NT + i:NT + i + 1], in_=gq2), reads=[R + "gq2"], writes=["gAB"])

        def stage_D(t):
            hTt = hT[t % 2]
            hT_names = ["hT%d_%d" % (t % 2, j) for j in range(SUB)]
            for j in range(SUB):
                i = t * SUB + j
                dma(SP, xres[i % 2], x[i * 128:(i + 1) * 128, :], "xr%d" % (i % 2), writes=["xr%d" % (i % 2)])
            for j in range(SUB):
                i = t * SUB + j
                pq, pqn = psum(512)
                for k in range(8):
                    P.op(PE, lambda e, k=k, pq=pq, j=j: e.matmul(pq, lhsT=hTt[:, k, j * 128:(j + 1) * 128], rhs=Win[:, k, Q0:Q0 + 512], start=(k == 0), stop=(k == 7)),
                         reads=["hT%d_%d" % (t % 2, j)] + WinD_n, writes=pqn, cost=260)
                qk_post(pq, pqn, 8, qkg[:, 0:512], i)
                pt, pn = psum_bf(1024)
                for h in range(8):
                    P.op(PE, lambda e, h=h, pt=pt: e.transpose(out=pt[0:64, h * 128:(h + 1) * 128], in_=qkb[:, h * 64:(h + 1) * 64], identity=identb),
                         reads=["qkb", "identb"], writes=pn, cost=100)
                qTi = qT[i % 2]
                P.op(ACT, lambda e, pt=pt, qTi=qTi: e.activation(out=qTi[0:64, :, :], in_=pt[0:64, :].rearrange("p (h n) -> p h n", h=8), func=AF.Copy),
                     reads=pn, writes=["qT%d" % (i % 2)])
                for g in range(2):
                    blocks = [b for b in (i - 1, i, i + 1) if 0 <= b < KNS * SUB]
                    pod, podn = psum(512)
                    pts = []
                    for bi, b in enumerate(blocks):
                        slot = b % KVR
                        psS, psn = psum(512)
                        P.op(PE, lambda e, psS=psS, slot=slot, g=g, qTi=qTi: e.matmul(psS, lhsT=kT[0:64, g, slot * 128:(slot + 1) * 128],
                                                                                     rhs=qTi[0:64, 4 * g:4 * g + 4, :], start=True, stop=True),
                             reads=["k%d" % slot, "qT%d" % (i % 2)], writes=psn, cost=260)
                        pidx = pstate.setdefault("pT", 0)
                        pstate["pT"] = (pidx + 1) % 4
                        pTi = pT[pidx]
                        P.op(ACT, lambda e, psS=psS, pTi=pTi: e.activation(out=pTi, in_=psS, func=AF.Exp, scale=0.125),
                             reads=psn, writes=["pT%d" % pidx], cost=600)
                        if b != i:
                            mk_ = maskPb if b == i - 1 else maskNb
                            P.op(POOL, lambda e, pTi=pTi, mk_=mk_: e.tensor_tensor(out=pTi.rearrange("p (h q) -> p h q", h=4),
                                                                                 in0=pTi.rearrange("p (h q) -> p h q", h=4),
                                                                                 in1=mk_[:, None, :].broadcast_to([128, 4, 128]), op=ALU.mult),
                                 reads=["pT%d" % pidx, "maskPb", "maskNb"], writes=["pT%d" % pidx])
                        P.op(PE, lambda e, pod=pod, slot=slot, g=g, pTi=pTi, bi=bi: e.matmul(pod, lhsT=vring[:, slot, g, :], rhs=pTi, start=(bi == 0), stop=False),
                             reads=["v%d" % slot, "vones", "pT%d" % pidx], writes=podn, cost=260)
                    P.op(PE, lambda e, pod=pod, g=g: e.matmul(pod, lhsT=onesD2[0:2, :], rhs=esrow[0:2, 4 * g:4 * g + 4, :], start=False, stop=True),
                         reads=["onesD2", "esrow"], writes=podn, cost=260)
                    P.op(DVE, lambda e, pod=pod: e.reciprocal(out=rD[0:64, :], in_=pod[64:128, :]), reads=podn, writes=["rD"], cost=3400)
                    o4 = pod[0:64, :].rearrange("p (a b q) -> p a b q", a=2, b=2)
                    r4 = rD[0:64, :].rearrange("p (a b q) -> p a b q", a=2, b=2)
                    P.op(DVE, lambda e, o4=o4, r4=r4, g=g, j=j: e.tensor_tensor(out=aT[0:64, 2 * g:2 * g + 2, j * 128:(j + 1) * 128], in0=o4[:, :, 0, :], in1=r4[:, :, 0, :], op=ALU.mult),
                         reads=podn + ["rD"], writes=["aT"])
                    P.op(DVE, lambda e, o4=o4, r4=r4, g=g, j=j: e.tensor_tensor(out=aT[64:128, 2 * g:2 * g + 2, j * 128:(j + 1) * 128], in0=o4[:, :, 1, :], in1=r4[:, :, 1, :], op=ALU.mult),
                         reads=podn + ["rD"], writes=["aT"])
            ut = uring[t % UR]
            un = ["u%d" % (t % UR), "uh0_%d" % (t % UR), "uh1_%d" % (t % UR)]
            for c in range(4):
                pc, pcn = psum(T)
                for k in range(8):
                    P.op(PE, lambda e, k=k, pc=pc, c=c: e.matmul(pc, lhsT=Win[:, k, CB0 + c * 128:CB0 + (c + 1) * 128], rhs=hTt[:, k, :], start=(k == 0), stop=(k == 7)),
                         reads=hT_names + WinD_n, writes=pcn, cost=160)
                P.op(ACT, lambda e, pc=pc, c=c: e.activation(out=cbT[:, c, :], in_=pc, func=AF.Copy), reads=pcn, writes=["cbT%d" % c])
                P.op(DVE, lambda e, c=c: e.tensor_scalar(out=cv1, in0=ut[:, c, 0:T], scalar1=cw[:, 3 * c:3 * c + 1], scalar2=None, op0=ALU.mult),
                     reads=un + ["cw"], writes=["cv1"])
                P.op(DVE, lambda e, c=c: e.scalar_tensor_tensor(out=cv2, in0=ut[:, c, 1:T + 1], scalar=cw[:, 3 * c + 1:3 * c + 2], in1=cv1, op0=ALU.mult, op1=ALU.add),
                     reads=un + ["cw", "cv1"], writes=["cv2"])
                P.op(DVE, lambda e, c=c: e.scalar_tensor_tensor(out=cv1, in0=ut[:, c, 2:T + 2], scalar=cw[:, 3 * c + 2:3 * c + 3], in1=cv2, op0=ALU.mult, op1=ALU.add),
                     reads=un + ["cw", "cv2"], writes=["cv1"])
                P.op(DVE, lambda e, c=c: e.scalar_tensor_tensor(out=cT[:, c, :], in0=cv1, scalar=cbias[:, c:c + 1], in1=cbT[:, c, :], op0=ALU.add, op1=ALU.mult),
                     reads=["cv1", "cbias", "cbT%d" % c], writes=["cT"])
            for m in range(8):
                pga, pgan = psum(T)
                for k in range(8):
                    P.op(PE, lambda e, k=k, pga=pga, m=m: e.matmul(pga, lhsT=Win[:, k, GA0 + m * 128:GA0 + (m + 1) * 128], rhs=hTt[:, k, :], start=(k == 0), stop=(k == 7)),
                         reads=hT_names + WinD_n, writes=pgan, cost=160)
                pgc, pgcn = psum(T)
                for k in range(8):
                    P.op(PE, lambda e, k=k, pgc=pgc, m=m: e.matmul(pgc, lhsT=Win[:, k, GC0 + m * 128:GC0 + (m + 1) * 128], rhs=hTt[:, k, :], start=(k == 0), stop=(k == 7)),
                         reads=hT_names + WinD_n, writes=pgcn, cost=160)
                pA, pAn = psum(T)
                for c in range(4):
                    P.op(PE, lambda e, c=c, pA=pA, m=m: e.matmul(pA, lhsT=Wa[:, c, m * 128:(m + 1) * 128], rhs=aT[:, c, :], start=(c == 0), stop=(c == 3)),
                         reads=["aT", "Wa"], writes=pAn, cost=160)
                pC, pCn = psum(T)
                for c in range(4):
                    P.op(PE, lambda e, c=c, pC=pC, m=m: e.matmul(pC, lhsT=Wc[:, c, m * 128:(m + 1) * 128], rhs=cT[:, c, :], start=(c == 0), stop=(c == 3)),
                         reads=["cT", "Wc"], writes=pCn, cost=160)
                ta_ = ta[m % 2]
                tc_ = tcg[m % 2]
                P.op(ACT, lambda e, pga=pga, ta_=ta_: e.activation(out=ta_, in_=pga, func=AF.Tanh, scale=0.5), reads=pgan, writes=["ta%d" % (m % 2)])
                P.op(ACT, lambda e, pgc=pgc, tc_=tc_: e.activation(out=tc_, in_=pgc, func=AF.Tanh, scale=0.5), reads=pgcn, writes=["tc%d" % (m % 2)])
                P.op(DVE, lambda e, pA=pA, ta_=ta_: e.scalar_tensor_tensor(out=m1, in0=ta_, scalar=1.0, in1=pA, op0=ALU.add, op1=ALU.mult),
                     reads=pAn + ["ta%d" % (m % 2)], writes=["m1"])
                P.op(DVE, lambda e, pC=pC, tc_=tc_: e.scalar_tensor_tensor(out=m2, in0=tc_, scalar=1.0, in1=pC, op0=ALU.add, op1=ALU.mult),
                     reads=pCn + ["tc%d" % (m % 2)], writes=["m2"])
                P.op(POOL, lambda e, m=m: e.tensor_tensor(out=mergedT[:, m, :], in0=m1, in1=m2, op=ALU.add),
                     reads=["m1", "m2"], writes=["mergedT"])
            for j in range(SUB):
                i = t * SUB + j
                r = i % 2
                xr = xres[r]
                for half in range(2):
                    po, pon = psum(512)
                    for k in range(8):
                        P.op(PE, lambda e, k=k, po=po, j=j, half=half: e.matmul(po, lhsT=mergedT[:, k, j * 128:(j + 1) * 128], rhs=Wout[:, k, half * 512:(half + 1) * 512], start=(k == 0), stop=(k == 7)),
                             reads=["mergedT", "Wout"], writes=pon, cost=260)
                    P.op(DVE, lambda e, po=po, xr=xr, half=half: e.scalar_tensor_tensor(out=xr[:, half * 512:(half + 1) * 512], in0=po, scalar=0.5, in1=xr[:, half * 512:(half + 1) * 512], op0=ALU.mult, op1=ALU.add),
                         reads=pon + ["xr%d" % r], writes=["xr%d" % r])
                dma(SP, out[i * 128:(i + 1) * 128, :], xr, "st_x2_%d" % r, reads=["xr%d" % r])
                if debug_phase1:
                    continue
                P.op(ACT, lambda e, xr=xr, r=r: e.activation(out=h2b, in_=xr, func=AF.Square, accum_out=ss2[r]),
                     reads=["xr%d" % r], writes=["h2b", "ss2_%d" % r], cost=1200)
                rms_rstd(ss2[r], rs2[r], D, "2_%d" % r)
                P.op(DVE, lambda e, xr=xr, r=r: e.scalar_tensor_tensor(out=h2b, in0=xr, scalar=rs2[r], in1=g2b, op0=ALU.mult, op1=ALU.mult),
                     reads=["xr%d" % r, "rs2_%d" % r, "g2b"], writes=["h2b"], cost=1200)
                pt, pn = psum_bf(1024)
                for k in range(8):
                    P.op(PE, lambda e, k=k, pt=pt: e.transpose(out=pt[:, k * 128:(k + 1) * 128], in_=h2b[:, k * 128:(k + 1) * 128], identity=identb),
                         reads=["h2b", "identb"], writes=pn, cost=100)
                h2Ti = h2T[r]
                P.op(ACT, lambda e, pt=pt, h2Ti=h2Ti: e.activation(out=h2Ti, in_=pt.rearrange("p (k n) -> p k n", k=8), func=AF.Copy),
                     reads=pn, writes=["h2T%d" % r], cost=1200)
                dma(SP, h2_d[i * 128:(i + 1) * 128, :], h2b, "st_h2", reads=["h2b"])
                router(i, h2Ti)

        for s in range(KNS + 1):
            if s < KNS:
                if KLEVEL >= 1:
                    stage_B(s)
                if KLEVEL >= 2:
                    stage_C(s)
            if s >= 1 and KLEVEL >= 3:
                stage_D(s - 1)

        if not debug_phase1:
            I32 = mybir.dt.int32
            M = Arena(arena_t, ARENA)
            M.off = persist_end
            h2flat = M.alloc(NT * D)
            h2sb = h2flat.rearrange("p (i d) -> p i d", i=NT)
            CR = 4
            yA = [h2flat[:, (3 * r_) * 2048:(3 * r_ + 1) * 2048].bitcast(F32) for r_ in range(CR)]
            yB = [h2flat[:, (3 * r_ + 1) * 2048:(3 * r_ + 2) * 2048].bitcast(F32) for r_ in range(CR)]
            x2t = [h2flat[:, (3 * r_ + 2) * 2048:(3 * r_ + 3) * 2048].bitcast(F32) for r_ in range(CR)]
            Msel = M.alloc(NT * NE).rearrange("p (i e) -> p i e", i=NT)
            Mcum = M.alloc((NT + 1) * NE).rearrange("p (i e) -> p i e", i=NT + 1)
            rank = M.alloc(NT * NE, F32).rearrange("p (i e) -> p i e", i=NT)
            pos = M.alloc(NT * NE, F32).rearrange("p (i e) -> p i e", i=NT)
            tmpA = M.alloc(NT * NE, F32).rearrange("p (i e) -> p i e", i=NT)
            cmoe = M.alloc(472, F32)
            identm = M.alloc(128)
            Lst = M.alloc(128)
            Ones = M.alloc(128)
            cnt = M.alloc(NE, F32)
            cmpT = M.alloc(NE * 16, F32).rearrange("p (e k) -> p e k", e=NE)
            nblk = M.alloc(NE, F32)
            pc = M.alloc(NE, F32)
            sc0 = M.alloc(NE, F32)
            sc1 = M.alloc(NE, F32)
            pst = M.alloc(NE, F32)
            cmpB = M.alloc(NB * NE, F32).rearrange("p (b e) -> p b e", b=NB)
            be_f = M.alloc(NB, F32)
            be_i = M.alloc(NB, F32).bitcast(I32)
            iw_f = M.alloc(NB, F32)
            dA_f = M.alloc(NT, F32)
            dB_f = M.alloc(NT, F32)
            dA_i = M.alloc(NT, F32).bitcast(I32)
            dB_i = M.alloc(NT, F32).bitcast(I32)
            NW = 3
            Wg = [M.alloc(8 * 256).rearrange("p (k n) -> p k n", k=8) for _ in range(NW)]
            Wu = [M.alloc(8 * 256).rearrange("p (k n) -> p k n", k=8) for _ in range(NW)]
            Wd = [M.alloc(2 * D).rearrange("p (k n) -> p k n", k=2) for _ in range(NW)]
            xb = [M.alloc(SB * D).rearrange("p (s d) -> p s d", s=SB) for _ in range(2)]
            XT = [M.alloc(8 * RB).rearrange("p (k n) -> p k n", k=8) for _ in range(2)]
            tg = [M.alloc(RB) for _ in range(2)]
            sg = [M.alloc(RB, F32) for _ in range(2)]
            hid = [M.alloc(2 * RB).rearrange("p (f n) -> p f n", f=2) for _ in range(2)]
            yb = [M.alloc(SB * D, F32).rearrange("p (s d) -> p s d", s=SB) for _ in range(2)]
            bar = M.alloc(16, F32)
            print("phase2 arena bytes", M.off * 2)
            PH = ["PH2"]
            P.barrier(DVE, lambda e: e.memset(bar, 0.0), extra=PH)

            dma(SP, cmoe, cmoe_d, "c_cmoe", reads=PH, writes=["cmoe"])
            for q4 in range(4):
                dma(SP, h2sb[:, q4 * 8:(q4 + 1) * 8, :], h2_d[q4 * 1024:(q4 + 1) * 1024, :].rearrange("(i p) d -> p i d", p=128),
                    "h2sb%d" % q4, reads=PH, writes=["h2sb%d" % q4], lat=12000.0)
            THk = cmoe[:, 0:16]
            BR_ = cmoe[:, 16:16 + NB]
            P.op(DVE, lambda e: e.tensor_copy(out=Lst, in_=cmoe[:, 80:208]), reads=PH + ["cmoe"], writes=["Lst"])
            P.op(DVE, lambda e: e.tensor_copy(out=Ones, in_=cmoe[:, 208:336]), reads=PH + ["cmoe"], writes=["Ones"])
            P.op(DVE, lambda e: e.tensor_copy(out=identm, in_=cmoe[:, 336:464]), reads=PH + ["cmoe"], writes=["identm"])
            P.op(DVE, lambda e: e.tensor_tensor(out=Msel, in0=ohA, in1=ohB, op=ALU.add), reads=PH + ["ohA", "ohB"], writes=["Msel"], cost=1100)
            P.op(DVE, lambda e: e.memset(Mcum[:, 0, :], 0.0), reads=PH, writes=["Mcum0"])
            for i in range(1, NT + 1):
                P.op(DVE, lambda e, i=i: e.tensor_tensor(out=Mcum[:, i, :], in0=Mcum[:, i - 1, :], in1=Msel[:, i - 1, :], op=ALU.add),
                     reads=PH + ["Mcum%d" % (i - 1), "Msel"], writes=["Mcum%d" % i], cost=150)
            for half in range(2):
                pr_, prn_ = psum(512)
                for ii in range(16):
                    i = half * 16 + ii
                    P.op(PE, lambda e, pr_=pr_, ii=ii, i=i: e.matmul(pr_[:, ii * 32:(ii + 1) * 32], lhsT=Ones, rhs=Mcum[:, i, :], start=True, stop=False),
                         reads=PH + ["Ones", "Mcum%d" % i], writes=prn_, cost=70)
                    P.op(PE, lambda e, pr_=pr_, ii=ii, i=i: e.matmul(pr_[:, ii * 32:(ii + 1) * 32], lhsT=Lst, rhs=Msel[:, i, :], start=False, stop=True),
                         reads=PH + ["Lst", "Msel"], writes=prn_, cost=70)
                P.op(ACT, lambda e, pr_=pr_, half=half: e.activation(out=rank[:, half * 16:(half + 1) * 16, :], in_=pr_.rearrange("p (i e) -> p i e", i=16), func=AF.Copy),
                     reads=PH + prn_, writes=["rank%d" % half])
            pcn_, pcnn = psum(32)
            P.op(PE, lambda e: e.matmul(pcn_, lhsT=Ones, rhs=Mcum[:, NT, :], start=True, stop=True), reads=PH + ["Ones", "Mcum%d" % NT], writes=pcnn, cost=70)
            P.op(ACT, lambda e: e.activation(out=cnt, in_=pcn_, func=AF.Copy), reads=PH + pcnn, writes=["cnt"])
            P.op(DVE, lambda e: e.tensor_tensor(out=cmpT, in0=cnt[:, :, None].broadcast_to([128, NE, 16]), in1=THk[:, None, :].broadcast_to([128, NE, 16]), op=ALU.is_gt),
                 reads=PH + ["cnt", "cmoe"], writes=["cmpT"])
            P.op(DVE, lambda e: e.tensor_reduce(out=nblk, in_=cmpT, axis=AX.X, op=ALU.add), reads=PH + ["cmpT"], writes=["nblk"])
            P.op(DVE, lambda e: e.tensor_scalar(out=pc, in0=nblk, scalar1=float(RB), scalar2=None, op0=ALU.mult), reads=PH + ["nblk"], writes=["pc"])
            bufs = [(sc0, "sc0"), (sc1, "sc1")]
            src, srcn = pc, "pc"
            bi = 0
            for dd in (1, 2, 4, 8, 16):
                dst, dstn = bufs[bi]
                P.op(DVE, lambda e, src=src, dst=dst, dd=dd: e.tensor_tensor(out=dst[:, dd:NE], in0=src[:, dd:NE], in1=src[:, 0:NE - dd], op=ALU.add),
                     reads=PH + [srcn, srcn + "h"], writes=[dstn])
                P.op(DVE, lambda e, src=src, dst=dst, dd=dd: e.tensor_copy(out=dst[:, 0:dd], in_=src[:, 0:dd]),
                     reads=PH + [srcn, srcn + "h"], writes=[dstn + "h"])
                src, srcn = dst, dstn
                bi ^= 1
            pendn = [srcn, srcn + "h"]
            pend = src
            P.op(DVE, lambda e: e.tensor_tensor(out=pst, in0=pend, in1=pc, op=ALU.subtract), reads=PH + pendn + ["pc"], writes=["pst"])
            P.op(DVE, lambda e: e.tensor_tensor(out=pos, in0=rank, in1=pst[:, None, :].broadcast_to([128, NT, NE]), op=ALU.add),
                 reads=PH + ["rank0", "rank1", "pst"], writes=["pos"], cost=1200)
            for (oh_, df_, di_, nm) in ((ohA, dA_f, dA_i, "A"), (ohB, dB_f, dB_i, "B")):
                P.op(DVE, lambda e, oh_=oh_: e.tensor_tensor(out=tmpA, in0=oh_, in1=pos, op=ALU.mult), reads=PH + ["ohA", "ohB", "pos"], writes=["tmpA"], cost=1200)
                P.op(DVE, lambda e, df_=df_: e.tensor_reduce(out=df_, in_=tmpA, axis=AX.X, op=ALU.add), reads=PH + ["tmpA"], writes=["d%s_f" % nm], cost=1200)
                P.op(DVE, lambda e, df_=df_, di_=di_: e.tensor_copy(out=di_, in_=df_), reads=PH + ["d%s_f" % nm], writes=["d%s_i" % nm])
            P.op(DVE, lambda e: e.tensor_tensor(out=cmpB, in0=pend[:, None, :].broadcast_to([128, NB, NE]), in1=BR_[:, :, None].broadcast_to([128, NB, NE]), op=ALU.is_le),
                 reads=PH + pendn + ["cmoe"], writes=["cmpB"], cost=2200)
            P.op(DVE, lambda e: e.tensor_reduce(out=be_f, in_=cmpB, axis=AX.X, op=ALU.add), reads=PH + ["cmpB"], writes=["be_f"], cost=2200)
            P.op(DVE, lambda e: e.tensor_scalar(out=be_f, in0=be_f, scalar1=float(NE - 1), scalar2=None, op0=ALU.min), reads=PH + ["be_f"], writes=["be_f"])
            P.op(DVE, lambda e: e.tensor_scalar(out=iw_f, in0=be_f, scalar1=128.0, scalar2=cmoe[:, 464:465], op0=ALU.mult, op1=ALU.add), reads=PH + ["be_f", "cmoe"], writes=["iw_f"])
            P.op(DVE, lambda e: e.tensor_copy(out=be_i, in_=iw_f), reads=PH + ["iw_f"], writes=["be_i"])
            zn = ["xs_z%d" % b_ for b_ in range(NB)]
            for i in range(NT):
                for (di_, nm) in ((dA_i, "A"), (dB_i, "B")):
                    P.op(POOL, lambda e, i=i, di_=di_: e.indirect_dma_start(out=xs_d[:, :], out_offset=bass.IndirectOffsetOnAxis(ap=di_[:, i:i + 1], axis=0),
                                                                           in_=h2sb[:, i, :], in_offset=None),
                         reads=PH + ["h2sb%d" % (i // 8), "d%s_i" % nm] + zn,
                         writes=["xs_w%d%s" % (i, nm)], dma="scat", lat=6000.0)
            xs_names = ["xs_w%d%s" % (i, nm) for i in range(NT) for nm in "AB"]
            for b_ in range(NB):
                w = b_ % NW
                r2 = b_ % 2

                for (dst, src_t, nm) in ((Wg[w], wgb_d, "g"), (Wu[w], wub_d, "u"), (Wd[w], wdb_d, "d")):
                    P.op(POOL, lambda e, dst=dst, src_t=src_t, b_=b_: e.indirect_dma_start(out=dst.rearrange("p k n -> p (k n)"), out_offset=None, in_=src_t[:, :],
                                                                                      in_offset=bass.IndirectOffsetOnAxis(ap=be_i[:, b_:b_ + 1], axis=0)),
                         reads=PH + ["be_i"], writes=["W%s%d" % (nm, w)], dma="w%s%d" % (nm, w), lat=9000.0, cost=1500)
                dma(SP, xb[r2], xs_d[b_ * RB:(b_ + 1) * RB, :].rearrange("(s p) d -> p s d", p=128), "xb%d" % r2,
                    reads=PH + xs_names, writes=["xb%d" % r2], lat=5000.0)
                for sb_ in range(SB):
                    pt, pn = psum_bf(1024)
                    for k in range(8):
                        P.op(PE, lambda e, k=k, pt=pt, sb_=sb_, r2=r2: e.transpose(out=pt[:, k * 128:(k + 1) * 128], in_=xb[r2][:, sb_, k * 128:(k + 1) * 128], identity=identm),
                             reads=PH + ["xb%d" % r2, "identm"], writes=pn, cost=100)
                    P.op(ACT, lambda e, pt=pt, sb_=sb_, r2=r2: e.activation(out=XT[r2][:, :, sb_ * 128:(sb_ + 1) * 128], in_=pt.rearrange("p (k n) -> p k n", k=8), func=AF.Copy),
                         reads=PH + pn, writes=["XT%d_%d" % (r2, sb_)], cost=1200)
                xtn = ["XT%d_%d" % (r2, sb_) for sb_ in range(SB)]
                for f in range(2):
                    pg_, pgn = psum(RB)
                    for k in range(8):
                        P.op(PE, lambda e, k=k, pg_=pg_, f=f, w=w, r2=r2: e.matmul(pg_, lhsT=Wg[w][:, k, f * 128:(f + 1) * 128], rhs=XT[r2][:, k, :], start=(k == 0), stop=(k == 7)),
                             reads=PH + ["Wg%d" % w] + xtn, writes=pgn, cost=160)
                    pu_, pun = psum(RB)
                    for k in range(8):
                        P.op(PE, lambda e, k=k, pu_=pu_, f=f, w=w, r2=r2: e.matmul(pu_, lhsT=Wu[w][:, k, f * 128:(f + 1) * 128], rhs=XT[r2][:, k, :], start=(k == 0), stop=(k == 7)),
                             reads=PH + ["Wu%d" % w] + xtn, writes=pun, cost=160)
                    tg_ = tg[f]
                    sg_ = sg[f]
                    P.op(ACT, lambda e, pg_=pg_, tg_=tg_: e.activation(out=tg_, in_=pg_, func=AF.Tanh, scale=0.5), reads=PH + pgn, writes=["tg%d" % f], cost=500)
                    P.op(DVE, lambda e, pg_=pg_, tg_=tg_, sg_=sg_: e.scalar_tensor_tensor(out=sg_, in0=tg_, scalar=1.0, in1=pg_, op0=ALU.add, op1=ALU.mult),
                         reads=PH + pgn + ["tg%d" % f], writes=["sg%d" % f], cost=400)
                    P.op(DVE, lambda e, pu_=pu_, sg_=sg_, f=f, r2=r2: e.tensor_tensor(out=hid[r2][:, f, :], in0=pu_, in1=sg_, op=ALU.mult),
                         reads=PH + pun + ["sg%d" % f], writes=["hid%d_%d" % (r2, f)], cost=400)
                for sb_ in range(SB):
                    for half in range(2):
                        py, pyn = psum(512)
                        for f in range(2):
                            P.op(PE, lambda e, f=f, py=py, sb_=sb_, half=half, w=w, r2=r2: e.matmul(py, lhsT=hid[r2][:, f, sb_ * 128:(sb_ + 1) * 128], rhs=Wd[w][:, f, half * 512:(half + 1) * 512], start=(f == 0), stop=(f == 1)),
                                 reads=PH + ["hid%d_0" % r2, "hid%d_1" % r2, "Wd%d" % w], writes=pyn, cost=260)
                        if (sb_ + half) % 2 == 0:
                            P.op(ACT, lambda e, py=py, sb_=sb_, half=half, r2=r2: e.activation(out=yb[r2][:, sb_, half * 512:(half + 1) * 512], in_=py, func=AF.Copy),
                                 reads=PH + pyn, writes=["yb%d_%d%d" % (r2, sb_, half)], cost=750)
                        else:
                            P.op(DVE, lambda e, py=py, sb_=sb_, half=half, r2=r2: e.tensor_copy(out=yb[r2][:, sb_, half * 512:(half + 1) * 512], in_=py),
                                 reads=PH + pyn, writes=["yb%d_%d%d" % (r2, sb_, half)], cost=650)
                dma(SP, ys_d[b_ * RB:(b_ + 1) * RB, :].rearrange("(s p) d -> p s d", p=128), yb[r2], "st_y%d" % r2,
                    reads=PH + ["yb%d_%d%d" % (r2, sb_, half) for sb_ in range(SB) for half in range(2)], writes=["ys_w%d" % b_], lat=5000.0)
            ys_names = ["ys_w%d" % b_ for b_ in range(NB)]
            for i in range(NT):
                r2 = i % CR
                dma(SP, x2t[r2], out[i * 128:(i + 1) * 128, :], "x2t%d" % r2, reads=PH + xs_names, writes=["x2t%d" % r2])
                for (ybuf, di_, nm) in ((yA[r2], dA_i, "A"), (yB[r2], dB_i, "B")):
                    P.op(POOL, lambda e, i=i, di_=di_, ybuf=ybuf: e.indirect_dma_start(out=ybuf, out_offset=None, in_=ys_d[:, :],
                                                                                    in_offset=bass.IndirectOffsetOnAxis(ap=di_[:, i:i + 1], axis=0)),
                         reads=PH + ["d%s_i" % nm] + ys_names, writes=["y%s%d" % (nm, r2)], dma="gy%s%d" % (nm, r2), lat=6000.0)
                P.op(DVE, lambda e, i=i, r2=r2: e.scalar_tensor_tensor(out=x2t[r2], in0=yA[r2], scalar=gAB[:, i:i + 1], in1=x2t[r2], op0=ALU.mult, op1=ALU.add),
                     reads=PH + ["yA%d" % r2, "gAB", "x2t%d" % r2], writes=["x2t%d" % r2], cost=1200)
                P.op(DVE, lambda e, i=i, r2=r2: e.scalar_tensor_tensor(out=x2t[r2], in0=yB[r2], scalar=gAB[:, NT + i:NT + i + 1], in1=x2t[r2], op0=ALU.mult, op1=ALU.add),
                     reads=PH + ["yB%d" % r2, "gAB", "x2t%d" % r2], writes=["x2t%d" % r2], cost=1200)
                dma(SP, out[i * 128:(i + 1) * 128, :], x2t[r2], "st_o%d" % r2, reads=PH + ["x2t%d" % r2])

        nsem = P.emit(nc)
        print("ops", len(P.ops), "sems", nsem, "model_us", getattr(P, "model_time", 0) / 1e3)
    return nc


_CACHE = {}


def _consts():
    ang_f = (500000.0 ** (-np.arange(0, 16, 2, dtype=np.float32) / 16.0)).astype(np.float32)
    ang = np.arange(S, dtype=np.float32)[:, None] * ang_f[None, :]
    c, s_ = np.cos(ang).astype(np.float32), np.sin(ang).astype(np.float32)
    ropeC = np.concatenate([c, c], axis=1).astype(np.float32)
    ropeS = np.concatenate([-s_, s_], axis=1).astype(np.float32)
    cmat = np.zeros((128, 512), np.float32)
    cmat[:, 0:128] = np.eye(128, dtype=np.float32)
    kk = np.arange(128)[:, None]
    qq = np.arange(128)[None, :]
    cmat[:, 128:256] = (qq <= kk).astype(np.float32)
    cmat[:, 256:384] = (kk <= qq).astype(np.float32)
    cmat[0:2, 384:448] = 0.0
    cmat[0:2, 448:512] = 1.0
    return ropeC, ropeS, cmat


def kernel(x, norm1_g, w_in, q_norm_g, k_norm_g, attn_sink, conv_w, conv_b,
           w_attn_proj, w_conv_proj, w_out, norm2_g, w_router_group, b_router_group,
           w_router_expert, b_router_expert, w_gate_e, w_up_e, w_down_e, _debug_phase1=False):
    f = lambda a: np.ascontiguousarray(np.asarray(a, dtype=np.float32))
    x = f(x)
    n = x.shape[0]
    key = bool(_debug_phase1)
    if key not in _CACHE:
        _CACHE[key] = build_nc(debug_phase1=key)
    nc = _CACHE[key]
    ropeC, ropeS, cmat = _consts()
    sel = np.zeros((128, 16), np.float32)
    shared = dict(
        w_in=f(w_in[0]), w_a=f(w_attn_proj[0]), w_c=f(w_conv_proj[0]), w_out=f(w_out[0]),
        w_r=f(np.concatenate([np.asarray(w_router_group[0]), np.asarray(w_router_expert[0])], axis=1).reshape(8, 128, 36).transpose(1, 0, 2).reshape(128, 288)),
        wg_l=f(np.asarray(w_gate_e[0]).reshape(NE, 8, 128, 256).transpose(0, 2, 1, 3).reshape(NE * 128, 2048)),
        wu_l=f(np.asarray(w_up_e[0]).reshape(NE, 8, 128, 256).transpose(0, 2, 1, 3).reshape(NE * 128, 2048)),
        wd_l=f(np.asarray(w_down_e[0]).reshape(NE, 2, 128, D).transpose(0, 2, 1, 3).reshape(NE * 128, 2048)),
        g1b=f(np.broadcast_to(np.asarray(norm1_g[0])[None, :], (128, D))),
        g2b=f(np.broadcast_to(np.asarray(norm2_g[0])[None, :], (128, D))),
        qkg=f(np.broadcast_to(np.concatenate([np.tile(np.asarray(q_norm_g[0]), 8), np.tile(np.asarray(k_norm_g[0]), 2)])[None, :], (128, 640))),
        cwl=f(np.asarray(conv_w[0]).reshape(3, 4, 128).transpose(2, 1, 0).reshape(128, 12)),
        cbl=f(np.asarray(conv_b[0]).reshape(4, 128).transpose(1, 0)),
        sinkb=f(np.broadcast_to(np.asarray(attn_sink[0])[None, :], (2, 8))),
        brb=f(np.broadcast_to(np.concatenate([np.asarray(b_router_group[0]), np.asarray(b_router_expert[0])])[None, :], (128, 36))),
        ropeC=f(ropeC.reshape(NT, 128, 16).transpose(1, 0, 2).reshape(128, NT * 16)),
        ropeS=f(ropeS.reshape(NT, 128, 16).transpose(1, 0, 2).reshape(128, NT * 16)), cmat=cmat,
    )
    cmoe = np.zeros((128, 472), np.float32)
    cmoe[:, 464] = np.arange(128, dtype=np.float32)
    cmoe[:, 0:16] = (np.arange(16, dtype=np.float32) * RB)[None, :]
    cmoe[:, 16:16 + NB] = (np.arange(NB, dtype=np.float32) * RB)[None, :]
    tt = np.arange(128)
    cmoe[:, 80:208] = (tt[:, None] < tt[None, :]).astype(np.float32)
    cmoe[:, 208:336] = 1.0
    cmoe[:, 336:464] = np.eye(128, dtype=np.float32)
    shared["cmoe"] = cmoe
    shared["zrows"] = np.zeros((RB, D // 2), np.float32)
    in_maps = [dict(shared, x=x[b]) for b in range(n)]
    res = run_bass_kernel_spmd(nc, in_maps, core_ids=list(range(n)))
    return np.stack([np.asarray(r["out"]) for r in res.results], axis=0).astype(np.float32)

CAL.update({989: 298, 225: 43, 363: 613, 398: 927, 405: 746, 476: 520, 422: 200, 424: 499, 479: 1246, 483: 84, 486: 1104, 496: 192, 511: 128, 498: 303, 430: 382, 431: 461, 433: 247, 435: 1159, 438: 474, 515: 115, 441: 469, 449: 352, 451: 278, 453: 237, 518: 421, 519: 383, 444: 532, 455: 368, 503: 154, 505: 408, 524: 164, 604: 338, 526: 171, 655: 309, 657: 494, 659: 512, 609: 134, 612: 1109, 621: 453, 627: 532, 635: 419, 631: 1134, 637: 459, 639: 3359, 642: 417, 644: 418, 652: 160, 667: 118, 654: 472, 671: 109, 683: 384, 684: 302, 661: 438, 675: 114, 679: 114, 685: 420, 687: 336, 689: 729, 699: 289, 701: 692, 706: 574, 709: 1238, 713: 101, 716: 1111, 536: 43, 561: 179, 562: 110, 563: 232, 564: 137, 567: 189, 565: 98, 570: 121, 566: 118, 571: 152, 578: 138, 572: 227, 573: 151, 582: 102, 574: 161, 575: 184, 576: 60, 586: 207, 577: 98, 579: 89, 580: 64, 590: 154, 581: 151, 591: 113, 801: 172, 808: 87, 810: 27, 812: 678, 827: 186, 829: 78, 840: 1130, 841: 1216, 842: 180, 854: 1110, 865: 1088, 873: 70, 875: 1059, 881: 115, 885: 109, 889: 327, 890: 380, 892: 334, 898: 286, 901: 644, 904: 649, 168: 3057, 914: 1256, 917: 1281, 919: 1199})
```

```python
import contextlib
import numpy as np
import concourse.bass as bass
import concourse.mybir as mybir
from concourse.bass_utils import run_bass_kernel_spmd

F32 = mybir.dt.float32
BF16 = mybir.dt.bfloat16
AF = mybir.ActivationFunctionType
ALU = mybir.AluOpType
AX = mybir.AxisListType

PE, ACT, DVE, POOL, SP = "pe", "act", "dve", "pool", "sp"
CAL = {}
USE_CAL = False

D = 1024
S = 4096
NT = S // 128
T = 256
SUB = T // 128
NS = S // T
PW = 4352
Q0, K0, V0, CB0, CC0, CX0, GA0, GC0 = 0, 512, 640, 768, 1280, 1792, 2304, 3328
NE = 32
EPS = 1e-6
KVR = 8
UR = 3
RB = 256
SB = RB // 128
NB = (2 * S) // RB + NE
NROW = NB * RB


class Prog:
    def __init__(self):
        self.ops = []
        self.res = {}

    DEF_COST = {PE: 200.0, ACT: 700.0, DVE: 450.0, POOL: 1200.0, SP: 100.0}

    def op(self, eng, fn, reads=(), writes=(), dma=None, cost=None, lat=None):
        if cost is None:
            cost = 100.0 if dma is not None else self.DEF_COST[eng]
        if lat is None:
            lat = 3500.0 if dma is not None else 0.0
        o = dict(id=len(self.ops), eng=eng, fn=fn, dma=dma, deps=set(), sig=None, used=False, cost=cost, lat=lat)
        try:
            cal = CAL.get(fn.__code__.co_firstlineno)
            if cal is not None and dma is None and USE_CAL:
                o["cost"] = float(cal)
        except Exception:
            pass
        for r in reads:
            st = self.res.setdefault(r, [None, []])
            if st[0] is not None:
                o["deps"].add(st[0]["id"])
        for w in writes:
            st = self.res.setdefault(w, [None, []])
            if st[0] is not None:
                o["deps"].add(st[0]["id"])
            for rd in st[1]:
                o["deps"].add(rd["id"])
        for r in reads:
            self.res[r][1].append(o)
        for w in writes:
            self.res[w] = [o, []]
        o["deps"].discard(o["id"])
        self.ops.append(o)
        return o

    def barrier(self, eng, fn, extra=()):
        last = {}
        for o in self.ops:
            key = ("dma", o["dma"]) if o["dma"] is not None else ("eng", o["eng"])
            last[key] = o["id"]
        o = dict(id=len(self.ops), eng=eng, fn=fn, dma=None, deps=set(last.values()), sig=None, used=False, cost=100.0, lat=0.0)
        for r in list(self.res.keys()) + list(extra):
            self.res[r] = [o, []]
        self.ops.append(o)
        return o

    def schedule(self):
        import heapq
        ops = self.ops
        n = len(ops)
        ndeps = [len(o["deps"]) for o in ops]
        users = [[] for _ in range(n)]
        for o in ops:
            for d in o["deps"]:
                users[d].append(o["id"])
        ready = [0.0] * n
        fin = [0.0] * n
        heaps = {PE: [], ACT: [], DVE: [], POOL: [], SP: []}
        free = {PE: 0.0, ACT: 0.0, DVE: 0.0, POOL: 0.0, SP: 0.0}
        for o in ops:
            if ndeps[o["id"]] == 0:
                heapq.heappush(heaps[o["eng"]], (0.0, o["id"]))
        order = []
        SEM = 50.0
        while len(order) < n:
            best = None
            for eng, h in heaps.items():
                if not h:
                    continue
                t = max(free[eng], h[0][0])
                if best is None or t < best[0]:
                    best = (t, eng)
            t, eng = best
            h = heaps[eng]
            cand = []
            while h and h[0][0] <= t:
                cand.append(heapq.heappop(h))
            pick = min(cand, key=lambda c: c[1])
            for c in cand:
                if c is not pick:
                    heapq.heappush(h, c)
            i = pick[1]
            o = ops[i]
            start = max(free[eng], ready[i])
            free[eng] = start + o["cost"]
            fin[i] = start + o["cost"] + o["lat"]
            order.append(i)
            for u in users[i]:
                ready[u] = max(ready[u], fin[i] + SEM)
                ndeps[u] -= 1
                if ndeps[u] == 0:
                    heapq.heappush(heaps[ops[u]["eng"]], (ready[u], u))
        self.model_time = max(fin)
        return order

    def emit(self, nc, reorder=True):
        ops = self.ops
        order = self.schedule() if reorder else list(range(len(ops)))
        ops_sched = [ops[i] for i in order]
        for o in ops:
            if o["eng"] == PE and o["dma"] is None:
                o["deps"] = {d for d in o["deps"]
                             if not (ops[d]["eng"] == PE and ops[d]["dma"] is None)}
            for d in o["deps"]:
                ops[d]["used"] = True
        semkeys = {}
        counts = {}
        for o in ops_sched:
            key = ("dma", o["dma"]) if o["dma"] is not None else ("eng", o["eng"])
            if o["dma"] is not None or o["used"]:
                inc = 16 if o["dma"] is not None else 1
                counts[key] = counts.get(key, 0) + inc
                o["sig"] = (key, counts[key], inc)
                semkeys[key] = None
        with contextlib.ExitStack() as es:
            for i, key in enumerate(semkeys):
                semkeys[key] = es.enter_context(nc.semaphore("s%d" % i))
            block = es.enter_context(nc.Block())
            per_eng = {PE: [], ACT: [], DVE: [], POOL: [], SP: []}
            for o in ops_sched:
                per_eng[o["eng"]].append(o)

            def mk(eng_name, eng_ops):
                def body(e):
                    known = {}
                    for o in eng_ops:
                        need = {}
                        for d in o["deps"]:
                            key, val, _ = ops[d]["sig"]
                            if need.get(key, 0) < val:
                                need[key] = val
                        for key, val in need.items():
                            if known.get(key, 0) < val:
                                e.wait_ge(semkeys[key], val)
                                known[key] = val
                        ins = o["fn"](e)
                        if o["sig"] is not None:
                            key, val, inc = o["sig"]
                            ins.then_inc(semkeys[key], inc)
                    if eng_name == SP:
                        for key, val in counts.items():
                            if known.get(key, 0) < val:
                                e.wait_ge(semkeys[key], val)
                return body

            reg = {PE: block.tensor, ACT: block.scalar, DVE: block.vector,
                   POOL: block.gpsimd, SP: block.sync}
            for eng_name, eng_ops in per_eng.items():
                reg[eng_name](mk(eng_name, eng_ops))
        return len(semkeys)


class Arena:
    def __init__(self, ap, size):
        self.ap = ap
        self.size = size
        self.off = 0

    def alloc(self, cols, dt=BF16):
        if dt == F32:
            self.off += self.off & 1
            a = self.ap[:, self.off:self.off + 2 * cols].bitcast(F32)
            self.off += 2 * cols
        else:
            a = self.ap[:, self.off:self.off + cols]
            self.off += cols
        assert self.off <= self.size, ("arena overflow", self.off, self.size)
        return a


def build_nc(debug_phase1=False):
    nc = bass.Bass("TRN2", target_bir_lowering=False)

    def din(name, shape, dt=F32):
        return nc.dram_tensor(name, list(shape), dt, kind="ExternalInput").ap()

    x = din("x", [S, D])
    w_in = din("w_in", [D, PW])
    w_a = din("w_a", [512, D])
    w_c = din("w_c", [512, D])
    w_out = din("w_out", [D, D])
    w_r = din("w_r", [128, 8 * 36])
    wg_l = din("wg_l", [NE * 128, 2048])
    wu_l = din("wu_l", [NE * 128, 2048])
    wd_l = din("wd_l", [NE * 128, 2048])
    g1b_d = din("g1b", [128, D])
    g2b_d = din("g2b", [128, D])
    qkg_d = din("qkg", [128, 640])
    cwl_d = din("cwl", [128, 12])
    cbl_d = din("cbl", [128, 4])
    sinkb_d = din("sinkb", [2, 8])
    brb_d = din("brb", [128, 36])
    ropeC_d = din("ropeC", [128, NT * 16])
    ropeS_d = din("ropeS", [128, NT * 16])
    cmat_d = din("cmat", [128, 512])
    cmoe_d = din("cmoe", [128, 472])
    zrows_d = din("zrows", [RB, D // 2])
    out = nc.dram_tensor("out", [S, D], F32, kind="ExternalOutput").ap()
    h2_d = nc.dram_tensor("h2_scr", [S, D], BF16).ap()
    xs_d = nc.dram_tensor("xs_scr", [NROW, D], BF16).ap()
    wgb_d = nc.dram_tensor("wgb_scr", [NE * 128, 2048], BF16).ap()
    wub_d = nc.dram_tensor("wub_scr", [NE * 128, 2048], BF16).ap()
    wdb_d = nc.dram_tensor("wdb_scr", [NE * 128, 2048], BF16).ap()
    ys_d = nc.dram_tensor("ys_scr", [NROW, D], F32).ap()

    P = Prog()
    ARENA = 106000
    with contextlib.ExitStack() as es:
        arena_t = es.enter_context(nc.sbuf_tensor("arena", [128, ARENA], BF16))
        psb = [es.enter_context(nc.psum_tensor("ps%d" % i, [128, 512], F32)) for i in range(8)]

        pstate = {"h": 0}

        def psum(cols):
            if cols <= 256:
                h = pstate["h"]
                pstate["h"] = (h + 1) % 16
                b, half = h // 2, h % 2
                return psb[b][:, half * 256:half * 256 + cols], ["psb%d" % b]
            h = pstate["h"]
            h += h & 1
            h %= 16
            pstate["h"] = (h + 2) % 16
            b = h // 2
            return psb[b][:, 0:cols], ["psb%d" % b]

        def psum_bf(cols):
            a, names = psum(512)
            return a.bitcast(BF16)[:, 0:cols], names

        A0 = Arena(arena_t, ARENA)
        ohA = A0.alloc(NT * NE).rearrange("p (i e) -> p i e", i=NT)
        ohB = A0.alloc(NT * NE).rearrange("p (i e) -> p i e", i=NT)
        gAB = A0.alloc(2 * NT, F32)
        persist_end = A0.off

        A = Arena(arena_t, ARENA)
        A.off = persist_end
        Win = A.alloc(8 * PW).rearrange("p (k n) -> p k n", k=8)
        Wa = A.alloc(4 * D).rearrange("p (k n) -> p k n", k=4)
        Wc = A.alloc(4 * D).rearrange("p (k n) -> p k n", k=4)
        Wout = A.alloc(8 * D).rearrange("p (k n) -> p k n", k=8)
        Wr = A.alloc(8 * 36).rearrange("p (k n) -> p k n", k=8)
        hT = [A.alloc(8 * T).rearrange("p (k n) -> p k n", k=8) for _ in range(2)]
        xring = [A.alloc(D, F32) for _ in range(2)]
        xres = [A.alloc(D, F32) for _ in range(2)]
        g1b = A.alloc(D, F32)
        g2b = A.alloc(D, F32)
        qkg = A.alloc(640, F32)
        ropeC = A.alloc(NT * 16, F32).rearrange("p (i c) -> p i c", i=NT)
        ropeS = A.alloc(NT * 16, F32).rearrange("p (i c) -> p i c", i=NT)
        hb = A.alloc(D)
        h2b = A.alloc(D)
        kT = A.alloc(2 * KVR * 128).rearrange("p (g n) -> p g n", g=2)
        vring = A.alloc(KVR * 2 * 128).rearrange("p (s g n) -> p s g n", s=KVR, g=2)
        uring = [A.alloc(4 * (T + 2)).rearrange("p (c n) -> p c n", c=4) for _ in range(UR)]
        qT = [A.alloc(8 * 128).rearrange("p (h n) -> p h n", h=8) for _ in range(2)]
        sq = A.alloc(512, F32)
        xn = A.alloc(512, F32)
        rt1 = A.alloc(128, F32)
        rt2 = A.alloc(128, F32)
        qkb = A.alloc(512)
        pT = [A.alloc(512) for _ in range(4)]
        aT = A.alloc(4 * T).rearrange("p (c n) -> p c n", c=4)
        rD = A.alloc(512, F32)
        cbT = A.alloc(4 * T).rearrange("p (c n) -> p c n", c=4)
        cct = [A.alloc(T, F32) for _ in range(2)]
        cv1 = A.alloc(T, F32)
        cv2 = A.alloc(T, F32)
        cT = A.alloc(4 * T).rearrange("p (c n) -> p c n", c=4)
        ta = [A.alloc(T) for _ in range(2)]
        tcg = [A.alloc(T) for _ in range(2)]
        m1 = A.alloc(T, F32)
        m2 = A.alloc(T, F32)
        mergedT = A.alloc(8 * T).rearrange("p (k n) -> p k n", k=8)
        h2T = [A.alloc(8 * 128).rearrange("p (k n) -> p k n", k=8) for _ in range(2)]
        identb = A.alloc(128)
        maskPb = A.alloc(128)
        maskNb = A.alloc(128)
        onesD2 = A.alloc(128)
        esrow = A.alloc(8 * 128).rearrange("p (h n) -> p h n", h=8)
        cmat = A.alloc(512, F32)
        cw = A.alloc(12, F32)
        cbias = A.alloc(4, F32)
        brb = A.alloc(36, F32)
        st = A.alloc(64, F32)
        rtmp = A.alloc(256, F32)
        sk = A.alloc(64, F32)
        skb = A.alloc(16)
        print("phase1 arena bytes", A.off * 2)

        mhalf = st[:, 0:16]
        ss1 = [st[:, 16 + r:17 + r] for r in range(2)]
        rs1 = [st[:, 18 + r:19 + r] for r in range(2)]
        ss2 = [st[:, 20 + r:21 + r] for r in range(2)]
        rs2 = [st[:, 22 + r:23 + r] for r in range(2)]
        ssq = st[:, 24:32]
        rsq = st[:, 32:40]
        vsq = st[:, 40:48]

        def dma(eng, out_ap, in_ap, key, reads=(), writes=(), lat=None):
            return P.op(eng, lambda e: e.dma_start(out=out_ap, in_=in_ap), reads=reads, writes=writes, dma=key, lat=lat)

        dma(SP, cmat, cmat_d, "c_cmat", writes=["cmat"])
        dma(SP, g1b, g1b_d, "c_g1b", writes=["g1b"])
        dma(SP, qkg, qkg_d, "c_qkg", writes=["qkg"])
        dma(SP, ropeC, ropeC_d.rearrange("p (i c) -> p i c", i=NT), "c_ropeC", writes=["ropeC"])
        dma(SP, ropeS, ropeS_d.rearrange("p (i c) -> p i c", i=NT), "c_ropeS", writes=["ropeS"])
        dma(SP, cw, cwl_d, "c_cw", writes=["cw"])
        dma(SP, cbias, cbl_d, "c_cb", writes=["cbias"])
        dma(SP, brb, brb_d, "c_brb", writes=["brb"])
        dma(SP, sk[0:2, 0:8], sinkb_d, "c_sink", writes=["sk"])
        dma(SP, g2b, g2b_d, "c_g2b", writes=["g2b"])
        for (c0, c1, nm_) in ((512, 768, "C"), (1280, 2304, "C"), (0, 512, "D"), (768, 1280, "D"), (2304, 4352, "D")):
            for k in range(8):
                dma(POOL, Win[:, k, c0:c1], w_in[k * 128:(k + 1) * 128, c0:c1], "w_in" + nm_, writes=["Win%s_%d_%d" % (nm_, k, c0)])
        WinC_n = ["WinC_%d_%d" % (k, c0) for k in range(8) for c0 in (512, 1280)]
        WinD_n = ["WinD_%d_%d" % (k, c0) for k in range(8) for c0 in (0, 768, 2304)]
        dma(POOL, Wa, w_a.rearrange("(c p) n -> p c n", p=128), "w_a", writes=["Wa"])
        dma(POOL, Wc, w_c.rearrange("(c p) n -> p c n", p=128), "w_c", writes=["Wc"])
        dma(POOL, Wout, w_out.rearrange("(k p) n -> p k n", p=128), "w_out", writes=["Wout"])
        dma(POOL, Wr, w_r.rearrange("p (k n) -> p k n", k=8), "w_r", writes=["Wr"])

        P.op(DVE, lambda e: e.memset(mhalf, -0.5), writes=["mhalf"])
        P.op(DVE, lambda e: e.tensor_copy(out=identb, in_=cmat[:, 0:128]), reads=["cmat"], writes=["identb"])
        P.op(DVE, lambda e: e.tensor_copy(out=maskPb, in_=cmat[:, 128:256]), reads=["cmat"], writes=["maskPb"])
        P.op(DVE, lambda e: e.tensor_copy(out=maskNb, in_=cmat[:, 256:384]), reads=["cmat"], writes=["maskNb"])
        P.op(DVE, lambda e: e.tensor_copy(out=onesD2[0:2, :], in_=cmat[0:2, 384:512]), reads=["cmat"], writes=["onesD2"])
        P.op(DVE, lambda e: e.tensor_scalar(out=g1b, in0=g1b, scalar1=32.0, scalar2=None, op0=ALU.mult), reads=["g1b"], writes=["g1b"])
        P.op(DVE, lambda e: e.tensor_scalar(out=g2b, in0=g2b, scalar1=32.0, scalar2=None, op0=ALU.mult), reads=["g2b"], writes=["g2b"])
        P.op(DVE, lambda e: e.tensor_scalar(out=qkg, in0=qkg, scalar1=8.0, scalar2=None, op0=ALU.mult), reads=["qkg"], writes=["qkg"])
        P.op(POOL, lambda e: e.memset(vring[:, :, :, 64:128], 1.0), writes=["vones"])
        for r in range(UR):
            P.op(POOL, lambda e, r=r: e.memset(uring[r], 0.0), writes=["u%d" % r, "uh0_%d" % r, "uh1_%d" % r])
        sel = cmat[0:2, 0:2]
        e_f = sk[0:2, 8:16]
        hi_f = sk[0:2, 16:24]
        lo_f = sk[0:2, 24:32]
        fin = sk[0:2, 32:40]
        P.op(ACT, lambda e: e.activation(out=e_f, in_=sk[0:2, 0:8], func=AF.Exp), reads=["sk"], writes=["sk_e"])
        P.op(DVE, lambda e: e.tensor_copy(out=skb[0:2, 0:8], in_=e_f), reads=["sk_e"], writes=["skb"])
        P.op(DVE, lambda e: e.tensor_copy(out=hi_f, in_=skb[0:2, 0:8]), reads=["skb"], writes=["sk_hi"])
        P.op(DVE, lambda e: e.tensor_tensor(out=lo_f, in0=e_f, in1=hi_f, op=ALU.subtract), reads=["sk_e", "sk_hi"], writes=["sk_lo"])
        P.op(DVE, lambda e: e.tensor_scalar(out=fin, in0=hi_f, scalar1=sel[:, 0:1], scalar2=None, op0=ALU.mult), reads=["sk_hi", "cmat"], writes=["sk_fin"])
        P.op(DVE, lambda e: e.scalar_tensor_tensor(out=fin, in0=lo_f, scalar=sel[:, 1:2], in1=fin, op0=ALU.mult, op1=ALU.add), reads=["sk_lo", "sk_fin", "cmat"], writes=["sk_fin"])
        P.op(DVE, lambda e: e.tensor_copy(out=esrow[0:2, :, :], in_=fin[:, :, None].broadcast_to([2, 8, 128])), reads=["sk_fin"], writes=["esrow"])

        import os
        KLEVEL = int(os.environ.get("KLEVEL", "3"))
        KNS = int(os.environ.get("KNS", str(NS)))
        def rms_rstd(ss, rs, n, tag):
            w = ss.shape[-1]
            P.op(POOL, lambda e: e.tensor_scalar(out=rs, in0=ss, scalar1=float(n * EPS), scalar2=None, op0=ALU.add),
                 reads=["ss" + tag], writes=["rs" + tag])
            P.op(POOL, lambda e: e.tensor_tensor(out=rs, in0=rs, in1=mhalf[:, 0:w], op=ALU.pow),
                 reads=["rs" + tag, "mhalf"], writes=["rs" + tag])

        def qk_post(ps_ap, ps_names, H, gain, i):
            W = H * 64
            P.op(ACT, lambda e: e.activation(out=sq[:, 0:W], in_=ps_ap, func=AF.Square), reads=ps_names, writes=["sq"])
            P.op(DVE, lambda e: e.tensor_reduce(out=ssq[:, 0:H], in_=sq[:, 0:W].rearrange("p (h d) -> p h d", h=H), axis=AX.X, op=ALU.add),
                 reads=["sq"], writes=["ssq"])
            P.op(POOL, lambda e: e.tensor_scalar(out=vsq[:, 0:H], in0=ssq[:, 0:H], scalar1=float(64 * EPS), scalar2=None, op0=ALU.add),
                 reads=["ssq"], writes=["vsq"])
            P.op(POOL, lambda e: e.tensor_tensor(out=rsq[:, 0:H], in0=vsq[:, 0:H], in1=mhalf[:, 0:H], op=ALU.pow),
                 reads=["vsq", "mhalf"], writes=["rsq"])
            xn3 = xn[:, 0:W].rearrange("p (h d) -> p h d", h=H)
            P.op(DVE, lambda e: e.tensor_tensor(out=xn3, in0=ps_ap.rearrange("p (h d) -> p h d", h=H),
                                                in1=rsq[:, 0:H, None].broadcast_to([128, H, 64]), op=ALU.mult),
                 reads=ps_names + ["rsq"], writes=["xn"])
            P.op(DVE, lambda e: e.tensor_tensor(out=xn[:, 0:W], in0=xn[:, 0:W], in1=gain, op=ALU.mult),
                 reads=["xn", "qkg"], writes=["xn"])
            qb3 = qkb[:, 0:W].rearrange("p (h d) -> p h d", h=H)
            P.op(ACT, lambda e: e.activation(out=qkb[:, 0:W], in_=xn[:, 0:W], func=AF.Copy), reads=["xn"], writes=["qkb"])
            t1 = rt1[:, 0:H * 16].rearrange("p (h c) -> p h c", h=H)
            t2 = rt2[:, 0:H * 16].rearrange("p (h c) -> p h c", h=H)
            rc = ropeC[:, i:i + 1, :]
            rs_ = ropeS[:, i:i + 1, :]
            P.op(POOL, lambda e: e.tensor_tensor(out=t1, in0=xn3[:, :, 0:16], in1=rc.broadcast_to([128, H, 16]), op=ALU.mult),
                 reads=["xn", "ropeC"], writes=["rt1"])
            P.op(POOL, lambda e: e.tensor_tensor(out=t2[:, :, 0:8], in0=xn3[:, :, 8:16], in1=rs_[:, :, 0:8].broadcast_to([128, H, 8]), op=ALU.mult),
                 reads=["xn", "ropeS"], writes=["rt2a"])
            P.op(POOL, lambda e: e.tensor_tensor(out=t2[:, :, 8:16], in0=xn3[:, :, 0:8], in1=rs_[:, :, 8:16].broadcast_to([128, H, 8]), op=ALU.mult),
                 reads=["xn", "ropeS"], writes=["rt2b"])
            P.op(POOL, lambda e: e.tensor_tensor(out=qb3[:, :, 0:16], in0=t1, in1=t2, op=ALU.add),
                 reads=["rt1", "rt2a", "rt2b", "qkb"], writes=["qkb"])

        zsrc = zrows_d.bitcast(BF16)

        def stage_B(s):
            if (not debug_phase1) and s >= 2:
                for b_ in range((s - 2) * 5, min(NB, (s - 1) * 5)):
                    dma(SP, xs_d[b_ * RB:(b_ + 1) * RB, :], zsrc, "zf%d" % (b_ % 4), reads=["tick%d" % (s - 1)], writes=["xs_z%d" % b_])
            if (not debug_phase1) and s >= 1:
                for ex_ in (range(2 * (s - 1), 2 * (s - 1) + 2) if s < NS - 1 else range(2 * (s - 1), NE)):
                    for (src_t, dst_t, nm) in ((wg_l, wgb_d, "g"), (wu_l, wub_d, "u"), (wd_l, wdb_d, "d")):
                        dma(POOL, dst_t[ex_ * 128:(ex_ + 1) * 128, :], src_t[ex_ * 128:(ex_ + 1) * 128, :], "pc%s%d" % (nm, ex_ % 2),
                            reads=["tick%d" % (s - 1)], writes=["wb_%s%d" % (nm, ex_)])
            for j in range(SUB):
                i = s * SUB + j
                r = i % 2
                xs = xring[r]
                dma(SP, xs, x[i * 128:(i + 1) * 128, :], "x%d" % r, writes=["x%d" % r])
                P.op(ACT, lambda e, xs=xs, r=r: e.activation(out=hb, in_=xs, func=AF.Square, accum_out=ss1[r]),
                     reads=["x%d" % r], writes=["hb", "ss1_%d" % r], cost=1200)
                rms_rstd(ss1[r], rs1[r], D, "1_%d" % r)
                P.op(DVE, lambda e, xs=xs, r=r: e.scalar_tensor_tensor(out=hb, in0=xs, scalar=rs1[r], in1=g1b, op0=ALU.mult, op1=ALU.mult),
                     reads=["x%d" % r, "rs1_%d" % r, "g1b"], writes=["hb"], cost=1200)
                pt, pn = psum_bf(1024)
                for k in range(8):
                    P.op(PE, lambda e, k=k, pt=pt: e.transpose(out=pt[:, k * 128:(k + 1) * 128], in_=hb[:, k * 128:(k + 1) * 128], identity=identb),
                         reads=["hb", "identb"], writes=pn, cost=100)
                dst = hT[s % 2][:, :, j * 128:(j + 1) * 128]
                P.op(ACT, lambda e, pt=pt, dst=dst: e.activation(out=dst, in_=pt.rearrange("p (k n) -> p k n", k=8), func=AF.Copy),
                     reads=pn, writes=["hT%d_%d" % (s % 2, j)] + (["tick%d" % s] if j == SUB - 1 else []), cost=1200)

        def stage_C(s):
            hTs = hT[s % 2]
            for j in range(SUB):
                i = s * SUB + j
                slot = i % KVR
                pk, pkn = psum(256)
                for k in range(8):
                    P.op(PE, lambda e, k=k, pk=pk, j=j: e.matmul(pk, lhsT=hTs[:, k, j * 128:(j + 1) * 128], rhs=Win[:, k, K0:K0 + 256], start=(k == 0), stop=(k == 7)),
                         reads=["hT%d_%d" % (s % 2, j)] + WinC_n, writes=pkn, cost=160)
                P.op(ACT, lambda e, pk=pk, slot=slot: e.activation(out=vring[:, slot, :, 0:64], in_=pk[:, 128:256].rearrange("p (g d) -> p g d", g=2), func=AF.Copy),
                     reads=pkn, writes=["v%d" % slot])
                qk_post(pk[:, 0:128], pkn, 2, qkg[:, 512:640], i)
                pt, pn = psum_bf(256)
                for g in range(2):
                    P.op(PE, lambda e, g=g, pt=pt: e.transpose(out=pt[0:64, g * 128:(g + 1) * 128], in_=qkb[:, g * 64:(g + 1) * 64], identity=identb),
                         reads=["qkb", "identb"], writes=pn, cost=100)
                P.op(ACT, lambda e, pt=pt, slot=slot: e.activation(out=kT[0:64, :, slot * 128:(slot + 1) * 128], in_=pt[0:64, 0:256].rearrange("p (g n) -> p g n", g=2), func=AF.Copy),
                     reads=pn, writes=["k%d" % slot])
            us = uring[s % UR]
            for c in range(4):
                pc, pcn = psum(T)
                for k in range(8):
                    P.op(PE, lambda e, k=k, pc=pc, c=c: e.matmul(pc, lhsT=Win[:, k, CC0 + c * 128:CC0 + (c + 1) * 128], rhs=hTs[:, k, :], start=(k == 0), stop=(k == 7)),
                         reads=["hT%d_%d" % (s % 2, j) for j in range(SUB)] + WinC_n, writes=pcn, cost=160)
                px, pxn = psum(T)
                for k in range(8):
                    P.op(PE, lambda e, k=k, px=px, c=c: e.matmul(px, lhsT=Win[:, k, CX0 + c * 128:CX0 + (c + 1) * 128], rhs=hTs[:, k, :], start=(k == 0), stop=(k == 7)),
                         reads=["hT%d_%d" % (s % 2, j) for j in range(SUB)] + WinC_n, writes=pxn, cost=160)
                cc_ = cct[c % 2]
                P.op(ACT, lambda e, pc=pc, cc_=cc_: e.activation(out=cc_, in_=pc, func=AF.Copy), reads=pcn, writes=["cct%d" % (c % 2)])
                P.op(DVE, lambda e, px=px, cc_=cc_, c=c: e.tensor_tensor(out=us[:, c, 1:T + 1], in0=px, in1=cc_, op=ALU.mult),
                     reads=pxn + ["cct%d" % (c % 2)], writes=["u%d" % (s % UR)])
            if s > 0:
                up = uring[(s - 1) % UR]
                P.op(POOL, lambda e: e.tensor_copy(out=us[:, :, 0:1], in_=up[:, :, T:T + 1]),
                     reads=["u%d" % ((s - 1) % UR)], writes=["uh0_%d" % (s % UR)])
                P.op(POOL, lambda e: e.tensor_copy(out=up[:, :, T + 1:T + 2], in_=us[:, :, 1:2]),
                     reads=["u%d" % (s % UR)], writes=["uh1_%d" % ((s - 1) % UR)])
            else:
                P.op(POOL, lambda e: e.memset(us[:, :, 0:1], 0.0), writes=["uh0_%d" % (s % UR)])
            if s == KNS - 1:
                P.op(POOL, lambda e: e.memset(us[:, :, T + 1:T + 2], 0.0), writes=["uh1_%d" % (s % UR)])

        def router(i, h2Ti):
            pr, prn = psum(36)
            for k in range(8):
                P.op(PE, lambda e, k=k: e.matmul(pr, lhsT=h2Ti[:, k, :], rhs=Wr[:, k, :], start=(k == 0), stop=(k == 7)),
                     reads=["h2T%d" % (i % 2), "Wr"], writes=prn, cost=70)
            lg = rtmp[:, 0:36]
            gmax = rtmp[:, 36:37]
            goh = rtmp[:, 40:44]
            gd = rtmp[:, 44:48]
            gex = rtmp[:, 48:52]
            gsum = rtmp[:, 52:53]
            pg = rtmp[:, 53:54]
            tmp48 = rtmp[:, 64:96].rearrange("p (g j) -> p g j", g=4)
            ein = rtmp[:, 96:104]
            mx1 = rtmp[:, 104:105]
            oh1 = rtmp[:, 112:120]
            e2 = rtmp[:, 120:128]
            mx2 = rtmp[:, 105:106]
            oh2 = rtmp[:, 128:136]
            d12 = rtmp[:, 106:107]
            t12 = rtmp[:, 107:108]
            pa = rtmp[:, 108:109]
            gq1 = rtmp[:, 109:110]
            gq2 = rtmp[:, 110:111]
            nt12 = rtmp[:, 111:112]
            gw = rtmp[:, 136:144]
            gw2 = rtmp[:, 144:152]
            R = "rt_"
            P.op(DVE, lambda e: e.tensor_tensor(out=lg, in0=pr, in1=brb, op=ALU.add), reads=prn + ["brb"], writes=[R + "lg"])
            P.op(DVE, lambda e: e.tensor_reduce(out=gmax, in_=lg[:, 0:4], axis=AX.X, op=ALU.max), reads=[R + "lg"], writes=[R + "gmax"])
            P.op(DVE, lambda e: e.tensor_scalar(out=goh, in0=lg[:, 0:4], scalar1=gmax, scalar2=None, op0=ALU.is_equal), reads=[R + "lg", R + "gmax"], writes=[R + "goh"])
            P.op(DVE, lambda e: e.tensor_scalar(out=gd, in0=lg[:, 0:4], scalar1=gmax, scalar2=None, op0=ALU.subtract), reads=[R + "lg", R + "gmax"], writes=[R + "gd"])
            P.op(ACT, lambda e: e.activation(out=gex, in_=gd, func=AF.Exp, accum_out=gsum), reads=[R + "gd"], writes=[R + "gex", R + "gsum"])
            P.op(DVE, lambda e: e.reciprocal(out=pg, in_=gsum), reads=[R + "gsum"], writes=[R + "pg"])
            P.op(DVE, lambda e: e.tensor_tensor(out=tmp48, in0=lg[:, 4:36].rearrange("p (g j) -> p g j", g=4),
                                                in1=goh[:, :, None].broadcast_to([128, 4, 8]), op=ALU.mult),
                 reads=[R + "lg", R + "goh"], writes=[R + "tmp48"])
            P.op(DVE, lambda e: e.tensor_reduce(out=ein, in_=tmp48.rearrange("p g j -> p j g"), axis=AX.X, op=ALU.add), reads=[R + "tmp48"], writes=[R + "ein"])
            P.op(DVE, lambda e: e.tensor_reduce(out=mx1, in_=ein, axis=AX.X, op=ALU.max), reads=[R + "ein"], writes=[R + "mx1"])
            P.op(DVE, lambda e: e.tensor_scalar(out=oh1, in0=ein, scalar1=mx1, scalar2=None, op0=ALU.is_equal), reads=[R + "ein", R + "mx1"], writes=[R + "oh1"])
            P.op(DVE, lambda e: e.scalar_tensor_tensor(out=e2, in0=oh1, scalar=-1e30, in1=ein, op0=ALU.mult, op1=ALU.add), reads=[R + "oh1", R + "ein"], writes=[R + "e2"])
            P.op(DVE, lambda e: e.tensor_reduce(out=mx2, in_=e2, axis=AX.X, op=ALU.max), reads=[R + "e2"], writes=[R + "mx2"])
            P.op(DVE, lambda e: e.tensor_scalar(out=oh2, in0=e2, scalar1=mx2, scalar2=None, op0=ALU.is_equal), reads=[R + "e2", R + "mx2"], writes=[R + "oh2"])
            P.op(DVE, lambda e: e.tensor_tensor(out=d12, in0=mx1, in1=mx2, op=ALU.subtract), reads=[R + "mx1", R + "mx2"], writes=[R + "d12"])
            P.op(ACT, lambda e: e.activation(out=t12, in_=d12, func=AF.Tanh, scale=0.5), reads=[R + "d12"], writes=[R + "t12"])
            P.op(DVE, lambda e: e.tensor_scalar(out=pa, in0=pg, scalar1=0.25, scalar2=None, op0=ALU.mult), reads=[R + "pg"], writes=[R + "pa"])
            P.op(DVE, lambda e: e.scalar_tensor_tensor(out=gq1, in0=t12, scalar=1.0, in1=pa, op0=ALU.add, op1=ALU.mult), reads=[R + "t12", R + "pa"], writes=[R + "gq1"])
            P.op(DVE, lambda e: e.tensor_scalar(out=nt12, in0=t12, scalar1=-1.0, scalar2=1.0, op0=ALU.mult, op1=ALU.add), reads=[R + "t12"], writes=[R + "nt12"])
            P.op(DVE, lambda e: e.tensor_tensor(out=gq2, in0=nt12, in1=pa, op=ALU.mult), reads=[R + "nt12", R + "pa"], writes=[R + "gq2"])
            P.op(DVE, lambda e: e.tensor_tensor(out=ohA[:, i, :].rearrange("p (g j) -> p g j", g=4),
                                                in0=goh[:, :, None].broadcast_to([128, 4, 8]),
                                                in1=oh1[:, None, :].broadcast_to([128, 4, 8]), op=ALU.mult),
                 reads=[R + "goh", R + "oh1"], writes=["ohA"])
            P.op(DVE, lambda e: e.tensor_tensor(out=ohB[:, i, :].rearrange("p (g j) -> p g j", g=4),
                                                in0=goh[:, :, None].broadcast_to([128, 4, 8]),
                                                in1=oh2[:, None, :].broadcast_to([128, 4, 8]), op=ALU.mult),
                 reads=[R + "goh", R + "oh2"], writes=["ohB"])
            P.op(DVE, lambda e: e.tensor_copy(out=gAB[:, i:i + 1], in_=gq1), reads=[R + "gq1"], writes=["gAB"])
            P.op(DVE, lambda e: e.tensor_copy(out=gAB[:, NT + i:NT + i + 1], in_=gq2), reads=[R + "gq2"], writes=["gAB"])

        def stage_D(t):
            hTt = hT[t % 2]
            hT_names = ["hT%d_%d" % (t % 2, j) for j in range(SUB)]
            for j in range(SUB):
                i = t * SUB + j
                dma(SP, xres[i % 2], x[i * 128:(i + 1) * 128, :], "xr%d" % (i % 2), writes=["xr%d" % (i % 2)])
            for j in range(SUB):
                i = t * SUB + j
                pq, pqn = psum(512)
                for k in range(8):
                    P.op(PE, lambda e, k=k, pq=pq, j=j: e.matmul(pq, lhsT=hTt[:, k, j * 128:(j + 1) * 128], rhs=Win[:, k, Q0:Q0 + 512], start=(k == 0), stop=(k == 7)),
                         reads=["hT%d_%d" % (t % 2, j)] + WinD_n, writes=pqn, cost=260)
                qk_post(pq, pqn, 8, qkg[:, 0:512], i)
                pt, pn = psum_bf(1024)
                for h in range(8):
                    P.op(PE, lambda e, h=h, pt=pt: e.transpose(out=pt[0:64, h * 128:(h + 1) * 128], in_=qkb[:, h * 64:(h + 1) * 64], identity=identb),
                         reads=["qkb", "identb"], writes=pn, cost=100)
                qTi = qT[i % 2]
                P.op(ACT, lambda e, pt=pt, qTi=qTi: e.activation(out=qTi[0:64, :, :], in_=pt[0:64, :].rearrange("p (h n) -> p h n", h=8), func=AF.Copy),
                     reads=pn, writes=["qT%d" % (i % 2)])
                for g in range(2):
                    blocks = [b for b in (i - 1, i, i + 1) if 0 <= b < KNS * SUB]
                    pod, podn = psum(512)
                    pts = []
                    for bi, b in enumerate(blocks):
                        slot = b % KVR
                        psS, psn = psum(512)
                        P.op(PE, lambda e, psS=psS, slot=slot, g=g, qTi=qTi: e.matmul(psS, lhsT=kT[0:64, g, slot * 128:(slot + 1) * 128],
                                                                                     rhs=qTi[0:64, 4 * g:4 * g + 4, :], start=True, stop=True),
                             reads=["k%d" % slot, "qT%d" % (i % 2)], writes=psn, cost=260)
                        pidx = pstate.setdefault("pT", 0)
                        pstate["pT"] = (pidx + 1) % 4
                        pTi = pT[pidx]
                        P.op(ACT, lambda e, psS=psS, pTi=pTi: e.activation(out=pTi, in_=psS, func=AF.Exp, scale=0.125),
                             reads=psn, writes=["pT%d" % pidx], cost=600)
                        if b != i:
                            mk_ = maskPb if b == i - 1 else maskNb
                            P.op(POOL, lambda e, pTi=pTi, mk_=mk_: e.tensor_tensor(out=pTi.rearrange("p (h q) -> p h q", h=4),
                                                                                 in0=pTi.rearrange("p (h q) -> p h q", h=4),
                                                                                 in1=mk_[:, None, :].broadcast_to([128, 4, 128]), op=ALU.mult),
                                 reads=["pT%d" % pidx, "maskPb", "maskNb"], writes=["pT%d" % pidx])
                        P.op(PE, lambda e, pod=pod, slot=slot, g=g, pTi=pTi, bi=bi: e.matmul(pod, lhsT=vring[:, slot, g, :], rhs=pTi, start=(bi == 0), stop=False),
                             reads=["v%d" % slot, "vones", "pT%d" % pidx], writes=podn, cost=260)
                    P.op(PE, lambda e, pod=pod, g=g: e.matmul(pod, lhsT=onesD2[0:2, :], rhs=esrow[0:2, 4 * g:4 * g + 4, :], start=False, stop=True),
                         reads=["onesD2", "esrow"], writes=podn, cost=260)
                    P.op(DVE, lambda e, pod=pod: e.reciprocal(out=rD[0:64, :], in_=pod[64:128, :]), reads=podn, writes=["rD"], cost=3400)
                    o4 = pod[0:64, :].rearrange("p (a b q) -> p a b q", a=2, b=2)
                    r4 = rD[0:64, :].rearrange("p (a b q) -> p a b q", a=2, b=2)
                    P.op(DVE, lambda e, o4=o4, r4=r4, g=g, j=j: e.tensor_tensor(out=aT[0:64, 2 * g:2 * g + 2, j * 128:(j + 1) * 128], in0=o4[:, :, 0, :], in1=r4[:, :, 0, :], op=ALU.mult),
                         reads=podn + ["rD"], writes=["aT"])
                    P.op(DVE, lambda e, o4=o4, r4=r4, g=g, j=j: e.tensor_tensor(out=aT[64:128, 2 * g:2 * g + 2, j * 128:(j + 1) * 128], in0=o4[:, :, 1, :], in1=r4[:, :, 1, :], op=ALU.mult),
                         reads=podn + ["rD"], writes=["aT"])
            ut = uring[t % UR]
            un = ["u%d" % (t % UR), "uh0_%d" % (t % UR), "uh1_%d" % (t % UR)]
            for c in range(4):
                pc, pcn = psum(T)
                for k in range(8):
                    P.op(PE, lambda e, k=k, pc=pc, c=c: e.matmul(pc, lhsT=Win[:, k, CB0 + c * 128:CB0 + (c + 1) * 128], rhs=hTt[:, k, :], start=(k == 0), stop=(k == 7)),
                         reads=hT_names + WinD_n, writes=pcn, cost=160)
                P.op(ACT, lambda e, pc=pc, c=c: e.activation(out=cbT[:, c, :], in_=pc, func=AF.Copy), reads=pcn, writes=["cbT%d" % c])
                P.op(DVE, lambda e, c=c: e.tensor_scalar(out=cv1, in0=ut[:, c, 0:T], scalar1=cw[:, 3 * c:3 * c + 1], scalar2=None, op0=ALU.mult),
                     reads=un + ["cw"], writes=["cv1"])
                P.op(DVE, lambda e, c=c: e.scalar_tensor_tensor(out=cv2, in0=ut[:, c, 1:T + 1], scalar=cw[:, 3 * c + 1:3 * c + 2], in1=cv1, op0=ALU.mult, op1=ALU.add),
                     reads=un + ["cw", "cv1"], writes=["cv2"])
                P.op(DVE, lambda e, c=c: e.scalar_tensor_tensor(out=cv1, in0=ut[:, c, 2:T + 2], scalar=cw[:, 3 * c + 2:3 * c + 3], in1=cv2, op0=ALU.mult, op1=ALU.add),
                     reads=un + ["cw", "cv2"], writes=["cv1"])
                P.op(DVE, lambda e, c=c: e.scalar_tensor_tensor(out=cT[:, c, :], in0=cv1, scalar=cbias[:, c:c + 1], in1=cbT[:, c, :], op0=ALU.add, op1=ALU.mult),
                     reads=["cv1", "cbias", "cbT%d" % c], writes=["cT"])
            for m in range(8):
                pga, pgan = psum(T)
                for k in range(8):
                    P.op(PE, lambda e, k=k, pga=pga, m=m: e.matmul(pga, lhsT=Win[:, k, GA0 + m * 128:GA0 + (m + 1) * 128], rhs=hTt[:, k, :], start=(k == 0), stop=(k == 7)),
                         reads=hT_names + WinD_n, writes=pgan, cost=160)
                pgc, pgcn = psum(T)
                for k in range(8):
                    P.op(PE, lambda e, k=k, pgc=pgc, m=m: e.matmul(pgc, lhsT=Win[:, k, GC0 + m * 128:GC0 + (m + 1) * 128], rhs=hTt[:, k, :], start=(k == 0), stop=(k == 7)),
                         reads=hT_names + WinD_n, writes=pgcn, cost=160)
                pA, pAn = psum(T)
                for c in range(4):
                    P.op(PE, lambda e, c=c, pA=pA, m=m: e.matmul(pA, lhsT=Wa[:, c, m * 128:(m + 1) * 128], rhs=aT[:, c, :], start=(c == 0), stop=(c == 3)),
                         reads=["aT", "Wa"], writes=pAn, cost=160)
                pC, pCn = psum(T)
                for c in range(4):
                    P.op(PE, lambda e, c=c, pC=pC, m=m: e.matmul(pC, lhsT=Wc[:, c, m * 128:(m + 1) * 128], rhs=cT[:, c, :], start=(c == 0), stop=(c == 3)),
                         reads=["cT", "Wc"], writes=pCn, cost=160)
                ta_ = ta[m % 2]
                tc_ = tcg[m % 2]
                P.op(ACT, lambda e, pga=pga, ta_=ta_: e.activation(out=ta_, in_=pga, func=AF.Tanh, scale=0.5), reads=pgan, writes=["ta%d" % (m % 2)])
                P.op(ACT, lambda e, pgc=pgc, tc_=tc_: e.activation(out=tc_, in_=pgc, func=AF.Tanh, scale=0.5), reads=pgcn, writes=["tc%d" % (m % 2)])
                P.op(DVE, lambda e, pA=pA, ta_=ta_: e.scalar_tensor_tensor(out=m1, in0=ta_, scalar=1.0, in1=pA, op0=ALU.add, op1=ALU.mult),
                     reads=pAn + ["ta%d" % (m % 2)], writes=["m1"])
                P.op(DVE, lambda e, pC=pC, tc_=tc_: e.scalar_tensor_tensor(out=m2, in0=tc_, scalar=1.0, in1=pC, op0=ALU.add, op1=ALU.mult),
                     reads=pCn + ["tc%d" % (m % 2)], writes=["m2"])
                P.op(POOL, lambda e, m=m: e.tensor_tensor(out=mergedT[:, m, :], in0=m1, in1=m2, op=ALU.add),
                     reads=["m1", "m2"], writes=["mergedT"])
            for j in range(SUB):
                i = t * SUB + j
                r = i % 2
                xr = xres[r]
                for half in range(2):
                    po, pon = psum(512)
                    for k in range(8):
                        P.op(PE, lambda e, k=k, po=po, j=j, half=half: e.matmul(po, lhsT=mergedT[:, k, j * 128:(j + 1) * 128], rhs=Wout[:, k, half * 512:(half + 1) * 512], start=(k == 0), stop=(k == 7)),
                             reads=["mergedT", "Wout"], writes=pon, cost=260)
                    P.op(DVE, lambda e, po=po, xr=xr, half=half: e.scalar_tensor_tensor(out=xr[:, half * 512:(half + 1) * 512], in0=po, scalar=0.5, in1=xr[:, half * 512:(half + 1) * 512], op0=ALU.mult, op1=ALU.add),
                         reads=pon + ["xr%d" % r], writes=["xr%d" % r])
                dma(SP, out[i * 128:(i + 1) * 128, :], xr, "st_x2_%d" % r, reads=["xr%d" % r])
                if debug_phase1:
                    continue
                P.op(ACT, lambda e, xr=xr, r=r: e.activation(out=h2b, in_=xr, func=AF.Square, accum_out=ss2[r]),
                     reads=["xr%d" % r], writes=["h2b", "ss2_%d" % r], cost=1200)
                rms_rstd(ss2[r], rs2[r], D, "2_%d" % r)
                P.op(DVE, lambda e, xr=xr, r=r: e.scalar_tensor_tensor(out=h2b, in0=xr, scalar=rs2[r], in1=g2b, op0=ALU.mult, op1=ALU.mult),
                     reads=["xr%d" % r, "rs2_%d" % r, "g2b"], writes=["h2b"], cost=1200)
                pt, pn = psum_bf(1024)
                for k in range(8):
                    P.op(PE, lambda e, k=k, pt=pt: e.transpose(out=pt[:, k * 128:(k + 1) * 128], in_=h2b[:, k * 128:(k + 1) * 128], identity=identb),
                         reads=["h2b", "identb"], writes=pn, cost=100)
                h2Ti = h2T[r]
                P.op(ACT, lambda e, pt=pt, h2Ti=h2Ti: e.activation(out=h2Ti, in_=pt.rearrange("p (k n) -> p k n", k=8), func=AF.Copy),
                     reads=pn, writes=["h2T%d" % r], cost=1200)
                dma(SP, h2_d[i * 128:(i + 1) * 128, :], h2b, "st_h2", reads=["h2b"])
                router(i, h2Ti)

        for s in range(KNS + 1):
            if s < KNS:
                if KLEVEL >= 1:
                    stage_B(s)
                if KLEVEL >= 2:
                    stage_C(s)
            if s >= 1 and KLEVEL >= 3:
                stage_D(s - 1)

        if not debug_phase1:
            I32 = mybir.dt.int32
            M = Arena(arena_t, ARENA)
            M.off = persist_end
            h2flat = M.alloc(NT * D)
            h2sb = h2flat.rearrange("p (i d) -> p i d", i=NT)
            CR = 4
            yA = [h2flat[:, (3 * r_) * 2048:(3 * r_ + 1) * 2048].bitcast(F32) for r_ in range(CR)]
            yB = [h2flat[:, (3 * r_ + 1) * 2048:(3 * r_ + 2) * 2048].bitcast(F32) for r_ in range(CR)]
            x2t = [h2flat[:, (3 * r_ + 2) * 2048:(3 * r_ + 3) * 2048].bitcast(F32) for r_ in range(CR)]
            Msel = M.alloc(NT * NE).rearrange("p (i e) -> p i e", i=NT)
            Mcum = M.alloc((NT + 1) * NE).rearrange("p (i e) -> p i e", i=NT + 1)
            rank = M.alloc(NT * NE, F32).rearrange("p (i e) -> p i e", i=NT)
            pos = M.alloc(NT * NE, F32).rearrange("p (i e) -> p i e", i=NT)
            tmpA = M.alloc(NT * NE, F32).rearrange("p (i e) -> p i e", i=NT)
            cmoe = M.alloc(472, F32)
            identm = M.alloc(128)
            Lst = M.alloc(128)
            Ones = M.alloc(128)
            cnt = M.alloc(NE, F32)
            cmpT = M.alloc(NE * 16, F32).rearrange("p (e k) -> p e k", e=NE)
            nblk = M.alloc(NE, F32)
            pc = M.alloc(NE, F32)
            sc0 = M.alloc(NE, F32)
            sc1 = M.alloc(NE, F32)
            pst = M.alloc(NE, F32)
            cmpB = M.alloc(NB * NE, F32).rearrange("p (b e) -> p b e", b=NB)
            be_f = M.alloc(NB, F32)
            be_i = M.alloc(NB, F32).bitcast(I32)
            iw_f = M.alloc(NB, F32)
            dA_f = M.alloc(NT, F32)
            dB_f = M.alloc(NT, F32)
            dA_i = M.alloc(NT, F32).bitcast(I32)
            dB_i = M.alloc(NT, F32).bitcast(I32)
            NW = 3
            Wg = [M.alloc(8 * 256).rearrange("p (k n) -> p k n", k=8) for _ in range(NW)]
            Wu = [M.alloc(8 * 256).rearrange("p (k n) -> p k n", k=8) for _ in range(NW)]
            Wd = [M.alloc(2 * D).rearrange("p (k n) -> p k n", k=2) for _ in range(NW)]
            xb = [M.alloc(SB * D).rearrange("p (s d) -> p s d", s=SB) for _ in range(2)]
            XT = [M.alloc(8 * RB).rearrange("p (k n) -> p k n", k=8) for _ in range(2)]
            tg = [M.alloc(RB) for _ in range(2)]
            sg = [M.alloc(RB, F32) for _ in range(2)]
            hid = [M.alloc(2 * RB).rearrange("p (f n) -> p f n", f=2) for _ in range(2)]
            yb = [M.alloc(SB * D, F32).rearrange("p (s d) -> p s d", s=SB) for _ in range(2)]
            bar = M.alloc(16, F32)
            print("phase2 arena bytes", M.off * 2)
            PH = ["PH2"]
            P.barrier(DVE, lambda e: e.memset(bar, 0.0), extra=PH)

            dma(SP, cmoe, cmoe_d, "c_cmoe", reads=PH, writes=["cmoe"])
            for q4 in range(4):
                dma(SP, h2sb[:, q4 * 8:(q4 + 1) * 8, :], h2_d[q4 * 1024:(q4 + 1) * 1024, :].rearrange("(i p) d -> p i d", p=128),
                    "h2sb%d" % q4, reads=PH, writes=["h2sb%d" % q4], lat=12000.0)
            THk = cmoe[:, 0:16]
            BR_ = cmoe[:, 16:16 + NB]
            P.op(DVE, lambda e: e.tensor_copy(out=Lst, in_=cmoe[:, 80:208]), reads=PH + ["cmoe"], writes=["Lst"])
            P.op(DVE, lambda e: e.tensor_copy(out=Ones, in_=cmoe[:, 208:336]), reads=PH + ["cmoe"], writes=["Ones"])
            P.op(DVE, lambda e: e.tensor_copy(out=identm, in_=cmoe[:, 336:464]), reads=PH + ["cmoe"], writes=["identm"])
            P.op(DVE, lambda e: e.tensor_tensor(out=Msel, in0=ohA, in1=ohB, op=ALU.add), reads=PH + ["ohA", "ohB"], writes=["Msel"], cost=1100)
            P.op(DVE, lambda e: e.memset(Mcum[:, 0, :], 0.0), reads=PH, writes=["Mcum0"])
            for i in range(1, NT + 1):
                P.op(DVE, lambda e, i=i: e.tensor_tensor(out=Mcum[:, i, :], in0=Mcum[:, i - 1, :], in1=Msel[:, i - 1, :], op=ALU.add),
                     reads=PH + ["Mcum%d" % (i - 1), "Msel"], writes=["Mcum%d" % i], cost=150)
            for half in range(2):
                pr_, prn_ = psum(512)
                for ii in range(16):
                    i = half * 16 + ii
                    P.op(PE, lambda e, pr_=pr_, ii=ii, i=i: e.matmul(pr_[:, ii * 32:(ii + 1) * 32], lhsT=Ones, rhs=Mcum[:, i, :], start=True, stop=False),
                         reads=PH + ["Ones", "Mcum%d" % i], writes=prn_, cost=70)
                    P.op(PE, lambda e, pr_=pr_, ii=ii, i=i: e.matmul(pr_[:, ii * 32:(ii + 1) * 32], lhsT=Lst, rhs=Msel[:, i, :], start=False, stop=True),
                         reads=PH + ["Lst", "Msel"], writes=prn_, cost=70)
                P.op(ACT, lambda e, pr_=pr_, half=half: e.activation(out=rank[:, half * 16:(half + 1) * 16, :], in_=pr_.rearrange("p (i e) -> p i e", i=16), func=AF.Copy),
                     reads=PH + prn_, writes=["rank%d" % half])
            pcn_, pcnn = psum(32)
            P.op(PE, lambda e: e.matmul(pcn_, lhsT=Ones, rhs=Mcum[:, NT, :], start=True, stop=True), reads=PH + ["Ones", "Mcum%d" % NT], writes=pcnn, cost=70)
            P.op(ACT, lambda e: e.activation(out=cnt, in_=pcn_, func=AF.Copy), reads=PH + pcnn, writes=["cnt"])
            P.op(DVE, lambda e: e.tensor_tensor(out=cmpT, in0=cnt[:, :, None].broadcast_to([128, NE, 16]), in1=THk[:, None, :].broadcast_to([128, NE, 16]), op=ALU.is_gt),
                 reads=PH + ["cnt", "cmoe"], writes=["cmpT"])
            P.op(DVE, lambda e: e.tensor_reduce(out=nblk, in_=cmpT, axis=AX.X, op=ALU.add), reads=PH + ["cmpT"], writes=["nblk"])
            P.op(DVE, lambda e: e.tensor_scalar(out=pc, in0=nblk, scalar1=float(RB), scalar2=None, op0=ALU.mult), reads=PH + ["nblk"], writes=["pc"])
            bufs = [(sc0, "sc0"), (sc1, "sc1")]
            src, srcn = pc, "pc"
            bi = 0
            for dd in (1, 2, 4, 8, 16):
                dst, dstn = bufs[bi]
                P.op(DVE, lambda e, src=src, dst=dst, dd=dd: e.tensor_tensor(out=dst[:, dd:NE], in0=src[:, dd:NE], in1=src[:, 0:NE - dd], op=ALU.add),
                     reads=PH + [srcn, srcn + "h"], writes=[dstn])
                P.op(DVE, lambda e, src=src, dst=dst, dd=dd: e.tensor_copy(out=dst[:, 0:dd], in_=src[:, 0:dd]),
                     reads=PH + [srcn, srcn + "h"], writes=[dstn + "h"])
                src, srcn = dst, dstn
                bi ^= 1
            pendn = [srcn, srcn + "h"]
            pend = src
            P.op(DVE, lambda e: e.tensor_tensor(out=pst, in0=pend, in1=pc, op=ALU.subtract), reads=PH + pendn + ["pc"], writes=["pst"])
            P.op(DVE, lambda e: e.tensor_tensor(out=pos, in0=rank, in1=pst[:, None, :].broadcast_to([128, NT, NE]), op=ALU.add),
                 reads=PH + ["rank0", "rank1", "pst"], writes=["pos"], cost=1200)
            for (oh_, df_, di_, nm) in ((ohA, dA_f, dA_i, "A"), (ohB, dB_f, dB_i, "B")):
                P.op(DVE, lambda e, oh_=oh_: e.tensor_tensor(out=tmpA, in0=oh_, in1=pos, op=ALU.mult), reads=PH + ["ohA", "ohB", "pos"], writes=["tmpA"], cost=1200)
                P.op(DVE, lambda e, df_=df_: e.tensor_reduce(out=df_, in_=tmpA, axis=AX.X, op=ALU.add), reads=PH + ["tmpA"], writes=["d%s_f" % nm], cost=1200)
                P.op(DVE, lambda e, df_=df_, di_=di_: e.tensor_copy(out=di_, in_=df_), reads=PH + ["d%s_f" % nm], writes=["d%s_i" % nm])
            P.op(DVE, lambda e: e.tensor_tensor(out=cmpB, in0=pend[:, None, :].broadcast_to([128, NB, NE]), in1=BR_[:, :, None].broadcast_to([128, NB, NE]), op=ALU.is_le),
                 reads=PH + pendn + ["cmoe"], writes=["cmpB"], cost=2200)
            P.op(DVE, lambda e: e.tensor_reduce(out=be_f, in_=cmpB, axis=AX.X, op=ALU.add), reads=PH + ["cmpB"], writes=["be_f"], cost=2200)
            P.op(DVE, lambda e: e.tensor_scalar(out=be_f, in0=be_f, scalar1=float(NE - 1), scalar2=None, op0=ALU.min), reads=PH + ["be_f"], writes=["be_f"])
            P.op(DVE, lambda e: e.tensor_scalar(out=iw_f, in0=be_f, scalar1=128.0, scalar2=cmoe[:, 464:465], op0=ALU.mult, op1=ALU.add), reads=PH + ["be_f", "cmoe"], writes=["iw_f"])
            P.op(DVE, lambda e: e.tensor_copy(out=be_i, in_=iw_f), reads=PH + ["iw_f"], writes=["be_i"])
            zn = ["xs_z%d" % b_ for b_ in range(NB)]
            for i in range(NT):
                for (di_, nm) in ((dA_i, "A"), (dB_i, "B")):
                    P.op(POOL, lambda e, i=i, di_=di_: e.indirect_dma_start(out=xs_d[:, :], out_offset=bass.IndirectOffsetOnAxis(ap=di_[:, i:i + 1], axis=0),
                                                                           in_=h2sb[:, i, :], in_offset=None),
                         reads=PH + ["h2sb%d" % (i // 8), "d%s_i" % nm] + zn,
                         writes=["xs_w%d%s" % (i, nm)], dma="scat", lat=6000.0)
            xs_names = ["xs_w%d%s" % (i, nm) for i in range(NT) for nm in "AB"]
            for b_ in range(NB):
                w = b_ % NW
                r2 = b_ % 2

                for (dst, src_t, nm) in ((Wg[w], wgb_d, "g"), (Wu[w], wub_d, "u"), (Wd[w], wdb_d, "d")):
                    P.op(POOL, lambda e, dst=dst, src_t=src_t, b_=b_: e.indirect_dma_start(out=dst.rearrange("p k n -> p (k n)"), out_offset=None, in_=src_t[:, :],
                                                                                      in_offset=bass.IndirectOffsetOnAxis(ap=be_i[:, b_:b_ + 1], axis=0)),
                         reads=PH + ["be_i"], writes=["W%s%d" % (nm, w)], dma="w%s%d" % (nm, w), lat=9000.0, cost=1500)
                dma(SP, xb[r2], xs_d[b_ * RB:(b_ + 1) * RB, :].rearrange("(s p) d -> p s d", p=128), "xb%d" % r2,
                    reads=PH + xs_names, writes=["xb%d" % r2], lat=5000.0)
                for sb_ in range(SB):
                    pt, pn = psum_bf(1024)
                    for k in range(8):
                        P.op(PE, lambda e, k=k, pt=pt, sb_=sb_, r2=r2: e.transpose(out=pt[:, k * 128:(k + 1) * 128], in_=xb[r2][:, sb_, k * 128:(k + 1) * 128], identity=identm),
                             reads=PH + ["xb%d" % r2, "identm"], writes=pn, cost=100)
                    P.op(ACT, lambda e, pt=pt, sb_=sb_, r2=r2: e.activation(out=XT[r2][:, :, sb_ * 128:(sb_ + 1) * 128], in_=pt.rearrange("p (k n) -> p k n", k=8), func=AF.Copy),
                         reads=PH + pn, writes=["XT%d_%d" % (r2, sb_)], cost=1200)
                xtn = ["XT%d_%d" % (r2, sb_) for sb_ in range(SB)]
                for f in range(2):
                    pg_, pgn = psum(RB)
                    for k in range(8):
                        P.op(PE, lambda e, k=k, pg_=pg_, f=f, w=w, r2=r2: e.matmul(pg_, lhsT=Wg[w][:, k, f * 128:(f + 1) * 128], rhs=XT[r2][:, k, :], start=(k == 0), stop=(k == 7)),
                             reads=PH + ["Wg%d" % w] + xtn, writes=pgn, cost=160)
                    pu_, pun = psum(RB)
                    for k in range(8):
                        P.op(PE, lambda e, k=k, pu_=pu_, f=f, w=w, r2=r2: e.matmul(pu_, lhsT=Wu[w][:, k, f * 128:(f + 1) * 128], rhs=XT[r2][:, k, :], start=(k == 0), stop=(k == 7)),
                             reads=PH + ["Wu%d" % w] + xtn, writes=pun, cost=160)
                    tg_ = tg[f]
                    sg_ = sg[f]
                    P.op(ACT, lambda e, pg_=pg_, tg_=tg_: e.activation(out=tg_, in_=pg_, func=AF.Tanh, scale=0.5), reads=PH + pgn, writes=["tg%d" % f], cost=500)
                    P.op(DVE, lambda e, pg_=pg_, tg_=tg_, sg_=sg_: e.scalar_tensor_tensor(out=sg_, in0=tg_, scalar=1.0, in1=pg_, op0=ALU.add, op1=ALU.mult),
                         reads=PH + pgn + ["tg%d" % f], writes=["sg%d" % f], cost=400)
                    P.op(DVE, lambda e, pu_=pu_, sg_=sg_, f=f, r2=r2: e.tensor_tensor(out=hid[r2][:, f, :], in0=pu_, in1=sg_, op=ALU.mult),
                         reads=PH + pun + ["sg%d" % f], writes=["hid%d_%d" % (r2, f)], cost=400)
                for sb_ in range(SB):
                    for half in range(2):
                        py, pyn = psum(512)
                        for f in range(2):
                            P.op(PE, lambda e, f=f, py=py, sb_=sb_, half=half, w=w, r2=r2: e.matmul(py, lhsT=hid[r2][:, f, sb_ * 128:(sb_ + 1) * 128], rhs=Wd[w][:, f, half * 512:(half + 1) * 512], start=(f == 0), stop=(f == 1)),
                                 reads=PH + ["hid%d_0" % r2, "hid%d_1" % r2, "Wd%d" % w], writes=pyn, cost=260)
                        if (sb_ + half) % 2 == 0:
                            P.op(ACT, lambda e, py=py, sb_=sb_, half=half, r2=r2: e.activation(out=yb[r2][:, sb_, half * 512:(half + 1) * 512], in_=py, func=AF.Copy),
                                 reads=PH + pyn, writes=["yb%d_%d%d" % (r2, sb_, half)], cost=750)
                        else:
                            P.op(DVE, lambda e, py=py, sb_=sb_, half=half, r2=r2: e.tensor_copy(out=yb[r2][:, sb_, half * 512:(half + 1) * 512], in_=py),
                                 reads=PH + pyn, writes=["yb%d_%d%d" % (r2, sb_, half)], cost=650)
                dma(SP, ys_d[b_ * RB:(b_ + 1) * RB, :].rearrange("(s p) d -> p s d", p=128), yb[r2], "st_y%d" % r2,
                    reads=PH + ["yb%d_%d%d" % (r2, sb_, half) for sb_ in range(SB) for half in range(2)], writes=["ys_w%d" % b_], lat=5000.0)
            ys_names = ["ys_w%d" % b_ for b_ in range(NB)]
            for i in range(NT):
                r2 = i % CR
                dma(SP, x2t[r2], out[i * 128:(i + 1) * 128, :], "x2t%d" % r2, reads=PH + xs_names, writes=["x2t%d" % r2])
                for (ybuf, di_, nm) in ((yA[r2], dA_i, "A"), (yB[r2], dB_i, "B")):
                    P.op(POOL, lambda e, i=i, di_=di_, ybuf=ybuf: e.indirect_dma_start(out=ybuf, out_offset=None, in_=ys_d[:, :],
                                                                                    in_offset=bass.IndirectOffsetOnAxis(ap=di_[:, i:i + 1], axis=0)),
                         reads=PH + ["d%s_i" % nm] + ys_names, writes=["y%s%d" % (nm, r2)], dma="gy%s%d" % (nm, r2), lat=6000.0)
                P.op(DVE, lambda e, i=i, r2=r2: e.scalar_tensor_tensor(out=x2t[r2], in0=yA[r2], scalar=gAB[:, i:i + 1], in1=x2t[r2], op0=ALU.mult, op1=ALU.add),
                     reads=PH + ["yA%d" % r2, "gAB", "x2t%d" % r2], writes=["x2t%d" % r2], cost=1200)
                P.op(DVE, lambda e, i=i, r2=r2: e.scalar_tensor_tensor(out=x2t[r2], in0=yB[r2], scalar=gAB[:, NT + i:NT + i + 1], in1=x2t[r2], op0=ALU.mult, op1=ALU.add),
                     reads=PH + ["yB%d" % r2, "gAB", "x2t%d" % r2], writes=["x2t%d" % r2], cost=1200)
                dma(SP, out[i * 128:(i + 1) * 128, :], x2t[r2], "st_o%d" % r2, reads=PH + ["x2t%d" % r2])

        nsem = P.emit(nc)
        print("ops", len(P.ops), "sems", nsem, "model_us", getattr(P, "model_time", 0) / 1e3)
    return nc


_CACHE = {}


def _consts():
    ang_f = (500000.0 ** (-np.arange(0, 16, 2, dtype=np.float32) / 16.0)).astype(np.float32)
    ang = np.arange(S, dtype=np.float32)[:, None] * ang_f[None, :]
    c, s_ = np.cos(ang).astype(np.float32), np.sin(ang).astype(np.float32)
    ropeC = np.concatenate([c, c], axis=1).astype(np.float32)
    ropeS = np.concatenate([-s_, s_], axis=1).astype(np.float32)
    cmat = np.zeros((128, 512), np.float32)
    cmat[:, 0:128] = np.eye(128, dtype=np.float32)
    kk = np.arange(128)[:, None]
    qq = np.arange(128)[None, :]
    cmat[:, 128:256] = (qq <= kk).astype(np.float32)
    cmat[:, 256:384] = (kk <= qq).astype(np.float32)
    cmat[0:2, 384:448] = 0.0
    cmat[0:2, 448:512] = 1.0
    return ropeC, ropeS, cmat


def kernel(x, norm1_g, w_in, q_norm_g, k_norm_g, attn_sink, conv_w, conv_b,
           w_attn_proj, w_conv_proj, w_out, norm2_g, w_router_group, b_router_group,
           w_router_expert, b_router_expert, w_gate_e, w_up_e, w_down_e, _debug_phase1=False):
    f = lambda a: np.ascontiguousarray(np.asarray(a, dtype=np.float32))
    x = f(x)
    n = x.shape[0]
    key = bool(_debug_phase1)
    if key not in _CACHE:
        _CACHE[key] = build_nc(debug_phase1=key)
    nc = _CACHE[key]
    ropeC, ropeS, cmat = _consts()
    sel = np.zeros((128, 16), np.float32)
    shared = dict(
        w_in=f(w_in[0]), w_a=f(w_attn_proj[0]), w_c=f(w_conv_proj[0]), w_out=f(w_out[0]),
        w_r=f(np.concatenate([np.asarray(w_router_group[0]), np.asarray(w_router_expert[0])], axis=1).reshape(8, 128, 36).transpose(1, 0, 2).reshape(128, 288)),
        wg_l=f(np.asarray(w_gate_e[0]).reshape(NE, 8, 128, 256).transpose(0, 2, 1, 3).reshape(NE * 128, 2048)),
        wu_l=f(np.asarray(w_up_e[0]).reshape(NE, 8, 128, 256).transpose(0, 2, 1, 3).reshape(NE * 128, 2048)),
        wd_l=f(np.asarray(w_down_e[0]).reshape(NE, 2, 128, D).transpose(0, 2, 1, 3).reshape(NE * 128, 2048)),
        g1b=f(np.broadcast_to(np.asarray(norm1_g[0])[None, :], (128, D))),
        g2b=f(np.broadcast_to(np.asarray(norm2_g[0])[None, :], (128, D))),
        qkg=f(np.broadcast_to(np.concatenate([np.tile(np.asarray(q_norm_g[0]), 8), np.tile(np.asarray(k_norm_g[0]), 2)])[None, :], (128, 640))),
        cwl=f(np.asarray(conv_w[0]).reshape(3, 4, 128).transpose(2, 1, 0).reshape(128, 12)),
        cbl=f(np.asarray(conv_b[0]).reshape(4, 128).transpose(1, 0)),
        sinkb=f(np.broadcast_to(np.asarray(attn_sink[0])[None, :], (2, 8))),
        brb=f(np.broadcast_to(np.concatenate([np.asarray(b_router_group[0]), np.asarray(b_router_expert[0])])[None, :], (128, 36))),
        ropeC=f(ropeC.reshape(NT, 128, 16).transpose(1, 0, 2).reshape(128, NT * 16)),
        ropeS=f(ropeS.reshape(NT, 128, 16).transpose(1, 0, 2).reshape(128, NT * 16)), cmat=cmat,
    )
    cmoe = np.zeros((128, 472), np.float32)
    cmoe[:, 464] = np.arange(128, dtype=np.float32)
    cmoe[:, 0:16] = (np.arange(16, dtype=np.float32) * RB)[None, :]
    cmoe[:, 16:16 + NB] = (np.arange(NB, dtype=np.float32) * RB)[None, :]
    tt = np.arange(128)
    cmoe[:, 80:208] = (tt[:, None] < tt[None, :]).astype(np.float32)
    cmoe[:, 208:336] = 1.0
    cmoe[:, 336:464] = np.eye(128, dtype=np.float32)
    shared["cmoe"] = cmoe
    shared["zrows"] = np.zeros((RB, D // 2), np.float32)
    in_maps = [dict(shared, x=x[b]) for b in range(n)]
    res = run_bass_kernel_spmd(nc, in_maps, core_ids=list(range(n)))
    return np.stack([np.asarray(r["out"]) for r in res.results], axis=0).astype(np.float32)

CAL.update({989: 298, 225: 43, 363: 613, 398: 927, 405: 746, 476: 520, 422: 200, 424: 499, 479: 1246, 483: 84, 486: 1104, 496: 192, 511: 128, 498: 303, 430: 382, 431: 461, 433: 247, 435: 1159, 438: 474, 515: 115, 441: 469, 449: 352, 451: 278, 453: 237, 518: 421, 519: 383, 444: 532, 455: 368, 503: 154, 505: 408, 524: 164, 604: 338, 526: 171, 655: 309, 657: 494, 659: 512, 609: 134, 612: 1109, 621: 453, 627: 532, 635: 419, 631: 1134, 637: 459, 639: 3359, 642: 417, 644: 418, 652: 160, 667: 118, 654: 472, 671: 109, 683: 384, 684: 302, 661: 438, 675: 114, 679: 114, 685: 420, 687: 336, 689: 729, 699: 289, 701: 692, 706: 574, 709: 1238, 713: 101, 716: 1111, 536: 43, 561: 179, 562: 110, 563: 232, 564: 137, 567: 189, 565: 98, 570: 121, 566: 118, 571: 152, 578: 138, 572: 227, 573: 151, 582: 102, 574: 161, 575: 184, 576: 60, 586: 207, 577: 98, 579: 89, 580: 64, 590: 154, 581: 151, 591: 113, 801: 172, 808: 87, 810: 27, 812: 678, 827: 186, 829: 78, 840: 1130, 841: 1216, 842: 180, 854: 1110, 865: 1088, 873: 70, 875: 1059, 881: 115, 885: 109, 889: 327, 890: 380, 892: 334, 898: 286, 901: 644, 904: 649, 168: 3057, 914: 1256, 917: 1281, 919: 1199})
```

```python
import contextlib
import numpy as np
import concourse.bass as bass
import concourse.mybir as mybir
from concourse.bass_utils import run_bass_kernel_spmd

F32 = mybir.dt.float32
BF16 = mybir.dt.bfloat16
AF = mybir.ActivationFunctionType
ALU = mybir.AluOpType
AX = mybir.AxisListType

PE, ACT, DVE, POOL, SP = "pe", "act", "dve", "pool", "sp"
CAL = {}
USE_CAL = False

D = 1024
S = 4096
NT = S // 128
T = 256
SUB = T // 128
NS = S // T
PW = 4352
Q0, K0, V0, CB0, CC0, CX0, GA0, GC0 = 0, 512, 640, 768, 1280, 1792, 2304, 3328
NE = 32
EPS = 1e-6
KVR = 8
UR = 3
RB = 256
SB = RB // 128
NB = (2 * S) // RB + NE
NROW = NB * RB


class Prog:
    def __init__(self):
        self.ops = []
        self.res = {}

    DEF_COST = {PE: 200.0, ACT: 700.0, DVE: 450.0, POOL: 1200.0, SP: 100.0}

    def op(self, eng, fn, reads=(), writes=(), dma=None, cost=None, lat=None):
        if cost is None:
            cost = 100.0 if dma is not None else self.DEF_COST[eng]
        if lat is None:
            lat = 3500.0 if dma is not None else 0.0
        o = dict(id=len(self.ops), eng=eng, fn=fn, dma=dma, deps=set(), sig=None, used=False, cost=cost, lat=lat)
        try:
            cal = CAL.get(fn.__code__.co_firstlineno)
            if cal is not None and dma is None and USE_CAL:
                o["cost"] = float(cal)
        except Exception:
            pass
        for r in reads:
            st = self.res.setdefault(r, [None, []])
            if st[0] is not None:
                o["deps"].add(st[0]["id"])
        for w in writes:
            st = self.res.setdefault(w, [None, []])
            if st[0] is not None:
                o["deps"].add(st[0]["id"])
            for rd in st[1]:
                o["deps"].add(rd["id"])
        for r in reads:
            self.res[r][1].append(o)
        for w in writes:
            self.res[w] = [o, []]
        o["deps"].discard(o["id"])
        self.ops.append(o)
        return o

    def barrier(self, eng, fn, extra=()):
        last = {}
        for o in self.ops:
            key = ("dma", o["dma"]) if o["dma"] is not None else ("eng", o["eng"])
            last[key] = o["id"]
        o = dict(id=len(self.ops), eng=eng, fn=fn, dma=None, deps=set(last.values()), sig=None, used=False, cost=100.0, lat=0.0)
        for r in list(self.res.keys()) + list(extra):
            self.res[r] = [o, []]
        self.ops.append(o)
        return o

    def schedule(self):
        import heapq
        ops = self.ops
        n = len(ops)
        ndeps = [len(o["deps"]) for o in ops]
        users = [[] for _ in range(n)]
        for o in ops:
            for d in o["deps"]:
                users[d].append(o["id"])
        ready = [0.0] * n
        fin = [0.0] * n
        heaps = {PE: [], ACT: [], DVE: [], POOL: [], SP: []}
        free = {PE: 0.0, ACT: 0.0, DVE: 0.0, POOL: 0.0, SP: 0.0}
        for o in ops:
            if ndeps[o["id"]] == 0:
                heapq.heappush(heaps[o["eng"]], (0.0, o["id"]))
        order = []
        SEM = 150.0
        while len(order) < n:
            best = None
            for eng, h in heaps.items():
                if not h:
                    continue
                t = max(free[eng], h[0][0])
                if best is None or t < best[0]:
                    best = (t, eng)
            t, eng = best
            h = heaps[eng]
            cand = []
            while h and h[0][0] <= t:
                cand.append(heapq.heappop(h))
            pick = min(cand, key=lambda c: c[1])
            for c in cand:
                if c is not pick:
                    heapq.heappush(h, c)
            i = pick[1]
            o = ops[i]
            start = max(free[eng], ready[i])
            free[eng] = start + o["cost"]
            fin[i] = start + o["cost"] + o["lat"]
            order.append(i)
            for u in users[i]:
                ready[u] = max(ready[u], fin[i] + SEM)
                ndeps[u] -= 1
                if ndeps[u] == 0:
                    heapq.heappush(heaps[ops[u]["eng"]], (ready[u], u))
        self.model_time = max(fin)
        return order

    def emit(self, nc, reorder=True):
        ops = self.ops
        order = self.schedule() if reorder else list(range(len(ops)))
        ops_sched = [ops[i] for i in order]
        for o in ops:
            if o["eng"] == PE and o["dma"] is None:
                o["deps"] = {d for d in o["deps"]
                             if not (ops[d]["eng"] == PE and ops[d]["dma"] is None)}
            for d in o["deps"]:
                ops[d]["used"] = True
        semkeys = {}
        counts = {}
        for o in ops_sched:
            key = ("dma", o["dma"]) if o["dma"] is not None else ("eng", o["eng"])
            if o["dma"] is not None or o["used"]:
                inc = 16 if o["dma"] is not None else 1
                counts[key] = counts.get(key, 0) + inc
                o["sig"] = (key, counts[key], inc)
                semkeys[key] = None
        with contextlib.ExitStack() as es:
            for i, key in enumerate(semkeys):
                semkeys[key] = es.enter_context(nc.semaphore("s%d" % i))
            block = es.enter_context(nc.Block())
            per_eng = {PE: [], ACT: [], DVE: [], POOL: [], SP: []}
            for o in ops_sched:
                per_eng[o["eng"]].append(o)

            def mk(eng_name, eng_ops):
                def body(e):
                    known = {}
                    for o in eng_ops:
                        need = {}
                        for d in o["deps"]:
                            key, val, _ = ops[d]["sig"]
                            if need.get(key, 0) < val:
                                need[key] = val
                        for key, val in need.items():
                            if known.get(key, 0) < val:
                                e.wait_ge(semkeys[key], val)
                                known[key] = val
                        ins = o["fn"](e)
                        if o["sig"] is not None:
                            key, val, inc = o["sig"]
                            ins.then_inc(semkeys[key], inc)
                    if eng_name == SP:
                        for key, val in counts.items():
                            if known.get(key, 0) < val:
                                e.wait_ge(semkeys[key], val)
                return body

            reg = {PE: block.tensor, ACT: block.scalar, DVE: block.vector,
                   POOL: block.gpsimd, SP: block.sync}
            for eng_name, eng_ops in per_eng.items():
                reg[eng_name](mk(eng_name, eng_ops))
        return len(semkeys)


class Arena:
    def __init__(self, ap, size):
        self.ap = ap
        self.size = size
        self.off = 0

    def alloc(self, cols, dt=BF16):
        if dt == F32:
            self.off += self.off & 1
            a = self.ap[:, self.off:self.off + 2 * cols].bitcast(F32)
            self.off += 2 * cols
        else:
            a = self.ap[:, self.off:self.off + cols]
            self.off += cols
        assert self.off <= self.size, ("arena overflow", self.off, self.size)
        return a


def build_nc(debug_phase1=False):
    nc = bass.Bass("TRN2", target_bir_lowering=False)

    def din(name, shape, dt=F32):
        return nc.dram_tensor(name, list(shape), dt, kind="ExternalInput").ap()

    x = din("x", [S, D])
    w_in = din("w_in", [D, PW])
    w_a = din("w_a", [512, D])
    w_c = din("w_c", [512, D])
    w_out = din("w_out", [D, D])
    w_r = din("w_r", [128, 8 * 36])
    wg_l = din("wg_l", [NE * 128, 2048])
    wu_l = din("wu_l", [NE * 128, 2048])
    wd_l = din("wd_l", [NE * 128, 2048])
    g1b_d = din("g1b", [128, D])
    g2b_d = din("g2b", [128, D])
    qkg_d = din("qkg", [128, 640])
    cwl_d = din("cwl", [128, 12])
    cbl_d = din("cbl", [128, 4])
    sinkb_d = din("sinkb", [2, 8])
    brb_d = din("brb", [128, 36])
    ropeC_d = din("ropeC", [128, NT * 16])
    ropeS_d = din("ropeS", [128, NT * 16])
    cmat_d = din("cmat", [128, 512])
    cmoe_d = din("cmoe", [128, 472])
    zrows_d = din("zrows", [RB, D // 2])
    out = nc.dram_tensor("out", [S, D], F32, kind="ExternalOutput").ap()
    h2_d = nc.dram_tensor("h2_scr", [S, D], BF16).ap()
    xs_d = nc.dram_tensor("xs_scr", [NROW, D], BF16).ap()
    wgb_d = nc.dram_tensor("wgb_scr", [NE * 128, 2048], BF16).ap()
    wub_d = nc.dram_tensor("wub_scr", [NE * 128, 2048], BF16).ap()
    wdb_d = nc.dram_tensor("wdb_scr", [NE * 128, 2048], BF16).ap()
    ys_d = nc.dram_tensor("ys_scr", [NROW, D], F32).ap()

    P = Prog()
    ARENA = 106000
    with contextlib.ExitStack() as es:
        arena_t = es.enter_context(nc.sbuf_tensor("arena", [128, ARENA], BF16))
        psb = [es.enter_context(nc.psum_tensor("ps%d" % i, [128, 512], F32)) for i in range(8)]

        pstate = {"h": 0}

        def psum(cols):
            if cols <= 256:
                h = pstate["h"]
                pstate["h"] = (h + 1) % 16
                b, half = h // 2, h % 2
                return psb[b][:, half * 256:half * 256 + cols], ["psb%d" % b]
            h = pstate["h"]
            h += h & 1
            h %= 16
            pstate["h"] = (h + 2) % 16
            b = h // 2
            return psb[b][:, 0:cols], ["psb%d" % b]

        def psum_bf(cols):
            a, names = psum(512)
            return a.bitcast(BF16)[:, 0:cols], names

        A0 = Arena(arena_t, ARENA)
        ohA = A0.alloc(NT * NE).rearrange("p (i e) -> p i e", i=NT)
        ohB = A0.alloc(NT * NE).rearrange("p (i e) -> p i e", i=NT)
        gAB = A0.alloc(2 * NT, F32)
        persist_end = A0.off

        A = Arena(arena_t, ARENA)
        A.off = persist_end
        Win = A.alloc(8 * PW).rearrange("p (k n) -> p k n", k=8)
        Wa = A.alloc(4 * D).rearrange("p (k n) -> p k n", k=4)
        Wc = A.alloc(4 * D).rearrange("p (k n) -> p k n", k=4)
        Wout = A.alloc(8 * D).rearrange("p (k n) -> p k n", k=8)
        Wr = A.alloc(8 * 36).rearrange("p (k n) -> p k n", k=8)
        hT = [A.alloc(8 * T).rearrange("p (k n) -> p k n", k=8) for _ in range(2)]
        xring = [A.alloc(D, F32) for _ in range(2)]
        xres = [A.alloc(D, F32) for _ in range(2)]
        g1b = A.alloc(D, F32)
        g2b = A.alloc(D, F32)
        qkg = A.alloc(640, F32)
        ropeC = A.alloc(NT * 16, F32).rearrange("p (i c) -> p i c", i=NT)
        ropeS = A.alloc(NT * 16, F32).rearrange("p (i c) -> p i c", i=NT)
        hb = A.alloc(D)
        h2b = A.alloc(D)
        kT = A.alloc(2 * KVR * 128).rearrange("p (g n) -> p g n", g=2)
        vring = A.alloc(KVR * 2 * 128).rearrange("p (s g n) -> p s g n", s=KVR, g=2)
        uring = [A.alloc(4 * (T + 2)).rearrange("p (c n) -> p c n", c=4) for _ in range(UR)]
        qT = [A.alloc(8 * 128).rearrange("p (h n) -> p h n", h=8) for _ in range(2)]
        sq = A.alloc(512, F32)
        xn = A.alloc(512, F32)
        rt1 = A.alloc(128, F32)
        rt2 = A.alloc(128, F32)
        qkb = A.alloc(512)
        pT = [A.alloc(512) for _ in range(4)]
        aT = A.alloc(4 * T).rearrange("p (c n) -> p c n", c=4)
        rD = A.alloc(512, F32)
        cbT = A.alloc(4 * T).rearrange("p (c n) -> p c n", c=4)
        cct = [A.alloc(T, F32) for _ in range(2)]
        cv1 = A.alloc(T, F32)
        cv2 = A.alloc(T, F32)
        cT = A.alloc(4 * T).rearrange("p (c n) -> p c n", c=4)
        ta = [A.alloc(T) for _ in range(2)]
        tcg = [A.alloc(T) for _ in range(2)]
        m1 = A.alloc(T, F32)
        m2 = A.alloc(T, F32)
        mergedT = A.alloc(8 * T).rearrange("p (k n) -> p k n", k=8)
        h2T = [A.alloc(8 * 128).rearrange("p (k n) -> p k n", k=8) for _ in range(2)]
        identb = A.alloc(128)
        maskPb = A.alloc(128)
        maskNb = A.alloc(128)
        onesD2 = A.alloc(128)
        esrow = A.alloc(8 * 128).rearrange("p (h n) -> p h n", h=8)
        cmat = A.alloc(512, F32)
        cw = A.alloc(12, F32)
        cbias = A.alloc(4, F32)
        brb = A.alloc(36, F32)
        st = A.alloc(64, F32)
        rtmp = A.alloc(256, F32)
        sk = A.alloc(64, F32)
        skb = A.alloc(16)
        print("phase1 arena bytes", A.off * 2)

        mhalf = st[:, 0:16]
        ss1 = [st[:, 16 + r:17 + r] for r in range(2)]
        rs1 = [st[:, 18 + r:19 + r] for r in range(2)]
        ss2 = [st[:, 20 + r:21 + r] for r in range(2)]
        rs2 = [st[:, 22 + r:23 + r] for r in range(2)]
        ssq = st[:, 24:32]
        rsq = st[:, 32:40]
        vsq = st[:, 40:48]

        def dma(eng, out_ap, in_ap, key, reads=(), writes=(), lat=None):
            return P.op(eng, lambda e: e.dma_start(out=out_ap, in_=in_ap), reads=reads, writes=writes, dma=key, lat=lat)

        dma(SP, cmat, cmat_d, "c_cmat", writes=["cmat"])
        dma(SP, g1b, g1b_d, "c_g1b", writes=["g1b"])
        dma(SP, qkg, qkg_d, "c_qkg", writes=["qkg"])
        dma(SP, ropeC, ropeC_d.rearrange("p (i c) -> p i c", i=NT), "c_ropeC", writes=["ropeC"])
        dma(SP, ropeS, ropeS_d.rearrange("p (i c) -> p i c", i=NT), "c_ropeS", writes=["ropeS"])
        dma(SP, cw, cwl_d, "c_cw", writes=["cw"])
        dma(SP, cbias, cbl_d, "c_cb", writes=["cbias"])
        dma(SP, brb, brb_d, "c_brb", writes=["brb"])
        dma(SP, sk[0:2, 0:8], sinkb_d, "c_sink", writes=["sk"])
        dma(SP, g2b, g2b_d, "c_g2b", writes=["g2b"])
        for (c0, c1, nm_) in ((512, 768, "C"), (1280, 2304, "C"), (0, 512, "D"), (768, 1280, "D"), (2304, 4352, "D")):
            for k in range(8):
                dma(POOL, Win[:, k, c0:c1], w_in[k * 128:(k + 1) * 128, c0:c1], "w_in" + nm_, writes=["Win%s_%d_%d" % (nm_, k, c0)])
        WinC_n = ["WinC_%d_%d" % (k, c0) for k in range(8) for c0 in (512, 1280)]
        WinD_n = ["WinD_%d_%d" % (k, c0) for k in range(8) for c0 in (0, 768, 2304)]
        dma(POOL, Wa, w_a.rearrange("(c p) n -> p c n", p=128), "w_a", writes=["Wa"])
        dma(POOL, Wc, w_c.rearrange("(c p) n -> p c n", p=128), "w_c", writes=["Wc"])
        dma(POOL, Wout, w_out.rearrange("(k p) n -> p k n", p=128), "w_out", writes=["Wout"])
        dma(POOL, Wr, w_r.rearrange("p (k n) -> p k n", k=8), "w_r", writes=["Wr"])

        P.op(DVE, lambda e: e.memset(mhalf, -0.5), writes=["mhalf"])
        P.op(DVE, lambda e: e.tensor_copy(out=identb, in_=cmat[:, 0:128]), reads=["cmat"], writes=["identb"])
        P.op(DVE, lambda e: e.tensor_copy(out=maskPb, in_=cmat[:, 128:256]), reads=["cmat"], writes=["maskPb"])
        P.op(DVE, lambda e: e.tensor_copy(out=maskNb, in_=cmat[:, 256:384]), reads=["cmat"], writes=["maskNb"])
        P.op(DVE, lambda e: e.tensor_copy(out=onesD2[0:2, :], in_=cmat[0:2, 384:512]), reads=["cmat"], writes=["onesD2"])
        P.op(DVE, lambda e: e.tensor_scalar(out=g1b, in0=g1b, scalar1=32.0, scalar2=None, op0=ALU.mult), reads=["g1b"], writes=["g1b"])
        P.op(DVE, lambda e: e.tensor_scalar(out=g2b, in0=g2b, scalar1=32.0, scalar2=None, op0=ALU.mult), reads=["g2b"], writes=["g2b"])
        P.op(DVE, lambda e: e.tensor_scalar(out=qkg, in0=qkg, scalar1=8.0, scalar2=None, op0=ALU.mult), reads=["qkg"], writes=["qkg"])
        P.op(POOL, lambda e: e.memset(vring[:, :, :, 64:128], 1.0), writes=["vones"])
        for r in range(UR):
            P.op(POOL, lambda e, r=r: e.memset(uring[r], 0.0), writes=["u%d" % r, "uh0_%d" % r, "uh1_%d" % r])
        sel = cmat[0:2, 0:2]
        e_f = sk[0:2, 8:16]
        hi_f = sk[0:2, 16:24]
        lo_f = sk[0:2, 24:32]
        fin = sk[0:2, 32:40]
        P.op(ACT, lambda e: e.activation(out=e_f, in_=sk[0:2, 0:8], func=AF.Exp), reads=["sk"], writes=["sk_e"])
        P.op(DVE, lambda e: e.tensor_copy(out=skb[0:2, 0:8], in_=e_f), reads=["sk_e"], writes=["skb"])
        P.op(DVE, lambda e: e.tensor_copy(out=hi_f, in_=skb[0:2, 0:8]), reads=["skb"], writes=["sk_hi"])
        P.op(DVE, lambda e: e.tensor_tensor(out=lo_f, in0=e_f, in1=hi_f, op=ALU.subtract), reads=["sk_e", "sk_hi"], writes=["sk_lo"])
        P.op(DVE, lambda e: e.tensor_scalar(out=fin, in0=hi_f, scalar1=sel[:, 0:1], scalar2=None, op0=ALU.mult), reads=["sk_hi", "cmat"], writes=["sk_fin"])
        P.op(DVE, lambda e: e.scalar_tensor_tensor(out=fin, in0=lo_f, scalar=sel[:, 1:2], in1=fin, op0=ALU.mult, op1=ALU.add), reads=["sk_lo", "sk_fin", "cmat"], writes=["sk_fin"])
        P.op(DVE, lambda e: e.tensor_copy(out=esrow[0:2, :, :], in_=fin[:, :, None].broadcast_to([2, 8, 128])), reads=["sk_fin"], writes=["esrow"])

        import os
        KLEVEL = int(os.environ.get("KLEVEL", "3"))
        KNS = int(os.environ.get("KNS", str(NS)))
        def rms_rstd(ss, rs, n, tag):
            w = ss.shape[-1]
            P.op(POOL, lambda e: e.tensor_scalar(out=rs, in0=ss, scalar1=float(n * EPS), scalar2=None, op0=ALU.add),
                 reads=["ss" + tag], writes=["rs" + tag])
            P.op(POOL, lambda e: e.tensor_tensor(out=rs, in0=rs, in1=mhalf[:, 0:w], op=ALU.pow),
                 reads=["rs" + tag, "mhalf"], writes=["rs" + tag])

        def qk_post(ps_ap, ps_names, H, gain, i):
            W = H * 64
            P.op(ACT, lambda e: e.activation(out=sq[:, 0:W], in_=ps_ap, func=AF.Square), reads=ps_names, writes=["sq"])
            P.op(DVE, lambda e: e.tensor_reduce(out=ssq[:, 0:H], in_=sq[:, 0:W].rearrange("p (h d) -> p h d", h=H), axis=AX.X, op=ALU.add),
                 reads=["sq"], writes=["ssq"])
            P.op(POOL, lambda e: e.tensor_scalar(out=vsq[:, 0:H], in0=ssq[:, 0:H], scalar1=float(64 * EPS), scalar2=None, op0=ALU.add),
                 reads=["ssq"], writes=["vsq"])
            P.op(POOL, lambda e: e.tensor_tensor(out=rsq[:, 0:H], in0=vsq[:, 0:H], in1=mhalf[:, 0:H], op=ALU.pow),
                 reads=["vsq", "mhalf"], writes=["rsq"])
            xn3 = xn[:, 0:W].rearrange("p (h d) -> p h d", h=H)
            P.op(DVE, lambda e: e.tensor_tensor(out=xn3, in0=ps_ap.rearrange("p (h d) -> p h d", h=H),
                                                in1=rsq[:, 0:H, None].broadcast_to([128, H, 64]), op=ALU.mult),
                 reads=ps_names + ["rsq"], writes=["xn"])
            P.op(DVE, lambda e: e.tensor_tensor(out=xn[:, 0:W], in0=xn[:, 0:W], in1=gain, op=ALU.mult),
                 reads=["xn", "qkg"], writes=["xn"])
            qb3 = qkb[:, 0:W].rearrange("p (h d) -> p h d", h=H)
            P.op(ACT, lambda e: e.activation(out=qkb[:, 0:W], in_=xn[:, 0:W], func=AF.Copy), reads=["xn"], writes=["qkb"])
            t1 = rt1[:, 0:H * 16].rearrange("p (h c) -> p h c", h=H)
            t2 = rt2[:, 0:H * 16].rearrange("p (h c) -> p h c", h=H)
            rc = ropeC[:, i:i + 1, :]
            rs_ = ropeS[:, i:i + 1, :]
            P.op(POOL, lambda e: e.tensor_tensor(out=t1, in0=xn3[:, :, 0:16], in1=rc.broadcast_to([128, H, 16]), op=ALU.mult),
                 reads=["xn", "ropeC"], writes=["rt1"])
            P.op(POOL, lambda e: e.tensor_tensor(out=t2[:, :, 0:8], in0=xn3[:, :, 8:16], in1=rs_[:, :, 0:8].broadcast_to([128, H, 8]), op=ALU.mult),
                 reads=["xn", "ropeS"], writes=["rt2a"])
            P.op(POOL, lambda e: e.tensor_tensor(out=t2[:, :, 8:16], in0=xn3[:, :, 0:8], in1=rs_[:, :, 8:16].broadcast_to([128, H, 8]), op=ALU.mult),
                 reads=["xn", "ropeS"], writes=["rt2b"])
            P.op(POOL, lambda e: e.tensor_tensor(out=qb3[:, :, 0:16], in0=t1, in1=t2, op=ALU.add),
                 reads=["rt1", "rt2a", "rt2b", "qkb"], writes=["qkb"])

        zsrc = zrows_d.bitcast(BF16)

        def stage_B(s):
            if (not debug_phase1) and s >= 2:
                for b_ in range((s - 2) * 5, min(NB, (s - 1) * 5)):
                    dma(SP, xs_d[b_ * RB:(b_ + 1) * RB, :], zsrc, "zf%d" % (b_ % 4), reads=["tick%d" % (s - 1)], writes=["xs_z%d" % b_])
            if (not debug_phase1) and s >= 1:
                for ex_ in (range(2 * (s - 1), 2 * (s - 1) + 2) if s < NS - 1 else range(2 * (s - 1), NE)):
                    for (src_t, dst_t, nm) in ((wg_l, wgb_d, "g"), (wu_l, wub_d, "u"), (wd_l, wdb_d, "d")):
                        dma(POOL, dst_t[ex_ * 128:(ex_ + 1) * 128, :], src_t[ex_ * 128:(ex_ + 1) * 128, :], "pc%s%d" % (nm, ex_ % 2),
                            reads=["tick%d" % (s - 1)], writes=["wb_%s%d" % (nm, ex_)])
            for j in range(SUB):
                i = s * SUB + j
                r = i % 2
                xs = xring[r]
                dma(SP, xs, x[i * 128:(i + 1) * 128, :], "x%d" % r, writes=["x%d" % r])
                P.op(ACT, lambda e, xs=xs, r=r: e.activation(out=hb, in_=xs, func=AF.Square, accum_out=ss1[r]),
                     reads=["x%d" % r], writes=["hb", "ss1_%d" % r], cost=1200)
                rms_rstd(ss1[r], rs1[r], D, "1_%d" % r)
                P.op(DVE, lambda e, xs=xs, r=r: e.scalar_tensor_tensor(out=hb, in0=xs, scalar=rs1[r], in1=g1b, op0=ALU.mult, op1=ALU.mult),
                     reads=["x%d" % r, "rs1_%d" % r, "g1b"], writes=["hb"], cost=1200)
                pt, pn = psum_bf(1024)
                for k in range(8):
                    P.op(PE, lambda e, k=k, pt=pt: e.transpose(out=pt[:, k * 128:(k + 1) * 128], in_=hb[:, k * 128:(k + 1) * 128], identity=identb),
                         reads=["hb", "identb"], writes=pn, cost=100)
                dst = hT[s % 2][:, :, j * 128:(j + 1) * 128]
                P.op(ACT, lambda e, pt=pt, dst=dst: e.activation(out=dst, in_=pt.rearrange("p (k n) -> p k n", k=8), func=AF.Copy),
                     reads=pn, writes=["hT%d_%d" % (s % 2, j)] + (["tick%d" % s] if j == SUB - 1 else []), cost=1200)

        def stage_C(s):
            hTs = hT[s % 2]
            for j in range(SUB):
                i = s * SUB + j
                slot = i % KVR
                pk, pkn = psum(256)
                for k in range(8):
                    P.op(PE, lambda e, k=k, pk=pk, j=j: e.matmul(pk, lhsT=hTs[:, k, j * 128:(j + 1) * 128], rhs=Win[:, k, K0:K0 + 256], start=(k == 0), stop=(k == 7)),
                         reads=["hT%d_%d" % (s % 2, j)] + WinC_n, writes=pkn, cost=160)
                P.op(ACT, lambda e, pk=pk, slot=slot: e.activation(out=vring[:, slot, :, 0:64], in_=pk[:, 128:256].rearrange("p (g d) -> p g d", g=2), func=AF.Copy),
                     reads=pkn, writes=["v%d" % slot])
                qk_post(pk[:, 0:128], pkn, 2, qkg[:, 512:640], i)
                pt, pn = psum_bf(256)
                for g in range(2):
                    P.op(PE, lambda e, g=g, pt=pt: e.transpose(out=pt[0:64, g * 128:(g + 1) * 128], in_=qkb[:, g * 64:(g + 1) * 64], identity=identb),
                         reads=["qkb", "identb"], writes=pn, cost=100)
                P.op(ACT, lambda e, pt=pt, slot=slot: e.activation(out=kT[0:64, :, slot * 128:(slot + 1) * 128], in_=pt[0:64, 0:256].rearrange("p (g n) -> p g n", g=2), func=AF.Copy),
                     reads=pn, writes=["k%d" % slot])
            us = uring[s % UR]
            for c in range(4):
                pc, pcn = psum(T)
                for k in range(8):
                    P.op(PE, lambda e, k=k, pc=pc, c=c: e.matmul(pc, lhsT=Win[:, k, CC0 + c * 128:CC0 + (c + 1) * 128], rhs=hTs[:, k, :], start=(k == 0), stop=(k == 7)),
                         reads=["hT%d_%d" % (s % 2, j) for j in range(SUB)] + WinC_n, writes=pcn, cost=160)
                px, pxn = psum(T)
                for k in range(8):
                    P.op(PE, lambda e, k=k, px=px, c=c: e.matmul(px, lhsT=Win[:, k, CX0 + c * 128:CX0 + (c + 1) * 128], rhs=hTs[:, k, :], start=(k == 0), stop=(k == 7)),
                         reads=["hT%d_%d" % (s % 2, j) for j in range(SUB)] + WinC_n, writes=pxn, cost=160)
                cc_ = cct[c % 2]
                P.op(ACT, lambda e, pc=pc, cc_=cc_: e.activation(out=cc_, in_=pc, func=AF.Copy), reads=pcn, writes=["cct%d" % (c % 2)])
                P.op(DVE, lambda e, px=px, cc_=cc_, c=c: e.tensor_tensor(out=us[:, c, 1:T + 1], in0=px, in1=cc_, op=ALU.mult),
                     reads=pxn + ["cct%d" % (c % 2)], writes=["u%d" % (s % UR)])
            if s > 0:
                up = uring[(s - 1) % UR]
                P.op(POOL, lambda e: e.tensor_copy(out=us[:, :, 0:1], in_=up[:, :, T:T + 1]),
                     reads=["u%d" % ((s - 1) % UR)], writes=["uh0_%d" % (s % UR)])
                P.op(POOL, lambda e: e.tensor_copy(out=up[:, :, T + 1:T + 2], in_=us[:, :, 1:2]),
                     reads=["u%d" % (s % UR)], writes=["uh1_%d" % ((s - 1) % UR)])
            else:
                P.op(POOL, lambda e: e.memset(us[:, :, 0:1], 0.0), writes=["uh0_%d" % (s % UR)])
            if s == KNS - 1:
                P.op(POOL, lambda e: e.memset(us[:, :, T + 1:T + 2], 0.0), writes=["uh1_%d" % (s % UR)])

        def router(i, h2Ti):
            pr, prn = psum(36)
            for k in range(8):
                P.op(PE, lambda e, k=k: e.matmul(pr, lhsT=h2Ti[:, k, :], rhs=Wr[:, k, :], start=(k == 0), stop=(k == 7)),
                     reads=["h2T%d" % (i % 2), "Wr"], writes=prn, cost=70)
            lg = rtmp[:, 0:36]
            gmax = rtmp[:, 36:37]
            goh = rtmp[:, 40:44]
            gd = rtmp[:, 44:48]
            gex = rtmp[:, 48:52]
            gsum = rtmp[:, 52:53]
            pg = rtmp[:, 53:54]
            tmp48 = rtmp[:, 64:96].rearrange("p (g j) -> p g j", g=4)
            ein = rtmp[:, 96:104]
            mx1 = rtmp[:, 104:105]
            oh1 = rtmp[:, 112:120]
            e2 = rtmp[:, 120:128]
            mx2 = rtmp[:, 105:106]
            oh2 = rtmp[:, 128:136]
            d12 = rtmp[:, 106:107]
            t12 = rtmp[:, 107:108]
            pa = rtmp[:, 108:109]
            gq1 = rtmp[:, 109:110]
            gq2 = rtmp[:, 110:111]
            nt12 = rtmp[:, 111:112]
            gw = rtmp[:, 136:144]
            gw2 = rtmp[:, 144:152]
            R = "rt_"
            P.op(DVE, lambda e: e.tensor_tensor(out=lg, in0=pr, in1=brb, op=ALU.add), reads=prn + ["brb"], writes=[R + "lg"])
            P.op(DVE, lambda e: e.tensor_reduce(out=gmax, in_=lg[:, 0:4], axis=AX.X, op=ALU.max), reads=[R + "lg"], writes=[R + "gmax"])
            P.op(DVE, lambda e: e.tensor_scalar(out=goh, in0=lg[:, 0:4], scalar1=gmax, scalar2=None, op0=ALU.is_equal), reads=[R + "lg", R + "gmax"], writes=[R + "goh"])
            P.op(DVE, lambda e: e.tensor_scalar(out=gd, in0=lg[:, 0:4], scalar1=gmax, scalar2=None, op0=ALU.subtract), reads=[R + "lg", R + "gmax"], writes=[R + "gd"])
            P.op(ACT, lambda e: e.activation(out=gex, in_=gd, func=AF.Exp, accum_out=gsum), reads=[R + "gd"], writes=[R + "gex", R + "gsum"])
            P.op(DVE, lambda e: e.reciprocal(out=pg, in_=gsum), reads=[R + "gsum"], writes=[R + "pg"])
            P.op(DVE, lambda e: e.tensor_tensor(out=tmp48, in0=lg[:, 4:36].rearrange("p (g j) -> p g j", g=4),
                                                in1=goh[:, :, None].broadcast_to([128, 4, 8]), op=ALU.mult),
                 reads=[R + "lg", R + "goh"], writes=[R + "tmp48"])
            P.op(DVE, lambda e: e.tensor_reduce(out=ein, in_=tmp48.rearrange("p g j -> p j g"), axis=AX.X, op=ALU.add), reads=[R + "tmp48"], writes=[R + "ein"])
            P.op(DVE, lambda e: e.tensor_reduce(out=mx1, in_=ein, axis=AX.X, op=ALU.max), reads=[R + "ein"], writes=[R + "mx1"])
            P.op(DVE, lambda e: e.tensor_scalar(out=oh1, in0=ein, scalar1=mx1, scalar2=None, op0=ALU.is_equal), reads=[R + "ein", R + "mx1"], writes=[R + "oh1"])
            P.op(DVE, lambda e: e.scalar_tensor_tensor(out=e2, in0=oh1, scalar=-1e30, in1=ein, op0=ALU.mult, op1=ALU.add), reads=[R + "oh1", R + "ein"], writes=[R + "e2"])
            P.op(DVE, lambda e: e.tensor_reduce(out=mx2, in_=e2, axis=AX.X, op=ALU.max), reads=[R + "e2"], writes=[R + "mx2"])
            P.op(DVE, lambda e: e.tensor_scalar(out=oh2, in0=e2, scalar1=mx2, scalar2=None, op0=ALU.is_equal), reads=[R + "e2", R + "mx2"], writes=[R + "oh2"])
            P.op(DVE, lambda e: e.tensor_tensor(out=d12, in0=mx1, in1=mx2, op=ALU.subtract), reads=[R + "mx1", R + "mx2"], writes=[R + "d12"])
            P.op(ACT, lambda e: e.activation(out=t12, in_=d12, func=AF.Tanh, scale=0.5), reads=[R + "d12"], writes=[R + "t12"])
            P.op(DVE, lambda e: e.tensor_scalar(out=pa, in0=pg, scalar1=0.25, scalar2=None, op0=ALU.mult), reads=[R + "pg"], writes=[R + "pa"])
            P.op(DVE, lambda e: e.scalar_tensor_tensor(out=gq1, in0=t12, scalar=1.0, in1=pa, op0=ALU.add, op1=ALU.mult), reads=[R + "t12", R + "pa"], writes=[R + "gq1"])
            P.op(DVE, lambda e: e.tensor_scalar(out=nt12, in0=t12, scalar1=-1.0, scalar2=1.0, op0=ALU.mult, op1=ALU.add), reads=[R + "t12"], writes=[R + "nt12"])
            P.op(DVE, lambda e: e.tensor_tensor(out=gq2, in0=nt12, in1=pa, op=ALU.mult), reads=[R + "nt12", R + "pa"], writes=[R + "gq2"])
            P.op(DVE, lambda e: e.tensor_tensor(out=ohA[:, i, :].rearrange("p (g j) -> p g j", g=4),
                                                in0=goh[:, :, None].broadcast_to([128, 4, 8]),
                                                in1=oh1[:, None, :].broadcast_to([128, 4, 8]), op=ALU.mult),
                 reads=[R + "goh", R + "oh1"], writes=["ohA"])
            P.op(DVE, lambda e: e.tensor_tensor(out=ohB[:, i, :].rearrange("p (g j) -> p g j", g=4),
                                                in0=goh[:, :, None].broadcast_to([128, 4, 8]),
                                                in1=oh2[:, None, :].broadcast_to([128, 4, 8]), op=ALU.mult),
                 reads=[R + "goh", R + "oh2"], writes=["ohB"])
            P.op(DVE, lambda e: e.tensor_copy(out=gAB[:, i:i + 1], in_=gq1), reads=[R + "gq1"], writes=["gAB"])
            P.op(DVE, lambda e: e.tensor_copy(out=gAB[:, NT + i:NT + i + 1], in_=gq2), reads=[R + "gq2"], writes=["gAB"])

        def stage_D(t):
            hTt = hT[t % 2]
            hT_names = ["hT%d_%d" % (t % 2, j) for j in range(SUB)]
            for j in range(SUB):
                i = t * SUB + j
                dma(SP, xres[i % 2], x[i * 128:(i + 1) * 128, :], "xr%d" % (i % 2), writes=["xr%d" % (i % 2)])
            for j in range(SUB):
                i = t * SUB + j
                pq, pqn = psum(512)
                for k in range(8):
                    P.op(PE, lambda e, k=k, pq=pq, j=j: e.matmul(pq, lhsT=hTt[:, k, j * 128:(j + 1) * 128], rhs=Win[:, k, Q0:Q0 + 512], start=(k == 0), stop=(k == 7)),
                         reads=["hT%d_%d" % (t % 2, j)] + WinD_n, writes=pqn, cost=260)
                qk_post(pq, pqn, 8, qkg[:, 0:512], i)
                pt, pn = psum_bf(1024)
                for h in range(8):
                    P.op(PE, lambda e, h=h, pt=pt: e.transpose(out=pt[0:64, h * 128:(h + 1) * 128], in_=qkb[:, h * 64:(h + 1) * 64], identity=identb),
                         reads=["qkb", "identb"], writes=pn, cost=100)
                qTi = qT[i % 2]
                P.op(ACT, lambda e, pt=pt, qTi=qTi: e.activation(out=qTi[0:64, :, :], in_=pt[0:64, :].rearrange("p (h n) -> p h n", h=8), func=AF.Copy),
                     reads=pn, writes=["qT%d" % (i % 2)])
                for g in range(2):
                    blocks = [b for b in (i - 1, i, i + 1) if 0 <= b < KNS * SUB]
                    pod, podn = psum(512)
                    pts = []
                    for bi, b in enumerate(blocks):
                        slot = b % KVR
                        psS, psn = psum(512)
                        P.op(PE, lambda e, psS=psS, slot=slot, g=g, qTi=qTi: e.matmul(psS, lhsT=kT[0:64, g, slot * 128:(slot + 1) * 128],
                                                                                     rhs=qTi[0:64, 4 * g:4 * g + 4, :], start=True, stop=True),
                             reads=["k%d" % slot, "qT%d" % (i % 2)], writes=psn, cost=260)
                        pidx = pstate.setdefault("pT", 0)
                        pstate["pT"] = (pidx + 1) % 4
                        pTi = pT[pidx]
                        P.op(ACT, lambda e, psS=psS, pTi=pTi: e.activation(out=pTi, in_=psS, func=AF.Exp, scale=0.125),
                             reads=psn, writes=["pT%d" % pidx], cost=600)
                        if b != i:
                            mk_ = maskPb if b == i - 1 else maskNb
                            P.op(POOL, lambda e, pTi=pTi, mk_=mk_: e.tensor_tensor(out=pTi.rearrange("p (h q) -> p h q", h=4),
                                                                                 in0=pTi.rearrange("p (h q) -> p h q", h=4),
                                                                                 in1=mk_[:, None, :].broadcast_to([128, 4, 128]), op=ALU.mult),
                                 reads=["pT%d" % pidx, "maskPb", "maskNb"], writes=["pT%d" % pidx])
                        P.op(PE, lambda e, pod=pod, slot=slot, g=g, pTi=pTi, bi=bi: e.matmul(pod, lhsT=vring[:, slot, g, :], rhs=pTi, start=(bi == 0), stop=False),
                             reads=["v%d" % slot, "vones", "pT%d" % pidx], writes=podn, cost=260)
                    P.op(PE, lambda e, pod=pod, g=g: e.matmul(pod, lhsT=onesD2[0:2, :], rhs=esrow[0:2, 4 * g:4 * g + 4, :], start=False, stop=True),
                         reads=["onesD2", "esrow"], writes=podn, cost=260)
                    P.op(DVE, lambda e, pod=pod: e.reciprocal(out=rD[0:64, :], in_=pod[64:128, :]), reads=podn, writes=["rD"], cost=3400)
                    o4 = pod[0:64, :].rearrange("p (a b q) -> p a b q", a=2, b=2)
                    r4 = rD[0:64, :].rearrange("p (a b q) -> p a b q", a=2, b=2)
                    P.op(DVE, lambda e, o4=o4, r4=r4, g=g, j=j: e.tensor_tensor(out=aT[0:64, 2 * g:2 * g + 2, j * 128:(j + 1) * 128], in0=o4[:, :, 0, :], in1=r4[:, :, 0, :], op=ALU.mult),
                         reads=podn + ["rD"], writes=["aT"])
                    P.op(DVE, lambda e, o4=o4, r4=r4, g=g, j=j: e.tensor_tensor(out=aT[64:128, 2 * g:2 * g + 2, j * 128:(j + 1) * 128], in0=o4[:, :, 1, :], in1=r4[:, :, 1, :], op=ALU.mult),
                         reads=podn + ["rD"], writes=["aT"])
            ut = uring[t % UR]
            un = ["u%d" % (t % UR), "uh0_%d" % (t % UR), "uh1_%d" % (t % UR)]
            for c in range(4):
                pc, pcn = psum(T)
                for k in range(8):
                    P.op(PE, lambda e, k=k, pc=pc, c=c: e.matmul(pc, lhsT=Win[:, k, CB0 + c * 128:CB0 + (c + 1) * 128], rhs=hTt[:, k, :], start=(k == 0), stop=(k == 7)),
                         reads=hT_names + WinD_n, writes=pcn, cost=160)
                P.op(ACT, lambda e, pc=pc, c=c: e.activation(out=cbT[:, c, :], in_=pc, func=AF.Copy), reads=pcn, writes=["cbT%d" % c])
                P.op(DVE, lambda e, c=c: e.tensor_scalar(out=cv1, in0=ut[:, c, 0:T], scalar1=cw[:, 3 * c:3 * c + 1], scalar2=None, op0=ALU.mult),
                     reads=un + ["cw"], writes=["cv1"])
                P.op(DVE, lambda e, c=c: e.scalar_tensor_tensor(out=cv2, in0=ut[:, c, 1:T + 1], scalar=cw[:, 3 * c + 1:3 * c + 2], in1=cv1, op0=ALU.mult, op1=ALU.add),
                     reads=un + ["cw", "cv1"], writes=["cv2"])
                P.op(DVE, lambda e, c=c: e.scalar_tensor_tensor(out=cv1, in0=ut[:, c, 2:T + 2], scalar=cw[:, 3 * c + 2:3 * c + 3], in1=cv2, op0=ALU.mult, op1=ALU.add),
                     reads=un + ["cw", "cv2"], writes=["cv1"])
                P.op(DVE, lambda e, c=c: e.scalar_tensor_tensor(out=cT[:, c, :], in0=cv1, scalar=cbias[:, c:c + 1], in1=cbT[:, c, :], op0=ALU.add, op1=ALU.mult),
                     reads=["cv1", "cbias", "cbT%d" % c], writes=["cT"])
            for m in range(8):
                pga, pgan = psum(T)
                for k in range(8):
                    P.op(PE, lambda e, k=k, pga=pga, m=m: e.matmul(pga, lhsT=Win[:, k, GA0 + m * 128:GA0 + (m + 1) * 128], rhs=hTt[:, k, :], start=(k == 0), stop=(k == 7)),
                         reads=hT_names + WinD_n, writes=pgan, cost=160)
                pgc, pgcn = psum(T)
                for k in range(8):
                    P.op(PE, lambda e, k=k, pgc=pgc, m=m: e.matmul(pgc, lhsT=Win[:, k, GC0 + m * 128:GC0 + (m + 1) * 128], rhs=hTt[:, k, :], start=(k == 0), stop=(k == 7)),
                         reads=hT_names + WinD_n, writes=pgcn, cost=160)
                pA, pAn = psum(T)
                for c in range(4):
                    P.op(PE, lambda e, c=c, pA=pA, m=m: e.matmul(pA, lhsT=Wa[:, c, m * 128:(m + 1) * 128], rhs=aT[:, c, :], start=(c == 0), stop=(c == 3)),
                         reads=["aT", "Wa"], writes=pAn, cost=160)
                pC, pCn = psum(T)
                for c in range(4):
                    P.op(PE, lambda e, c=c, pC=pC, m=m: e.matmul(pC, lhsT=Wc[:, c, m * 128:(m + 1) * 128], rhs=cT[:, c, :], start=(c == 0), stop=(c == 3)),
                         reads=["cT", "Wc"], writes=pCn, cost=160)
                ta_ = ta[m % 2]
                tc_ = tcg[m % 2]
                P.op(ACT, lambda e, pga=pga, ta_=ta_: e.activation(out=ta_, in_=pga, func=AF.Tanh, scale=0.5), reads=pgan, writes=["ta%d" % (m % 2)])
                P.op(ACT, lambda e, pgc=pgc, tc_=tc_: e.activation(out=tc_, in_=pgc, func=AF.Tanh, scale=0.5), reads=pgcn, writes=["tc%d" % (m % 2)])
                P.op(DVE, lambda e, pA=pA, ta_=ta_: e.scalar_tensor_tensor(out=m1, in0=ta_, scalar=1.0, in1=pA, op0=ALU.add, op1=ALU.mult),
                     reads=pAn + ["ta%d" % (m % 2)], writes=["m1"])
                P.op(DVE, lambda e, pC=pC, tc_=tc_: e.scalar_tensor_tensor(out=m2, in0=tc_, scalar=1.0, in1=pC, op0=ALU.add, op1=ALU.mult),
                     reads=pCn + ["tc%d" % (m % 2)], writes=["m2"])
                P.op(POOL, lambda e, m=m: e.tensor_tensor(out=mergedT[:, m, :], in0=m1, in1=m2, op=ALU.add),
                     reads=["m1", "m2"], writes=["mergedT"])
            for j in range(SUB):
                i = t * SUB + j
                r = i % 2
                xr = xres[r]
                for half in range(2):
                    po, pon = psum(512)
                    for k in range(8):
                        P.op(PE, lambda e, k=k, po=po, j=j, half=half: e.matmul(po, lhsT=mergedT[:, k, j * 128:(j + 1) * 128], rhs=Wout[:, k, half * 512:(half + 1) * 512], start=(k == 0), stop=(k == 7)),
                             reads=["mergedT", "Wout"], writes=pon, cost=260)
                    P.op(DVE, lambda e, po=po, xr=xr, half=half: e.scalar_tensor_tensor(out=xr[:, half * 512:(half + 1) * 512], in0=po, scalar=0.5, in1=xr[:, half * 512:(half + 1) * 512], op0=ALU.mult, op1=ALU.add),
                         reads=pon + ["xr%d" % r], writes=["xr%d" % r])
                dma(SP, out[i * 128:(i + 1) * 128, :], xr, "st_x2_%d" % r, reads=["xr%d" % r])
                if debug_phase1:
                    continue
                P.op(ACT, lambda e, xr=xr, r=r: e.activation(out=h2b, in_=xr, func=AF.Square, accum_out=ss2[r]),
                     reads=["xr%d" % r], writes=["h2b", "ss2_%d" % r], cost=1200)
                rms_rstd(ss2[r], rs2[r], D, "2_%d" % r)
                P.op(DVE, lambda e, xr=xr, r=r: e.scalar_tensor_tensor(out=h2b, in0=xr, scalar=rs2[r], in1=g2b, op0=ALU.mult, op1=ALU.mult),
                     reads=["xr%d" % r, "rs2_%d" % r, "g2b"], writes=["h2b"], cost=1200)
                pt, pn = psum_bf(1024)
                for k in range(8):
                    P.op(PE, lambda e, k=k, pt=pt: e.transpose(out=pt[:, k * 128:(k + 1) * 128], in_=h2b[:, k * 128:(k + 1) * 128], identity=identb),
                         reads=["h2b", "identb"], writes=pn, cost=100)
                h2Ti = h2T[r]
                P.op(ACT, lambda e, pt=pt, h2Ti=h2Ti: e.activation(out=h2Ti, in_=pt.rearrange("p (k n) -> p k n", k=8), func=AF.Copy),
                     reads=pn, writes=["h2T%d" % r], cost=1200)
                dma(SP, h2_d[i * 128:(i + 1) * 128, :], h2b, "st_h2", reads=["h2b"])
                router(i, h2Ti)

        for s in range(KNS + 1):
            if s < KNS:
                if KLEVEL >= 1:
                    stage_B(s)
                if KLEVEL >= 2:
                    stage_C(s)
            if s >= 1 and KLEVEL >= 3:
                stage_D(s - 1)

        if not debug_phase1:
            I32 = mybir.dt.int32
            M = Arena(arena_t, ARENA)
            M.off = persist_end
            h2flat = M.alloc(NT * D)
            h2sb = h2flat.rearrange("p (i d) -> p i d", i=NT)
            CR = 4
            yA = [h2flat[:, (3 * r_) * 2048:(3 * r_ + 1) * 2048].bitcast(F32) for r_ in range(CR)]
            yB = [h2flat[:, (3 * r_ + 1) * 2048:(3 * r_ + 2) * 2048].bitcast(F32) for r_ in range(CR)]
            x2t = [h2flat[:, (3 * r_ + 2) * 2048:(3 * r_ + 3) * 2048].bitcast(F32) for r_ in range(CR)]
            Msel = M.alloc(NT * NE).rearrange("p (i e) -> p i e", i=NT)
            Mcum = M.alloc((NT + 1) * NE).rearrange("p (i e) -> p i e", i=NT + 1)
            rank = M.alloc(NT * NE, F32).rearrange("p (i e) -> p i e", i=NT)
            pos = M.alloc(NT * NE, F32).rearrange("p (i e) -> p i e", i=NT)
            tmpA = M.alloc(NT * NE, F32).rearrange("p (i e) -> p i e", i=NT)
            cmoe = M.alloc(472, F32)
            identm = M.alloc(128)
            Lst = M.alloc(128)
            Ones = M.alloc(128)
            cnt = M.alloc(NE, F32)
            cmpT = M.alloc(NE * 16, F32).rearrange("p (e k) -> p e k", e=NE)
            nblk = M.alloc(NE, F32)
            pc = M.alloc(NE, F32)
            sc0 = M.alloc(NE, F32)
            sc1 = M.alloc(NE, F32)
            pst = M.alloc(NE, F32)
            cmpB = M.alloc(NB * NE, F32).rearrange("p (b e) -> p b e", b=NB)
            be_f = M.alloc(NB, F32)
            be_i = M.alloc(NB, F32).bitcast(I32)
            iw_f = M.alloc(NB, F32)
            usedf = M.alloc(NB, F32)
            dA_f = M.alloc(NT, F32)
            dB_f = M.alloc(NT, F32)
            dA_i = M.alloc(NT, F32).bitcast(I32)
            dB_i = M.alloc(NT, F32).bitcast(I32)
            NW = 3
            Wg = [M.alloc(8 * 256).rearrange("p (k n) -> p k n", k=8) for _ in range(NW)]
            Wu = [M.alloc(8 * 256).rearrange("p (k n) -> p k n", k=8) for _ in range(NW)]
            Wd = [M.alloc(2 * D).rearrange("p (k n) -> p k n", k=2) for _ in range(NW)]
            xb = [M.alloc(SB * D).rearrange("p (s d) -> p s d", s=SB) for _ in range(2)]
            XT = [M.alloc(8 * RB).rearrange("p (k n) -> p k n", k=8) for _ in range(2)]
            tg = [M.alloc(RB) for _ in range(2)]
            sg = [M.alloc(RB, F32) for _ in range(2)]
            hid = [M.alloc(2 * RB).rearrange("p (f n) -> p f n", f=2) for _ in range(2)]
            yb = [M.alloc(SB * D, F32).rearrange("p (s d) -> p s d", s=SB) for _ in range(2)]
            bar = M.alloc(16, F32)
            print("phase2 arena bytes", M.off * 2)
            PH = ["PH2"]
            P.barrier(DVE, lambda e: e.memset(bar, 0.0), extra=PH)

            dma(SP, cmoe, cmoe_d, "c_cmoe", reads=PH, writes=["cmoe"])
            for q4 in range(4):
                dma(SP, h2sb[:, q4 * 8:(q4 + 1) * 8, :], h2_d[q4 * 1024:(q4 + 1) * 1024, :].rearrange("(i p) d -> p i d", p=128),
                    "h2sb%d" % q4, reads=PH, writes=["h2sb%d" % q4], lat=12000.0)
            THk = cmoe[:, 0:16]
            BR_ = cmoe[:, 16:16 + NB]
            P.op(DVE, lambda e: e.tensor_copy(out=Lst, in_=cmoe[:, 80:208]), reads=PH + ["cmoe"], writes=["Lst"])
            P.op(DVE, lambda e: e.tensor_copy(out=Ones, in_=cmoe[:, 208:336]), reads=PH + ["cmoe"], writes=["Ones"])
            P.op(DVE, lambda e: e.tensor_copy(out=identm, in_=cmoe[:, 336:464]), reads=PH + ["cmoe"], writes=["identm"])
            P.op(DVE, lambda e: e.tensor_tensor(out=Msel, in0=ohA, in1=ohB, op=ALU.add), reads=PH + ["ohA", "ohB"], writes=["Msel"], cost=1100)
            P.op(DVE, lambda e: e.memset(Mcum[:, 0, :], 0.0), reads=PH, writes=["Mcum0"])
            for i in range(1, NT + 1):
                P.op(DVE, lambda e, i=i: e.tensor_tensor(out=Mcum[:, i, :], in0=Mcum[:, i - 1, :], in1=Msel[:, i - 1, :], op=ALU.add),
                     reads=PH + ["Mcum%d" % (i - 1), "Msel"], writes=["Mcum%d" % i], cost=150)
            for half in range(2):
                pr_, prn_ = psum(512)
                for ii in range(16):
                    i = half * 16 + ii
                    P.op(PE, lambda e, pr_=pr_, ii=ii, i=i: e.matmul(pr_[:, ii * 32:(ii + 1) * 32], lhsT=Ones, rhs=Mcum[:, i, :], start=True, stop=False),
                         reads=PH + ["Ones", "Mcum%d" % i], writes=prn_, cost=70)
                    P.op(PE, lambda e, pr_=pr_, ii=ii, i=i: e.matmul(pr_[:, ii * 32:(ii + 1) * 32], lhsT=Lst, rhs=Msel[:, i, :], start=False, stop=True),
                         reads=PH + ["Lst", "Msel"], writes=prn_, cost=70)
                P.op(ACT, lambda e, pr_=pr_, half=half: e.activation(out=rank[:, half * 16:(half + 1) * 16, :], in_=pr_.rearrange("p (i e) -> p i e", i=16), func=AF.Copy),
                     reads=PH + prn_, writes=["rank%d" % half])
            pcn_, pcnn = psum(32)
            P.op(PE, lambda e: e.matmul(pcn_, lhsT=Ones, rhs=Mcum[:, NT, :], start=True, stop=True), reads=PH + ["Ones", "Mcum%d" % NT], writes=pcnn, cost=70)
            P.op(ACT, lambda e: e.activation(out=cnt, in_=pcn_, func=AF.Copy), reads=PH + pcnn, writes=["cnt"])
            P.op(DVE, lambda e: e.tensor_tensor(out=cmpT, in0=cnt[:, :, None].broadcast_to([128, NE, 16]), in1=THk[:, None, :].broadcast_to([128, NE, 16]), op=ALU.is_gt),
                 reads=PH + ["cnt", "cmoe"], writes=["cmpT"])
            P.op(DVE, lambda e: e.tensor_reduce(out=nblk, in_=cmpT, axis=AX.X, op=ALU.add), reads=PH + ["cmpT"], writes=["nblk"])
            P.op(DVE, lambda e: e.tensor_scalar(out=pc, in0=nblk, scalar1=float(RB), scalar2=None, op0=ALU.mult), reads=PH + ["nblk"], writes=["pc"])
            bufs = [(sc0, "sc0"), (sc1, "sc1")]
            src, srcn = pc, "pc"
            bi = 0
            for dd in (1, 2, 4, 8, 16):
                dst, dstn = bufs[bi]
                P.op(DVE, lambda e, src=src, dst=dst, dd=dd: e.tensor_tensor(out=dst[:, dd:NE], in0=src[:, dd:NE], in1=src[:, 0:NE - dd], op=ALU.add),
                     reads=PH + [srcn, srcn + "h"], writes=[dstn])
                P.op(DVE, lambda e, src=src, dst=dst, dd=dd: e.tensor_copy(out=dst[:, 0:dd], in_=src[:, 0:dd]),
                     reads=PH + [srcn, srcn + "h"], writes=[dstn + "h"])
                src, srcn = dst, dstn
                bi ^= 1
            pendn = [srcn, srcn + "h"]
            pend = src
            P.op(DVE, lambda e: e.tensor_tensor(out=pst, in0=pend, in1=pc, op=ALU.subtract), reads=PH + pendn + ["pc"], writes=["pst"])
            P.op(DVE, lambda e: e.tensor_tensor(out=pos, in0=rank, in1=pst[:, None, :].broadcast_to([128, NT, NE]), op=ALU.add),
                 reads=PH + ["rank0", "rank1", "pst"], writes=["pos"], cost=1200)
            for (oh_, df_, di_, nm) in ((ohA, dA_f, dA_i, "A"), (ohB, dB_f, dB_i, "B")):
                P.op(DVE, lambda e, oh_=oh_: e.tensor_tensor(out=tmpA, in0=oh_, in1=pos, op=ALU.mult), reads=PH + ["ohA", "ohB", "pos"], writes=["tmpA"], cost=1200)
                P.op(DVE, lambda e, df_=df_: e.tensor_reduce(out=df_, in_=tmpA, axis=AX.X, op=ALU.add), reads=PH + ["tmpA"], writes=["d%s_f" % nm], cost=1200)
                P.op(DVE, lambda e, df_=df_, di_=di_: e.tensor_copy(out=di_, in_=df_), reads=PH + ["d%s_f" % nm], writes=["d%s_i" % nm])
            P.op(DVE, lambda e: e.tensor_tensor(out=cmpB, in0=pend[:, None, :].broadcast_to([128, NB, NE]), in1=BR_[:, :, None].broadcast_to([128, NB, NE]), op=ALU.is_le),
                 reads=PH + pendn + ["cmoe"], writes=["cmpB"], cost=2200)
            P.op(DVE, lambda e: e.tensor_reduce(out=be_f, in_=cmpB, axis=AX.X, op=ALU.add), reads=PH + ["cmpB"], writes=["be_f"], cost=2200)
            P.op(DVE, lambda e: e.tensor_scalar(out=be_f, in0=be_f, scalar1=float(NE - 1), scalar2=None, op0=ALU.min), reads=PH + ["be_f"], writes=["be_f"])
            P.op(DVE, lambda e: e.tensor_scalar(out=iw_f, in0=be_f, scalar1=128.0, scalar2=cmoe[:, 464:465], op0=ALU.mult, op1=ALU.add), reads=PH + ["be_f", "cmoe"], writes=["iw_f"])
            P.op(DVE, lambda e: e.tensor_scalar(out=usedf, in0=BR_, scalar1=pend[:, NE - 1:NE], scalar2=None, op0=ALU.is_lt), reads=PH + pendn + ["cmoe"], writes=["usedf"])
            P.op(DVE, lambda e: e.tensor_scalar(out=usedf, in0=usedf, scalar1=-1.0e6, scalar2=1.0e6, op0=ALU.mult, op1=ALU.add), reads=PH + ["usedf"], writes=["usedf"])
            P.op(DVE, lambda e: e.tensor_tensor(out=iw_f, in0=iw_f, in1=usedf, op=ALU.add), reads=PH + ["iw_f", "usedf"], writes=["iw_f"])
            P.op(DVE, lambda e: e.tensor_copy(out=be_i, in_=iw_f), reads=PH + ["iw_f"], writes=["be_i"])
            zn = ["xs_z%d" % b_ for b_ in range(NB)]
            for i in range(NT):
                for (di_, nm) in ((dA_i, "A"), (dB_i, "B")):
                    P.op(POOL, lambda e, i=i, di_=di_: e.indirect_dma_start(out=xs_d[:, :], out_offset=bass.IndirectOffsetOnAxis(ap=di_[:, i:i + 1], axis=0),
                                                                           in_=h2sb[:, i, :], in_offset=None),
                         reads=PH + ["h2sb%d" % (i // 8), "d%s_i" % nm] + zn,
                         writes=["xs_w%d%s" % (i, nm)], dma="scat", lat=6000.0)
            xs_names = ["xs_w%d%s" % (i, nm) for i in range(NT) for nm in "AB"]
            for b_ in range(NB):
                w = b_ % NW
                r2 = b_ % 2

                for (dst, src_t, nm) in ((Wg[w], wgb_d, "g"), (Wu[w], wub_d, "u"), (Wd[w], wdb_d, "d")):
                    def wfn(e, dst=dst, src_t=src_t, b_=b_):
                        if "bv" not in pstate:
                            reg = e.alloc_register("wbound")
                            e.reg_mov(reg, NE * 128 - 1)
                            pstate["bv"] = e.snap(reg)
                        return e.indirect_dma_start(out=dst.rearrange("p k n -> p (k n)"), out_offset=None, in_=src_t[:, :],
                                                    in_offset=bass.IndirectOffsetOnAxis(ap=be_i[:, b_:b_ + 1], axis=0),
                                                    bounds_check=pstate["bv"], oob_is_err=False)
                    P.op(POOL, wfn,
                         reads=PH + ["be_i"], writes=["W%s%d" % (nm, w)], dma="w%s%d" % (nm, w), lat=9000.0, cost=1500)
                dma(SP, xb[r2], xs_d[b_ * RB:(b_ + 1) * RB, :].rearrange("(s p) d -> p s d", p=128), "xb%d" % r2,
                    reads=PH + xs_names, writes=["xb%d" % r2], lat=5000.0)
                for sb_ in range(SB):
                    pt, pn = psum_bf(1024)
                    for k in range(8):
                        P.op(PE, lambda e, k=k, pt=pt, sb_=sb_, r2=r2: e.transpose(out=pt[:, k * 128:(k + 1) * 128], in_=xb[r2][:, sb_, k * 128:(k + 1) * 128], identity=identm),
                             reads=PH + ["xb%d" % r2, "identm"], writes=pn, cost=100)
                    P.op(ACT, lambda e, pt=pt, sb_=sb_, r2=r2: e.activation(out=XT[r2][:, :, sb_ * 128:(sb_ + 1) * 128], in_=pt.rearrange("p (k n) -> p k n", k=8), func=AF.Copy),
                         reads=PH + pn, writes=["XT%d_%d" % (r2, sb_)], cost=1200)
                xtn = ["XT%d_%d" % (r2, sb_) for sb_ in range(SB)]
                for f in range(2):
                    pg_, pgn = psum(RB)
                    for k in range(8):
                        P.op(PE, lambda e, k=k, pg_=pg_, f=f, w=w, r2=r2: e.matmul(pg_, lhsT=Wg[w][:, k, f * 128:(f + 1) * 128], rhs=XT[r2][:, k, :], start=(k == 0), stop=(k == 7)),
                             reads=PH + ["Wg%d" % w] + xtn, writes=pgn, cost=160)
                    pu_, pun = psum(RB)
                    for k in range(8):
                        P.op(PE, lambda e, k=k, pu_=pu_, f=f, w=w, r2=r2: e.matmul(pu_, lhsT=Wu[w][:, k, f * 128:(f + 1) * 128], rhs=XT[r2][:, k, :], start=(k == 0), stop=(k == 7)),
                             reads=PH + ["Wu%d" % w] + xtn, writes=pun, cost=160)
                    tg_ = tg[f]
                    sg_ = sg[f]
                    P.op(ACT, lambda e, pg_=pg_, tg_=tg_: e.activation(out=tg_, in_=pg_, func=AF.Tanh, scale=0.5), reads=PH + pgn, writes=["tg%d" % f], cost=500)
                    P.op(DVE, lambda e, pg_=pg_, tg_=tg_, sg_=sg_: e.scalar_tensor_tensor(out=sg_, in0=tg_, scalar=1.0, in1=pg_, op0=ALU.add, op1=ALU.mult),
                         reads=PH + pgn + ["tg%d" % f], writes=["sg%d" % f], cost=400)
                    P.op(DVE, lambda e, pu_=pu_, sg_=sg_, f=f, r2=r2: e.tensor_tensor(out=hid[r2][:, f, :], in0=pu_, in1=sg_, op=ALU.mult),
                         reads=PH + pun + ["sg%d" % f], writes=["hid%d_%d" % (r2, f)], cost=400)
                for sb_ in range(SB):
                    for half in range(2):
                        py, pyn = psum(512)
                        for f in range(2):
                            P.op(PE, lambda e, f=f, py=py, sb_=sb_, half=half, w=w, r2=r2: e.matmul(py, lhsT=hid[r2][:, f, sb_ * 128:(sb_ + 1) * 128], rhs=Wd[w][:, f, half * 512:(half + 1) * 512], start=(f == 0), stop=(f == 1)),
                                 reads=PH + ["hid%d_0" % r2, "hid%d_1" % r2, "Wd%d" % w], writes=pyn, cost=260)
                        if (sb_ + half) % 2 == 0:
                            P.op(ACT, lambda e, py=py, sb_=sb_, half=half, r2=r2: e.activation(out=yb[r2][:, sb_, half * 512:(half + 1) * 512], in_=py, func=AF.Copy),
                                 reads=PH + pyn, writes=["yb%d_%d%d" % (r2, sb_, half)], cost=750)
                        else:
                            P.op(DVE, lambda e, py=py, sb_=sb_, half=half, r2=r2: e.tensor_copy(out=yb[r2][:, sb_, half * 512:(half + 1) * 512], in_=py),
                                 reads=PH + pyn, writes=["yb%d_%d%d" % (r2, sb_, half)], cost=650)
                dma(SP, ys_d[b_ * RB:(b_ + 1) * RB, :].rearrange("(s p) d -> p s d", p=128), yb[r2], "st_y%d" % r2,
                    reads=PH + ["yb%d_%d%d" % (r2, sb_, half) for sb_ in range(SB) for half in range(2)], writes=["ys_w%d" % b_], lat=5000.0)
            ys_names = ["ys_w%d" % b_ for b_ in range(NB)]
            for i in range(NT):
                r2 = i % CR
                dma(SP, x2t[r2], out[i * 128:(i + 1) * 128, :], "x2t%d" % r2, reads=PH + xs_names, writes=["x2t%d" % r2])
                for (ybuf, di_, nm) in ((yA[r2], dA_i, "A"), (yB[r2], dB_i, "B")):
                    P.op(POOL, lambda e, i=i, di_=di_, ybuf=ybuf: e.indirect_dma_start(out=ybuf, out_offset=None, in_=ys_d[:, :],
                                                                                    in_offset=bass.IndirectOffsetOnAxis(ap=di_[:, i:i + 1], axis=0)),
                         reads=PH + ["d%s_i" % nm] + ys_names, writes=["y%s%d" % (nm, r2)], dma="gy%s%d" % (nm, r2), lat=6000.0)
                P.op(DVE, lambda e, i=i, r2=r2: e.scalar_tensor_tensor(out=x2t[r2], in0=yA[r2], scalar=gAB[:, i:i + 1], in1=x2t[r2], op0=ALU.mult, op1=ALU.add),
                     reads=PH + ["yA%d" % r2, "gAB", "x2t%d" % r2], writes=["x2t%d" % r2], cost=1200)
                P.op(DVE, lambda e, i=i, r2=r2: e.scalar_tensor_tensor(out=x2t[r2], in0=yB[r2], scalar=gAB[:, NT + i:NT + i + 1], in1=x2t[r2], op0=ALU.mult, op1=ALU.add),
                     reads=PH + ["yB%d" % r2, "gAB", "x2t%d" % r2], writes=["x2t%d" % r2], cost=1200)
                dma(SP, out[i * 128:(i + 1) * 128, :], x2t[r2], "st_o%d" % r2, reads=PH + ["x2t%d" % r2])

        nsem = P.emit(nc)
        print("ops", len(P.ops), "sems", nsem, "model_us", getattr(P, "model_time", 0) / 1e3)
    return nc


_CACHE = {}


def _consts():
    ang_f = (500000.0 ** (-np.arange(0, 16, 2, dtype=np.float32) / 16.0)).astype(np.float32)
    ang = np.arange(S, dtype=np.float32)[:, None] * ang_f[None, :]
    c, s_ = np.cos(ang).astype(np.float32), np.sin(ang).astype(np.float32)
    ropeC = np.concatenate([c, c], axis=1).astype(np.float32)
    ropeS = np.concatenate([-s_, s_], axis=1).astype(np.float32)
    cmat = np.zeros((128, 512), np.float32)
    cmat[:, 0:128] = np.eye(128, dtype=np.float32)
    kk = np.arange(128)[:, None]
    qq = np.arange(128)[None, :]
    cmat[:, 128:256] = (qq <= kk).astype(np.float32)
    cmat[:, 256:384] = (kk <= qq).astype(np.float32)
    cmat[0:2, 384:448] = 0.0
    cmat[0:2, 448:512] = 1.0
    return ropeC, ropeS, cmat


def kernel(x, norm1_g, w_in, q_norm_g, k_norm_g, attn_sink, conv_w, conv_b,
           w_attn_proj, w_conv_proj, w_out, norm2_g, w_router_group, b_router_group,
           w_router_expert, b_router_expert, w_gate_e, w_up_e, w_down_e, _debug_phase1=False):
    f = lambda a: np.ascontiguousarray(np.asarray(a, dtype=np.float32))
    x = f(x)
    n = x.shape[0]
    key = bool(_debug_phase1)
    if key not in _CACHE:
        _CACHE[key] = build_nc(debug_phase1=key)
    nc = _CACHE[key]
    ropeC, ropeS, cmat = _consts()
    sel = np.zeros((128, 16), np.float32)
    shared = dict(
        w_in=f(w_in[0]), w_a=f(w_attn_proj[0]), w_c=f(w_conv_proj[0]), w_out=f(w_out[0]),
        w_r=f(np.concatenate([np.asarray(w_router_group[0]), np.asarray(w_router_expert[0])], axis=1).reshape(8, 128, 36).transpose(1, 0, 2).reshape(128, 288)),
        wg_l=f(np.asarray(w_gate_e[0]).reshape(NE, 8, 128, 256).transpose(0, 2, 1, 3).reshape(NE * 128, 2048)),
        wu_l=f(np.asarray(w_up_e[0]).reshape(NE, 8, 128, 256).transpose(0, 2, 1, 3).reshape(NE * 128, 2048)),
        wd_l=f(np.asarray(w_down_e[0]).reshape(NE, 2, 128, D).transpose(0, 2, 1, 3).reshape(NE * 128, 2048)),
        g1b=f(np.broadcast_to(np.asarray(norm1_g[0])[None, :], (128, D))),
        g2b=f(np.broadcast_to(np.asarray(norm2_g[0])[None, :], (128, D))),
        qkg=f(np.broadcast_to(np.concatenate([np.tile(np.asarray(q_norm_g[0]), 8), np.tile(np.asarray(k_norm_g[0]), 2)])[None, :], (128, 640))),
        cwl=f(np.asarray(conv_w[0]).reshape(3, 4, 128).transpose(2, 1, 0).reshape(128, 12)),
        cbl=f(np.asarray(conv_b[0]).reshape(4, 128).transpose(1, 0)),
        sinkb=f(np.broadcast_to(np.asarray(attn_sink[0])[None, :], (2, 8))),
        brb=f(np.broadcast_to(np.concatenate([np.asarray(b_router_group[0]), np.asarray(b_router_expert[0])])[None, :], (128, 36))),
        ropeC=f(ropeC.reshape(NT, 128, 16).transpose(1, 0, 2).reshape(128, NT * 16)),
        ropeS=f(ropeS.reshape(NT, 128, 16).transpose(1, 0, 2).reshape(128, NT * 16)), cmat=cmat,
    )
    cmoe = np.zeros((128, 472), np.float32)
    cmoe[:, 464] = np.arange(128, dtype=np.float32)
    cmoe[:, 0:16] = (np.arange(16, dtype=np.float32) * RB)[None, :]
    cmoe[:, 16:16 + NB] = (np.arange(NB, dtype=np.float32) * RB)[None, :]
    tt = np.arange(128)
    cmoe[:, 80:208] = (tt[:, None] < tt[None, :]).astype(np.float32)
    cmoe[:, 208:336] = 1.0
    cmoe[:, 336:464] = np.eye(128, dtype=np.float32)
    shared["cmoe"] = cmoe
    shared["zrows"] = np.zeros((RB, D // 2), np.float32)
    in_maps = [dict(shared, x=x[b]) for b in range(n)]
    res = run_bass_kernel_spmd(nc, in_maps, core_ids=list(range(n)))
    return np.stack([np.asarray(r["out"]) for r in res.results], axis=0).astype(np.float32)

CAL.update({1001: 298, 225: 43, 363: 613, 398: 927, 405: 746, 476: 520, 422: 200, 424: 499, 479: 1246, 483: 84, 486: 1104, 496: 192, 511: 128, 498: 303, 430: 382, 431: 461, 433: 247, 435: 1159, 438: 474, 515: 115, 441: 469, 449: 352, 451: 278, 453: 237, 518: 421, 519: 383, 444: 532, 455: 368, 503: 154, 505: 408, 524: 164, 604: 338, 526: 171, 655: 309, 657: 494, 659: 512, 609: 134, 612: 1109, 621: 453, 627: 532, 635: 419, 631: 1134, 637: 459, 639: 3359, 642: 417, 644: 418, 652: 160, 667: 118, 654: 472, 671: 109, 683: 384, 684: 302, 661: 438, 675: 114, 679: 114, 685: 420, 687: 336, 689: 729, 699: 289, 701: 692, 706: 574, 709: 1238, 713: 101, 716: 1111, 536: 43, 561: 179, 562: 110, 563: 232, 564: 137, 567: 189, 565: 98, 570: 121, 566: 118, 571: 152, 578: 138, 572: 227, 573: 151, 582: 102, 574: 161, 575: 184, 576: 60, 586: 207, 577: 98, 579: 89, 580: 64, 590: 154, 581: 151, 591: 113, 802: 172, 809: 87, 811: 27, 813: 678, 828: 186, 830: 78, 841: 1130, 842: 1216, 843: 180, 859: 1110, 885: 70, 887: 1059, 893: 115, 897: 109, 901: 327, 902: 380, 904: 334, 910: 286, 913: 644, 916: 649, 168: 3057, 926: 1256, 929: 1281, 931: 1199})
```
